# Optimizing a Trainium2 kernel written in Bass

```python
import jax, jax.numpy as jnp
from jax import lax
import numpy as np

D_MODEL = 1024
BATCH = 16
SEQ = 2048
DEPTH = 2

D_FF = 2816
CHUNK = 128
A_GROUPS = 8
A_GROUP_DIM = 128
A_WIDTH = A_GROUPS * A_GROUP_DIM
B_HEAD_DIM = 64
B_WIDTH = D_MODEL
B_HEADS = B_WIDTH // B_HEAD_DIM
DECAY_LORA = 64
AAA_LORA = 64
MV_LORA = 32
GATE_WIDTH = 2 * D_MODEL
SHIFT_WIDTH_FIRST = 3 * B_WIDTH + DECAY_LORA + AAA_LORA
SHIFT_WIDTH_REST = SHIFT_WIDTH_FIRST + MV_LORA
IN_WIDTH_FIRST = GATE_WIDTH + 2 * A_WIDTH + SHIFT_WIDTH_FIRST
IN_WIDTH_REST = GATE_WIDTH + 2 * A_WIDTH + SHIFT_WIDTH_REST

NORM_EPS = 1e-6
LN_EPS = 1e-5
GN_EPS = 64e-5

kernel_name = "hybrid_gmlp_rwkv7_macaron"


def rms_norm(x, g):
    xf = x.astype(jnp.float32)
    xf = xf * lax.rsqrt(jnp.mean(xf * xf, axis=-1, keepdims=True) + NORM_EPS)
    return (xf * g).astype(x.dtype)


def layer_norm(x, g, b):
    xf = x.astype(jnp.float32)
    mu = jnp.mean(xf, axis=-1, keepdims=True)
    var = jnp.mean(jnp.square(xf - mu), axis=-1, keepdims=True)
    return ((xf - mu) * lax.rsqrt(var + LN_EPS) * g + b).astype(x.dtype)


def swiglu_ffn(h, w_gu, w_down):
    gate, up = jnp.split(h @ w_gu, 2, axis=-1)
    return (jax.nn.silu(gate) * up) @ w_down


def token_shift_mix(p, mu):
    prev = jnp.pad(p, ((0, 0), (1, 0), (0, 0)))[:, :-1]
    return p + mu * (prev - p)


def chunked_sgu(u, v, ln_g, ln_b, w_s, b_s):
    u = jax.nn.gelu(u)
    v = layer_norm(jax.nn.gelu(v), ln_g, ln_b)
    bsz, t, _ = v.shape
    vc = v.reshape(bsz, t // CHUNK, CHUNK, A_GROUPS, A_GROUP_DIM)
    causal = jnp.tril(jnp.ones((CHUNK, CHUNK), dtype=bool))
    ws = jnp.where(causal, w_s, 0)
    s = jnp.einsum('gts,bcsgd->bctgd', ws, vc) + b_s.T[:, :, None]
    return u * s.reshape(bsz, t, A_WIDTH)


def rwkv7_time_mix(p, v_first, w0, w2, a0, a2, k_k, k_a, r_k, gn_g, gn_b, v0, v2):
    out_dtype = p.dtype
    p = p.astype(jnp.float32)
    bsz, t, _ = p.shape
    r = p[..., :B_WIDTH]
    k = p[..., B_WIDTH:2 * B_WIDTH]
    v = p[..., 2 * B_WIDTH:3 * B_WIDTH]
    off = 3 * B_WIDTH
    w_down = p[..., off:off + DECAY_LORA]
    off += DECAY_LORA
    a_down = p[..., off:off + AAA_LORA]
    off += AAA_LORA
    log_w = -jax.nn.softplus(-(w0 + jnp.tanh(w_down) @ w2)) - 0.5
    decay = jnp.exp(-jnp.exp(log_w))
    a = jax.nn.sigmoid(a0 + a_down @ a2)
    if v_first is None:
        v_first = v
    else:
        v_res_down = p[..., off:off + MV_LORA]
        v = v + (v_first - v) * jax.nn.sigmoid(v0 + v_res_down @ v2)

    def heads(z):
        return z.reshape(bsz, t, B_HEADS, B_HEAD_DIM)

    kk = heads(k * k_k)
    kk = kk / jnp.maximum(jnp.sqrt(jnp.sum(kk * kk, axis=-1, keepdims=True)), 1e-12)
    k = k * (1 + (a - 1) * k_a)
    r, k, v, decay, a = heads(r), heads(k), heads(v), heads(decay), heads(a)

    def time_major(z):
        return jnp.swapaxes(z, 0, 1)

    def step(state, inp):
        r_t, w_t, k_t, v_t, kk_t, a_t = inp
        sa = jnp.einsum('bhvk,bhk->bhv', state, kk_t)
        state = (state * w_t[:, :, None, :]
                 - sa[..., None] * (kk_t * a_t)[:, :, None, :]
                 + v_t[..., None] * k_t[:, :, None, :])
        return state, jnp.einsum('bhvk,bhk->bhv', state, r_t)

    state0 = jnp.zeros((bsz, B_HEADS, B_HEAD_DIM, B_HEAD_DIM), jnp.float32)
    _, y = lax.scan(step, state0, (time_major(r), time_major(decay), time_major(k),
                                   time_major(v), time_major(kk), time_major(a)))
    y = jnp.swapaxes(y, 0, 1)
    mu = jnp.mean(y, axis=-1, keepdims=True)
    var = jnp.mean(jnp.square(y - mu), axis=-1, keepdims=True)
    y = (y - mu) * lax.rsqrt(var + GN_EPS)
    y = y.reshape(bsz, t, B_WIDTH) * gn_g + gn_b
    bonus = jnp.sum(r * k * r_k, axis=-1, keepdims=True) * v
    y = y + bonus.reshape(bsz, t, B_WIDTH)
    return y.astype(out_dtype), v_first


def setup_inputs(seed: int = 0) -> dict:
    key = jax.random.key(seed)
    ks = iter(jax.random.split(key, 40))
    nrm = lambda shape, s: jax.random.normal(next(ks), shape, jnp.float32) * s
    gain = lambda shape: 1.0 + nrm(shape, 0.02)
    nr = max(DEPTH - 1, 0)
    return {
        "x": nrm((BATCH, SEQ, D_MODEL), 1.0),
        "w_in_first": nrm((D_MODEL, IN_WIDTH_FIRST), D_MODEL ** -0.5),
        "mu_first": jax.random.uniform(next(ks), (SHIFT_WIDTH_FIRST,), jnp.float32),
        "w_in_rest": nrm((nr, D_MODEL, IN_WIDTH_REST), D_MODEL ** -0.5),
        "mu_rest": jax.random.uniform(next(ks), (nr, SHIFT_WIDTH_REST), jnp.float32),
        "rwkv_v0": 1.0 + nrm((nr, B_WIDTH), 0.1),
        "rwkv_v2": nrm((nr, MV_LORA, B_WIDTH), 0.5 * MV_LORA ** -0.5),
        "ffn1_norm": gain((DEPTH, D_MODEL)),
        "ffn1_w_gu": nrm((DEPTH, D_MODEL, 2 * D_FF), D_MODEL ** -0.5),
        "ffn1_w_down": nrm((DEPTH, D_FF, D_MODEL), D_FF ** -0.5),
        "mix_norm": gain((DEPTH, D_MODEL)),
        "sgu_ln_g": gain((DEPTH, A_WIDTH)),
        "sgu_ln_b": nrm((DEPTH, A_WIDTH), 0.02),
        "sgu_w_s": nrm((DEPTH, A_GROUPS, CHUNK, CHUNK), CHUNK ** -0.5),
        "sgu_b_s": 1.0 + nrm((DEPTH, A_GROUPS, CHUNK), 0.02),
        "rwkv_w0": jax.random.uniform(next(ks), (DEPTH, B_WIDTH), jnp.float32, -5.0, 0.0),
        "rwkv_w2": nrm((DEPTH, DECAY_LORA, B_WIDTH), 0.5 * DECAY_LORA ** -0.5),
        "rwkv_a0": nrm((DEPTH, B_WIDTH), 0.1),
        "rwkv_a2": nrm((DEPTH, AAA_LORA, B_WIDTH), 0.5 * AAA_LORA ** -0.5),
        "rwkv_k_k": 0.85 + nrm((DEPTH, B_WIDTH), 0.02),
        "rwkv_k_a": gain((DEPTH, B_WIDTH)),
        "rwkv_r_k": nrm((DEPTH, B_HEADS, B_HEAD_DIM), 0.1),
        "rwkv_gn_g": gain((DEPTH, B_WIDTH)),
        "rwkv_gn_b": nrm((DEPTH, B_WIDTH), 0.02),
        "w_proj_a": nrm((DEPTH, A_WIDTH, D_MODEL), A_WIDTH ** -0.5),
        "w_proj_b": nrm((DEPTH, B_WIDTH, D_MODEL), B_WIDTH ** -0.5),
        "w_out": nrm((DEPTH, D_MODEL, D_MODEL), D_MODEL ** -0.5),
        "ffn2_norm": gain((DEPTH, D_MODEL)),
        "ffn2_w_gu": nrm((DEPTH, D_MODEL, 2 * D_FF), D_MODEL ** -0.5),
        "ffn2_w_down": nrm((DEPTH, D_FF, D_MODEL), D_FF ** -0.5),
        "final_norm": gain((D_MODEL,)),
    }


def reference(x, w_in_first, mu_first, w_in_rest, mu_rest, rwkv_v0, rwkv_v2,
              ffn1_norm, ffn1_w_gu, ffn1_w_down, mix_norm,
              sgu_ln_g, sgu_ln_b, sgu_w_s, sgu_b_s,
              rwkv_w0, rwkv_w2, rwkv_a0, rwkv_a2, rwkv_k_k, rwkv_k_a, rwkv_r_k,
              rwkv_gn_g, rwkv_gn_b, w_proj_a, w_proj_b, w_out,
              ffn2_norm, ffn2_w_gu, ffn2_w_down, final_norm):
    v_first = None
    for layer in range(DEPTH):
        h = rms_norm(x, ffn1_norm[layer])
        x = x + 0.5 * swiglu_ffn(h, ffn1_w_gu[layer], ffn1_w_down[layer])

        h = rms_norm(x, mix_norm[layer])
        if layer == 0:
            p = h @ w_in_first
            mu = mu_first
            v0, v2 = None, None
        else:
            p = h @ w_in_rest[layer - 1]
            mu = mu_rest[layer - 1]
            v0, v2 = rwkv_v0[layer - 1], rwkv_v2[layer - 1]
        gate_a = p[..., :D_MODEL]
        gate_b = p[..., D_MODEL:GATE_WIDTH]
        u_a = p[..., GATE_WIDTH:GATE_WIDTH + A_WIDTH]
        v_a = p[..., GATE_WIDTH + A_WIDTH:GATE_WIDTH + 2 * A_WIDTH]
        p_b = token_shift_mix(p[..., GATE_WIDTH + 2 * A_WIDTH:], mu)

        y_a = chunked_sgu(u_a, v_a, sgu_ln_g[layer], sgu_ln_b[layer],
                          sgu_w_s[layer], sgu_b_s[layer])
        y_b, v_first = rwkv7_time_mix(p_b, v_first, rwkv_w0[layer], rwkv_w2[layer],
                                      rwkv_a0[layer], rwkv_a2[layer], rwkv_k_k[layer],
                                      rwkv_k_a[layer], rwkv_r_k[layer],
                                      rwkv_gn_g[layer], rwkv_gn_b[layer], v0, v2)

        merged = (jax.nn.sigmoid(gate_a) * (y_a @ w_proj_a[layer])
                  + jax.nn.sigmoid(gate_b) * (y_b @ w_proj_b[layer]))
        x = x + merged @ w_out[layer]

        h = rms_norm(x, ffn2_norm[layer])
        x = x + 0.5 * swiglu_ffn(h, ffn2_w_gu[layer], ffn2_w_down[layer])
    return rms_norm(x, final_norm)
```

```python
import contextlib
import math
import numpy as np
import concourse.bass as bass
import concourse.mybir as mybir
from concourse.bass_utils import run_bass_kernel_spmd

F32 = mybir.dt.float32
BF16 = mybir.dt.bfloat16
ALU = mybir.AluOpType
AF = mybir.ActivationFunctionType
AX = mybir.AxisListType

D = 1024
DFF = 2816
NJ = 22
TT = 512
SEQ = 2048
NCORES = 8
R_RING = 6
NORM_EPS = 1e-6
LN_EPS = 1e-5
GN_EPS = 64e-5
C0 = math.exp(-0.5)
DBG = [None]
STATS = {}

(V_N1, V_NM, V_N2, V_LNG, V_LNB, V_MUR, V_MUK, V_MUV, V_W0, V_A0, V_KK, V_KA, V_RK, V_GNG, V_GNB, V_V0,
 V_FIN, V_MUX, V_OMR, V_OMK, V_OMV, V_OMX, V_OKA) = range(23)
NV = 23 * 8

CM_ID = 0
CM_ONESMEAN = 128
CM_ONESBD = 256
CM_MLO = 384
CM_MUPS = 448
CM_MUPI = 512
CM_SCAN = 576
CM_TRIU = 1088
CM_ONES1 = 1216
NCM = 1344


class Trk:
    __slots__ = ("w", "r", "excl")

    def __init__(self, excl=False):
        self.w = None
        self.r = {}
        self.excl = excl


class Prog:
    ENG = ["pe", "act", "dve", "pool", "sp"]

    def __init__(self):
        self.q = {e: [] for e in self.ENG}
        self.cnt = {e: 0 for e in self.ENG}
        self.seen = {e: {} for e in self.ENG}
        self.dma_cnt = {}

    def _emit(self, eng, fn, deps, dma_key=None):
        sn = self.seen[eng]
        d = {}
        for (k, v) in deps:
            if k == "pe" and eng == "pe":
                continue
            if sn.get(k, 0) < v:
                d[k] = max(d.get(k, 0), v)
        for k, v in d.items():
            sn[k] = v
        if dma_key is None:
            self.cnt[eng] += 1
            tok = (eng, self.cnt[eng])
        else:
            self.dma_cnt[dma_key] = self.dma_cnt.get(dma_key, 0) + 16
            tok = (dma_key, self.dma_cnt[dma_key])
        self.q[eng].append((list(d.items()), fn, tok, dma_key is not None))
        return tok

    def op(self, eng, fn, reads=(), writes=(), dma_key=None, extra=()):
        deps = list(extra)
        for t in reads:
            if t.w is not None:
                deps.append(t.w)
            if t.excl:
                deps.extend((k, v) for k, v in t.r.items() if k != eng)
        for t in writes:
            if t.w is not None:
                deps.append(t.w)
            deps.extend(t.r.items())
        tok = self._emit(eng, fn, deps, dma_key)
        k, v = tok
        for t in reads:
            if t.r.get(k, 0) < v:
                t.r[k] = v
        for t in writes:
            t.w = tok
            t.r = {}
        return tok

    def barrier(self, engines=("pe", "act", "dve")):
        toks = [(e, c) for e, c in self.cnt.items() if c > 0 and e != "sp"]
        for e in engines:
            sn = self.seen[e]
            d = {}
            for (k, v) in toks:
                if k == e:
                    continue
                if sn.get(k, 0) < v:
                    d[k] = v
                    sn[k] = v
            if d:
                self.q[e].append((list(d.items()), None, None, False))

    def run(self, nc):
        keys = sorted(set(self.ENG) | set(self.dma_cnt.keys()))
        with contextlib.ExitStack() as st:
            sems = {k: st.enter_context(nc.semaphore("s_" + k)) for k in keys}
            block = st.enter_context(nc.Block())

            waited = set()
            for name in self.ENG:
                for waits, fn, tok, is_dma in self.q[name]:
                    for kv in waits:
                        waited.add(tuple(kv))

            def mk(name):
                def body(e):
                    pending = 0
                    last_idx = max([i for i, it in enumerate(self.q[name]) if it[1] is not None and not it[3]], default=-1)
                    for idx, (waits, fn, tok, is_dma) in enumerate(self.q[name]):
                        for k, v in waits:
                            e.wait_ge(sems[k], v)
                        if fn is None:
                            continue
                        ins = fn(e)
                        if is_dma:
                            ins.then_inc(sems[tok[0]], 16)
                        elif tuple(tok) in waited or idx == last_idx:
                            ins.then_inc(sems[tok[0]], pending + 1)
                            STATS.setdefault(name, []).append(pending + 1)
                            pending = 0
                        else:
                            pending += 1
                return body

            block.tensor(mk("pe"))
            block.scalar(mk("act"))
            block.vector(mk("dve"))
            block.gpsimd(mk("pool"))
            block.sync(mk("sp"))


def in_chunk_order_a():
    return list(range(24, 32)) + list(range(0, 8)) + list(range(16, 24))


def in_chunk_order_b(layer):
    o = [56]
    if layer > 0:
        o.append(57)
    o += list(range(8, 16))
    for c in range(8):
        o += [32 + c, 40 + c, 48 + c]
    return o


def layer_recipe(layer):
    rec = []
    for which in (1, 2):
        if which == 2:
            ca = in_chunk_order_a()
            for i in range(0, len(ca), 2):
                rec.append(("in", layer, ca[i:i + 2]))
            rec.append(("sgu", layer))
            for mp in range(4):
                rec.append(("pj", layer, "a", mp))
            rec.append(("lora", layer))
            cb = in_chunk_order_b(layer)
            for i in range(0, len(cb), 2):
                rec.append(("in", layer, cb[i:i + 2]))
            for mp in range(4):
                rec.append(("pj", layer, "b", mp))
            for mp in range(4):
                rec.append(("pj", layer, "o", mp))
        f = 1 if which == 1 else 2
        for jg in range(11):
            rec.append(("gu", layer, f, 0, jg))
            rec.append(("gu", layer, f, 1, jg))
        for a in range(2):
            for jq in range(6):
                rec.append(("dn", layer, f, a, jq))
    n_ffn = 22 + 12
    ffn1 = rec[:n_ffn]
    rest = rec[n_ffn:]
    return ffn1 + rest


def _kc(arr):
    n = arr.shape[1]
    return np.ascontiguousarray(arr.reshape(8, 128, n).transpose(1, 0, 2)).reshape(128, 8 * n)


def build_stream(inp):
    slots = []
    for layer in range(2):
        w_in = inp["w_in_first"] if layer == 0 else inp["w_in_rest"][layer - 1]
        for r in layer_recipe(layer):
            s = np.zeros((128, 2048), np.float32)
            kind = r[0]
            if kind == "gu":
                _, l, f, half, jg = r
                w = (inp["ffn1_w_gu"] if f == 1 else inp["ffn2_w_gu"])[l]
                base = half * DFF + jg * 256
                s[:] = _kc(w[:, base:base + 256])
            elif kind == "dn":
                _, l, f, a, jq = r
                w = (inp["ffn1_w_down"] if f == 1 else inp["ffn2_w_down"])[l]
                v = s.reshape(128, 4, 512)
                for i in range(4):
                    j = 4 * jq + i
                    if j < NJ:
                        v[:, i, :] = w[j * 128:(j + 1) * 128, a * 512:(a + 1) * 512]
            elif kind == "in":
                _, l, chunks = r
                v = s.reshape(128, 8, 256)
                for jj, ch in enumerate(chunks):
                    cols = w_in[:, ch * 128:min((ch + 1) * 128, w_in.shape[1])]
                    n = cols.shape[1]
                    v[:, :, jj * 128:jj * 128 + n] = cols.reshape(8, 128, n).transpose(1, 0, 2)
            elif kind == "pj":
                _, l, which, mp = r
                w = {"a": inp["w_proj_a"], "b": inp["w_proj_b"], "o": inp["w_out"]}[which][l]
                s[:] = _kc(w[:, mp * 256:(mp + 1) * 256])
            elif kind == "sgu":
                _, l = r
                ws = inp["sgu_w_s"][l]
                s[:, 0:1024] = ws.transpose(2, 0, 1).reshape(128, 1024)
                s[0, 1024:2048] = inp["sgu_b_s"][l].reshape(1024)
            elif kind == "lora":
                _, l = r
                s[0:64, 0:1024] = inp["rwkv_w2"][l]
                s[64:128, 0:1024] = inp["rwkv_a2"][l]
                if l > 0:
                    s[0:32, 1024:2048] = inp["rwkv_v2"][l - 1]
            slots.append(s)
    return np.stack(slots, 0)


def _vec8(v):
    return np.ascontiguousarray(np.asarray(v, np.float32).reshape(8, 128).T)


def build_cvec(inp):
    out = np.zeros((128, 2, NV), np.float32)
    for l in range(2):
        mu = inp["mu_first"] if l == 0 else inp["mu_rest"][l - 1]
        o = out[:, l, :]

        def put(idx, v):
            o[:, idx * 8:(idx + 1) * 8] = _vec8(v)
        put(V_N1, inp["ffn1_norm"][l]); put(V_NM, inp["mix_norm"][l]); put(V_N2, inp["ffn2_norm"][l])
        put(V_LNG, inp["sgu_ln_g"][l]); put(V_LNB, inp["sgu_ln_b"][l])
        put(V_MUR, mu[0:1024]); put(V_MUK, mu[1024:2048]); put(V_MUV, mu[2048:3072])
        put(V_W0, inp["rwkv_w0"][l]); put(V_A0, inp["rwkv_a0"][l]); put(V_KK, inp["rwkv_k_k"][l])
        put(V_KA, inp["rwkv_k_a"][l]); put(V_RK, inp["rwkv_r_k"][l].reshape(1024))
        put(V_GNG, inp["rwkv_gn_g"][l]); put(V_GNB, inp["rwkv_gn_b"][l])
        if l > 0:
            put(V_V0, inp["rwkv_v0"][l - 1])
        put(V_FIN, inp["final_norm"])
        o[:, V_MUX * 8] = mu[3072:3200]
        if l > 0:
            o[0:32, V_MUX * 8 + 1] = mu[3200:3232]
    return out.reshape(128, 2 * NV)


def build_cmat():
    m = np.zeros((128, NCM), np.float32)
    m[:, CM_ID:CM_ID + 128] = np.eye(128)
    m[:, CM_ONESMEAN:CM_ONESMEAN + 128] = 1.0 / 1024.0
    bd = np.zeros((128, 128), np.float32)
    bd[0:64, 0:64] = 1.0
    bd[64:128, 64:128] = 1.0
    m[:, CM_ONESBD:CM_ONESBD + 128] = bd
    i = (np.arange(128) % 64)[:, None]
    j = np.arange(64)[None, :]
    m[:, CM_MLO:CM_MLO + 64] = (j < i)
    m[:, CM_MUPS:CM_MUPS + 64] = (j > i)
    m[:, CM_MUPI:CM_MUPI + 64] = (j >= i)
    sc = np.ones((512,), np.float32)
    sc[::64] = 0.0
    m[:, CM_SCAN:CM_SCAN + 512] = sc[None, :]
    s = np.arange(128)[:, None]
    t = np.arange(128)[None, :]
    m[:, CM_TRIU:CM_TRIU + 128] = (t >= s)
    m[:, CM_ONES1:CM_ONES1 + 128] = 1.0
    return m


def build_program(ntiles, nlayers=2, stop_after=None):
    ntok = ntiles * TT
    nc = bass.Bass("TRN2", target_bir_lowering=False)
    x_d = nc.dram_tensor("x", [ntok, D], F32, kind="ExternalInput").ap()
    rec = [layer_recipe(l) for l in range(2)]
    nslots = sum(len(r) for r in rec)
    ws_d = nc.dram_tensor("wstream", [nslots, 128, 2048], F32, kind="ExternalInput").ap()
    cv_d = nc.dram_tensor("cvec", [128, 2 * NV], F32, kind="ExternalInput").ap()
    cm_d = nc.dram_tensor("cmat", [128, NCM], F32, kind="ExternalInput").ap()
    y_d = nc.dram_tensor("y", [ntok, D], F32, kind="ExternalOutput").ap()
    dbg16 = nc.dram_tensor("dbg16", [8, 128, 4096], BF16, kind="ExternalOutput").ap() if DBG[0] else None
    dbg32 = nc.dram_tensor("dbg32", [8, 128, 512], F32, kind="ExternalOutput").ap() if DBG[0] else None
    P = Prog()
    layer_base = [0, len(rec[0])]

    with contextlib.ExitStack() as st:
        cnt = [0]

        def sb(shape, dt=F32):
            cnt[0] += 1
            return st.enter_context(nc.sbuf_tensor("sb%d" % cnt[0], shape, dt))

        banks = []
        for i in range(8):
            banks.append(st.enter_context(nc.psum_tensor("bank%d" % i, [128, 512], F32)))
        tbank = [Trk(excl=True) for _ in range(8)]

        def bk(i):
            return banks[i][:]

        def bk16(i):
            return banks[i][:].bitcast(BF16)

        x = sb([128, 8, 512]); tx = [Trk() for _ in range(8)]
        ring = [sb([128, 2048], BF16) for _ in range(R_RING)]
        tring = [Trk() for _ in range(R_RING)]
        cvec = sb([128, 2 * NV]); tcv = Trk()
        cmat = sb([128, NCM]); tcm = Trk()
        identb = sb([128, 128], BF16); onesmeanb = sb([128, 128], BF16); onesbdb = sb([128, 128], BF16)
        ones1b = sb([1, 128], BF16)
        Sst = sb([128, 2, 8, 64], BF16); tS = [Trk(), Trk()]
        carry = sb([128, 2, 2, 26]); tcar = [[[Trk() for _ in range(26)] for _ in range(2)] for _ in range(2)]
        cur_tile = [0]
        vfirst = sb([128, 8, 512], BF16); tvf = [Trk() for _ in range(8)]
        M_h = sb([128, 8, 512], BF16)
        M_sga = sb([128, 8, 512], BF16)
        M_sgb = sb([128, 8, 512], BF16)
        M_mer = sb([128, 8, 512], BF16)
        M_yb = sb([128, 8, 512], BF16)
        M_bon = sb([128, 8, 512], BF16)
        lora = sb([128, 2048], BF16)
        U = sb([128, 10240])
        Vr = sb([128, 13312])

        def cv(l, idx, c=0, rows=slice(0, 128)):
            o = l * NV + idx * 8 + c
            return cvec[rows, o:o + 1]

        def cm(off, n, rows=slice(0, 128)):
            return cmat[rows, off:off + n]

        P.op("sp", lambda e: e.dma_start(out=cvec[:], in_=cv_d), writes=[tcv], dma_key="c0")
        P.op("sp", lambda e: e.dma_start(out=cmat[:], in_=cm_d), writes=[tcm], dma_key="c1")
        for l in range(2):
            for (src, dst) in ((V_MUR, V_OMR), (V_MUK, V_OMK), (V_MUV, V_OMV), (V_MUX, V_OMX), (V_KA, V_OKA)):
                P.op("dve", lambda e, l=l, src=src, dst=dst: e.tensor_scalar(
                    out=cvec[:, l * NV + dst * 8:l * NV + dst * 8 + 8], in0=cvec[:, l * NV + src * 8:l * NV + src * 8 + 8],
                    scalar1=-1.0, scalar2=1.0, op0=ALU.mult, op1=ALU.add), reads=[], writes=[tcv])
        P.op("dve", lambda e: e.tensor_copy(out=identb[:], in_=cm(CM_ID, 128)), reads=[tcm], writes=[tcm])
        P.op("dve", lambda e: e.tensor_copy(out=onesmeanb[:], in_=cm(CM_ONESMEAN, 128)), writes=[tcm])
        P.op("dve", lambda e: e.tensor_copy(out=onesbdb[:], in_=cm(CM_ONESBD, 128)), writes=[tcm])
        P.op("dve", lambda e: e.tensor_copy(out=ones1b[:], in_=cmat[0:1, CM_ONES1:CM_ONES1 + 128]), writes=[tcm])
        P.barrier()

        spos = [0]

        def next_slot(layer, expect):
            li = spos[0]
            assert rec[layer][li][0] == expect[0], (rec[layer][li], expect)
            spos[0] += 1
            gi = layer_base[layer] + li
            nslot[0] += 1
            s = nslot[0] % R_RING
            P.op("pool", lambda e, s=s, gi=gi: e.dma_start(out=ring[s][:], in_=ws_d[gi]), writes=[tring[s]],
                 dma_key="w%d" % s)
            return ring[s], tring[s]

        nslot = [0]

        def mm(out, lhsT, rhs, start, stop, reads, writes):
            P.op("pe", lambda e: e.matmul(out, lhsT=lhsT, rhs=rhs, start=start, stop=stop), reads=reads, writes=writes)

        def tr(out, in_, ident, reads, writes):
            P.op("pe", lambda e: e.transpose(out, in_, ident), reads=reads, writes=writes)

        def act(out, in_, func, reads, writes, bias=None, scale=None):
            kw = {}
            if bias is not None:
                kw["bias"] = bias
            if scale is not None:
                kw["scale"] = scale
            P.op("act", lambda e: e.activation(out=out, in_=in_, func=func, **kw), reads=reads, writes=writes)

        def tt(out, in0, in1, op, reads, writes, eng="dve"):
            P.op(eng, lambda e: e.tensor_tensor(out=out, in0=in0, in1=in1, op=op), reads=reads, writes=writes)

        def stt(out, in0, scalar, in1, op0, op1, reads, writes):
            P.op("dve", lambda e: e.scalar_tensor_tensor(out=out, in0=in0, scalar=scalar, in1=in1, op0=op0, op1=op1),
                 reads=reads, writes=writes)

        def tsc(out, in0, s1, s2, op0, op1, reads, writes, eng="dve"):
            P.op(eng, lambda e: e.tensor_scalar(out=out, in0=in0, scalar1=s1, scalar2=s2, op0=op0, op1=op1),
                 reads=reads, writes=writes)

        def cp(out, in_, reads, writes, eng="dve"):
            if eng == "act":
                P.op(eng, lambda e: e.activation(out=out, in_=in_, func=AF.Copy), reads=reads, writes=writes)
            else:
                P.op(eng, lambda e: e.tensor_copy(out=out, in_=in_), reads=reads, writes=writes)

        def recip(out, in_, reads, writes):
            P.op("dve", lambda e: e.reciprocal(out=out, in_=in_), reads=reads, writes=writes)

        def Uf(off, n):
            return U[:, off:off + n]

        def U16(off, n):
            return U[:, off:off + n].bitcast(BF16)

        def Vf(off, n):
            return Vr[:, off:off + n]

        def V16(off, n):
            return Vr[:, off:off + n].bitcast(BF16)

        def rmsnorm(gidx, l, h, th, tmpbase=0):
            sq = [V16(tmpbase, 256), V16(tmpbase + 256, 256)]
            tsq = [Trk(), Trk()]
            rstd = Vf(tmpbase + 512, 512); trs = Trk()
            for c in range(8):
                act(sq[c % 2], x[:, c, :], AF.Square, [tx[c]], [tsq[c % 2]])
                mm(bk(4), onesmeanb[:], sq[c % 2], c == 0, c == 7, [tsq[c % 2]], [tbank[4]])
            act(rstd, bk(4), AF.Ln, [tbank[4]], [trs], bias=NORM_EPS)
            act(rstd, rstd, AF.Exp, [trs], [trs], scale=-0.5)
            for c in range(8):
                stt(h[:, c, :], x[:, c, :], cv(l, gidx, c), rstd, ALU.mult, ALU.mult, [tx[c], trs, tcv], [th[c]])

        def ffn(l, f):
            P.barrier()
            h = U16(0, 2048).rearrange("p (c t) -> p c t", c=8)
            g = U16(2048, 5632).rearrange("p (j t) -> p j t", j=NJ)
            th = [Trk() for _ in range(8)]; tg = [Trk() for _ in range(NJ)]
            sgt = [Uf(7680, 512), Uf(8192, 512)]; tsg = [Trk(), Trk()]
            rmsnorm(V_N1 if f == 1 else V_N2, l, h, th)
            for jg in range(11):
                sg_, tsg_ = next_slot(l, ("gu",))
                su_, tsu_ = next_slot(l, ("gu",))
                sgv = sg_[:].rearrange("p (k n) -> p k n", k=8)
                suv = su_[:].rearrange("p (k n) -> p k n", k=8)
                for jj in range(2):
                    j = 2 * jg + jj
                    bg = j % 2; bu = 2 + j % 2
                    for k in range(8):
                        mm(bk(bg), sgv[:, k, jj * 128:(jj + 1) * 128], h[:, k, :], k == 0, k == 7, [tsg_, th[k]], [tbank[bg]])
                    for k in range(8):
                        mm(bk(bu), suv[:, k, jj * 128:(jj + 1) * 128], h[:, k, :], k == 0, k == 7, [tsu_, th[k]], [tbank[bu]])
                    act(sgt[j % 2], bk(bg), AF.Silu, [tbank[bg]], [tsg[j % 2]])
                    tt(g[:, j, :], sgt[j % 2], bk(bu), ALU.mult, [tsg[j % 2], tbank[bu]], [tg[j]])
            for a in range(2):
                for jq in range(6):
                    sd_, tsd_ = next_slot(l, ("dn",))
                    sdv = sd_[:].rearrange("p (i n) -> p i n", i=4)
                    for i in range(4):
                        j = 4 * jq + i
                        if j >= NJ:
                            continue
                        for m in range(4):
                            mm(bk(4 + m), sdv[:, i, m * 128:(m + 1) * 128], g[:, j, :], j == 0, j == NJ - 1,
                               [tsd_, tg[j]], [tbank[4 + m]])
                for m in range(4):
                    c = a * 4 + m
                    stt(x[:, c, :], bk(4 + m), 0.5, x[:, c, :], ALU.mult, ALU.add, [tbank[4 + m]], [tx[c]])

        def inproj(slotv, tslot, jj, h, th, b, mcols=128):
            for k in range(8):
                mm(bk(b)[0:mcols, :], slotv[:, k, jj * 128:jj * 128 + mcols], h[:, k, :], k == 0, k == 7,
                   [tslot, th[k]], [tbank[b]])

        class InStream:
            def __init__(self, l, order, h, th):
                self.l = l; self.order = order; self.h = h; self.th = th; self.i = 0
                self.slot = None

            def next(self, mcols=128):
                if self.i % 2 == 0:
                    s_, t_ = next_slot(self.l, ("in",))
                    self.slot = (s_[:].rearrange("p (k n) -> p k n", k=8), t_)
                b = self.i % 2
                inproj(self.slot[0], self.slot[1], self.i % 2, self.h, self.th, b, mcols)
                self.i += 1
                return b

        def token_shift(b, l, muidx, omidx, c, cidx, out, tout, rows=slice(0, 128)):
            pw = cur_tile[0] % 2; pr = 1 - pw
            act(out, bk(b)[rows, :], AF.Copy, [tbank[b], tcv], [tout], scale=cv(l, omidx, c, rows))
            cp(carry[rows, pw, l, cidx:cidx + 1], bk(b)[rows, 511:512], [tbank[b]], [tcar[pw][l][cidx]], eng="act")
            stt(out[:, 1:512], bk(b)[rows, 0:511], cv(l, muidx, c, rows), out[:, 1:512], ALU.mult, ALU.add,
                [tbank[b], tcv], [tout])
            stt(out[:, 0:1], carry[rows, pr, l, cidx:cidx + 1], cv(l, muidx, c, rows), out[:, 0:1], ALU.mult, ALU.add,
                [tcar[pr][l][cidx], tcv], [tout])

        def mixer(l):
            P.barrier()
            h = M_h; th = [Trk() for _ in range(8)]
            rmsnorm(V_NM, l, h, th)
            sga = M_sga; tsga = [Trk() for _ in range(8)]
            sgb = M_sgb; tsgb = [Trk() for _ in range(8)]
            mer = M_mer; tmer = [Trk() for _ in range(8)]
            ua = U16(0, 2048).rearrange("p (c t) -> p c t", c=8); tua = [Trk() for _ in range(8)]
            gv = Uf(2048, 4096).rearrange("p (c t) -> p c t", c=8); tgv = [Trk() for _ in range(8)]
            vn = U16(6144, 2048).rearrange("p (c t) -> p c t", c=8); tvn = [Trk() for _ in range(8)]
            vnT = U16(8192, 2048).rearrange("p (c t) -> p c t", c=8); tvnT = [Trk() for _ in range(8)]
            ins = InStream(l, in_chunk_order_a(), h, th)
            sq16 = [V16(1024, 256), V16(1280, 256)]; tsq16 = [Trk(), Trk()]
            gvb = [V16(1536, 256), V16(1792, 256)]; tgvb = [Trk(), Trk()]
            for c in range(8):
                b = ins.next()
                act(gv[:, c, :], bk(b), AF.Gelu_apprx_tanh, [tbank[b]], [tgv[c]])
                act(sq16[c % 2], gv[:, c, :], AF.Square, [tgv[c]], [tsq16[c % 2]])
                cp(gvb[c % 2], gv[:, c, :], [tgv[c]], [tgvb[c % 2]])
                mm(bk(4), onesmeanb[:], gvb[c % 2], c == 0, c == 7, [tgvb[c % 2]], [tbank[4]])
                mm(bk(5), onesmeanb[:], sq16[c % 2], c == 0, c == 7, [tsq16[c % 2]], [tbank[5]])
            mean = Vf(2048, 512); tmean = Trk()
            var = Vf(2560, 512); tvar = Trk()
            act(mean, bk(4), AF.Copy, [tbank[4]], [tmean])
            tt(var, mean, mean, ALU.mult, [tmean], [tvar])
            tt(var, bk(5), var, ALU.subtract, [tbank[5], tvar], [tvar])
            act(var, var, AF.Ln, [tvar], [tvar], bias=LN_EPS)
            act(var, var, AF.Exp, [tvar], [tvar], scale=-0.5)
            tmpl = [Vf(3072, 512), Vf(3584, 512)]; ttmpl = [Trk(), Trk()]
            for c in range(8):
                t_ = tmpl[c % 2]; tt_ = ttmpl[c % 2]
                tt(t_, gv[:, c, :], mean, ALU.subtract, [tgv[c], tmean], [tt_])
                tt(t_, t_, var, ALU.mult, [tt_, tvar], [tt_])
                act(vn[:, c, :], t_, AF.Identity, [tt_, tcv], [tvn[c]], bias=cv(l, V_LNB, c), scale=cv(l, V_LNG, c))
                b = ins.next()
                act(sga[:, c, :], bk(b), AF.Sigmoid, [tbank[b]], [tsga[c]])
            for c in range(8):
                b = ins.next()
                act(ua[:, c, :], bk(b), AF.Gelu_apprx_tanh, [tbank[b]], [tua[c]])
            ssg, tssg = next_slot(l, ("sgu",))
            wsT = V16(4096, 512).rearrange("p (g t) -> p g t", g=8); twsT = Trk()
            tt(wsT, ssg[:, 0:1024].rearrange("p (g t) -> p g t", g=8),
               cm(CM_TRIU, 128).unsqueeze(1).broadcast_to([128, 8, 128]), ALU.mult, [tssg, tcm], [twsT])
            for c in range(8):
                b = 6 + (c // 2) % 2
                off = (c % 2) * 512
                for cc in range(4):
                    tr(bk16(b)[:, off + cc * 128:off + (cc + 1) * 128], vn[:, c, cc * 128:(cc + 1) * 128], identb[:],
                       [tvn[c], tcm], [tbank[b]])
                cp(vnT[:, c, :], bk16(b)[:, off:off + 512], [tbank[b]], [tvnT[c]], eng="act" if c % 2 else "dve")
            for c in range(8):
                b = 2 + c % 2
                for cc in range(4):
                    mm(bk(b)[:, cc * 128:(cc + 1) * 128], vnT[:, c, cc * 128:(cc + 1) * 128], wsT[:, c, :], True, False,
                       [tvnT[c], twsT], [tbank[b]])
                    mm(bk(b)[:, cc * 128:(cc + 1) * 128], ones1b[0:1, :], ssg[0:1, 1024 + c * 128:1024 + (c + 1) * 128],
                       False, True, [tssg, tcm], [tbank[b]])
                tt(ua[:, c, :], bk(b), ua[:, c, :], ALU.mult, [tbank[b]], [tua[c]])
            for mp in range(4):
                sp_, tsp_ = next_slot(l, ("pj",))
                spv = sp_[:].rearrange("p (k n) -> p k n", k=8)
                for mm_ in range(2):
                    m = 2 * mp + mm_
                    b = m % 2
                    for c in range(8):
                        mm(bk(b), spv[:, c, mm_ * 128:(mm_ + 1) * 128], ua[:, c, :], c == 0, c == 7, [tsp_, tua[c]], [tbank[b]])
                    tt(mer[:, m, :], bk(b), sga[:, m, :], ALU.mult, [tbank[b], tsga[m]], [tmer[m]])
            if DBG[0] == 'sgu':
                spos[0] += 1 + (len(in_chunk_order_b(l)) + 1) // 2 + 4
            if DBG[0] != 'sgu':
                P.barrier()
                til = [U16(i * 2048, 2048).rearrange("p (c t) -> p c t", c=8) for i in range(5)]
                ttil = [[Trk() for _ in range(8)] for _ in range(5)]
                Rt, Kt, Bt, At, Vt = til
                bon = M_bon; tbon = [Trk() for _ in range(8)]
                TA = [Vf(i * 512, 512) for i in range(11)]; tTA = [Trk() for _ in range(11)]
                TB = [Vf(7680 + i * 512, 512) for i in range(11)]; tTB = [Trk() for _ in range(11)]
                T = TA; tT = tTA
                sqb = V16(5632, 256); tsqb = Trk()
                rkb = V16(5888, 256); trkb = Trk()
                twad = V16(6144, 256); ttw = Trk()
                vres = V16(6400, 256); tvres = Trk()
                PC = Vf(6656, 64).rearrange("p (c q) -> p c q", c=8); tPC = Trk()
                slo_r, tslo_r = next_slot(l, ("lora",))
                slo = lora; tslo = Trk()
                cp(lora[:], slo_r[:], [tslo_r], [tslo])
                ins = InStream(l, in_chunk_order_b(l), h, th)
                b = ins.next()
                token_shift(b, l, V_MUX, V_OMX, 0, 24, T[0], tT[0])
                act(twad[0:64, :], T[0][0:64, :], AF.Tanh, [tT[0]], [ttw])
                act(twad[64:128, :], T[0][64:128, :], AF.Copy, [tT[0]], [ttw])
                if l > 0:
                    b = ins.next(mcols=32)
                    token_shift(b, l, V_MUX, V_OMX, 1, 25, T[1][0:32, :], tT[1], rows=slice(0, 32))
                    cp(vres[0:32, :], T[1][0:32, :], [tT[1]], [tvres])
                for c in range(8):
                    b = ins.next()
                    act(sgb[:, c, :], bk(b), AF.Sigmoid, [tbank[b]], [tsgb[c]])
                def stageA(c):
                        T, tT = (TA, tTA) if c % 2 == 0 else (TB, tTB)
                        rb, kb, vb = T[0], T[1], T[2]
                        b = ins.next(); token_shift(b, l, V_MUR, V_OMR, c, c, rb, tT[0])
                        b = ins.next(); token_shift(b, l, V_MUK, V_OMK, c, 8 + c, kb, tT[1])
                        b = ins.next(); token_shift(b, l, V_MUV, V_OMV, c, 16 + c, vb, tT[2])
                        mm(bk(2), slo[0:64, c * 128:(c + 1) * 128], twad[0:64, :], True, True, [tslo, ttw], [tbank[2]])
                        mm(bk(3), slo[64:128, c * 128:(c + 1) * 128], twad[64:128, :], True, True, [tslo, ttw], [tbank[3]])
                        act(T[3], bk(2), AF.Sigmoid, [tbank[2], tcv], [tT[3]], bias=cv(l, V_W0, c))
                        act(T[4], bk(3), AF.Sigmoid, [tbank[3], tcv], [tT[4]], bias=cv(l, V_A0, c))
                        P.op("dve", lambda e, o_=T[5], i_=T[3]: e.tensor_tensor_scan(out=o_, data0=cm(CM_SCAN, 512), data1=i_, initial=0.0,
                                                                   op0=ALU.mult, op1=ALU.add), reads=[tT[3], tcm], writes=[tT[5]])
                        tt(T[6], T[5], T[3], ALU.subtract, [tT[5], tT[3]], [tT[6]])
                        act(T[7], T[5], AF.Exp, [tT[5]], [tT[7]], scale=-C0)
                        act(T[8], T[5], AF.Exp, [tT[5]], [tT[8]], scale=C0)
                        act(T[6], T[6], AF.Exp, [tT[6]], [tT[6]], scale=-C0)
                        cp(PC[:, c, :], T[7].rearrange("p (q t) -> p q t", q=8)[:, :, 63], [tT[7]], [tPC])
                        act(T[9], kb, AF.Copy, [tT[1], tcv], [tT[9]], scale=cv(l, V_KK, c))
                        act(sqb, T[9], AF.Square, [tT[9]], [tsqb])
                        mm(bk(4), onesbdb[:], sqb, True, True, [tsqb, tcm], [tbank[4]])
                        tsc(T[10], bk(4), 1e-18, None, ALU.max, ALU.bypass, [tbank[4]], [tT[10]])
                        act(T[10], T[10], AF.Ln, [tT[10]], [tT[10]])
                        act(T[10], T[10], AF.Exp, [tT[10]], [tT[10]], scale=-0.5)
                        tt(T[9], T[9], T[10], ALU.mult, [tT[9], tT[10]], [tT[9]])
                        if l > 0:
                            mm(bk(5)[:, :], slo[0:32, 1024 + c * 128:1024 + (c + 1) * 128], vres[0:32, :], True, True,
                               [tslo, tvres], [tbank[5]])
                            act(T[5], bk(5), AF.Sigmoid, [tbank[5], tcv], [tT[5]], bias=cv(l, V_V0, c))
                def stageB(c):
                        T, tT = (TA, tTA) if c % 2 == 0 else (TB, tTB)
                        rb, kb, vb = T[0], T[1], T[2]
                        tsc(T[3], T[4], cv(l, V_KA, c), cv(l, V_OKA, c), ALU.mult, ALU.add, [tT[4], tcv], [tT[3]])
                        tt(T[3], kb, T[3], ALU.mult, [tT[1], tT[3]], [tT[3]])
                        tt(T[10], T[9], T[4], ALU.mult, [tT[9], tT[4]], [tT[10]])
                        if l > 0:
                            tt(T[4], vfirst[:, c, :], vb, ALU.subtract, [tvf[c], tT[2]], [tT[4]])
                            tt(T[4], T[4], T[5], ALU.mult, [tT[4], tT[5]], [tT[4]])
                            tt(vb, vb, T[4], ALU.add, [tT[2], tT[4]], [tT[2]])
                        else:
                            cp(vfirst[:, c, :], vb, [tT[2]], [tvf[c]], eng="act")
                        stt(rkb, rb, cv(l, V_RK, c), T[3], ALU.mult, ALU.mult, [tT[0], tT[3], tcv], [trkb])
                        mm(bk(6), onesbdb[:], rkb, True, True, [trkb, tcm], [tbank[6]])
                        tt(bon[:, c, :], bk(6), vb, ALU.mult, [tbank[6], tT[2]], [tbon[c]])
                        tt(Rt[:, c, :], rb, T[7], ALU.mult, [tT[0], tT[7]], [ttil[0][c]])
                        tt(Kt[:, c, :], T[3], T[8], ALU.mult, [tT[3], tT[8]], [ttil[1][c]])
                        tt(Bt[:, c, :], T[10], T[8], ALU.mult, [tT[10], tT[8]], [ttil[2][c]])
                        stt(At[:, c, :], T[9], -1.0, T[6], ALU.mult, ALU.mult, [tT[9], tT[6]], [ttil[3][c]])
                        cp(Vt[:, c, :], vb, [tT[2]], [ttil[4][c]], eng="act")
                stageA(0)
                for c in range(1, 8):
                    stageA(c)
                    stageB(c - 1)
                stageB(7)
            if DBG[0] == 'prep':
                spos[0] += 4
                for i in range(5):
                    tok = P.op("sp", lambda e, i=i: e.dma_start(out=dbg16[i], in_=U[:, i * 2048:(i + 1) * 2048].bitcast(BF16)),
                               reads=ttil[i], dma_key="dbg")
                    P.q["sp"].append(([tok], None, None, False))
                for i in range(8):
                    tok = P.op("sp", lambda e, i=i: e.dma_start(out=dbg32[i], in_=T[3 + i]), reads=[tT[3 + i]], dma_key="dbg")
                    P.q["sp"].append(([tok], None, None, False))
            if DBG[0] not in ('sgu', 'prep'):
                P.barrier()
                def bd(off):
                    return V16(off, 512).rearrange("p (c n) -> p c n", c=8)
                def st64(off):
                    return V16(off, 256).rearrange("p (c n) -> p c n", c=8)
                Abd = [bd(0), bd(512)]; Bbd = [bd(1024), bd(1536)]; Ttb = [bd(2048), bd(2560)]
                tAbd = [[Trk(), Trk()], [Trk(), Trk()]]; tBbd = [[Trk(), Trk()], [Trk(), Trk()]]
                tTt = [[Trk(), Trk()], [Trk(), Trk()]]
                AkT = st64(3072); ArbT = st64(3328); ArkT = st64(3584)
                Kst = st64(3840); Bst = st64(4096); Vst = st64(4352); Xs = st64(4608); Us = st64(4864); ynb = st64(5120)
                tAk, tArb, tArk, tKst, tBst, tVst, tXs, tUs, tynb = [Trk() for _ in range(9)]
                ysb = Vf(6144, 512); tysb = Trk()
                stat = Vf(6720, 64); tstat = Trk()
                ysq = Vf(6784, 512); tysq = Trk()
                P.op("dve", lambda e: e.memset(Abd[0], 0.0), writes=tAbd[0])
                P.op("dve", lambda e: e.memset(Bbd[0], 0.0), writes=tBbd[0])
                S = Sst[:, l, :, :]
                R0 = slice(0, 64); R1 = slice(64, 128)
                RW = (R0, R1)
                idb = identb[:].unsqueeze(1).broadcast_to([128, 8, 128])
                v3 = lambda ap, n=8: ap.rearrange("p (c n) -> p c n", c=n)
                for q in range(8):
                    cs = slice(q * 64, (q + 1) * 64)
                    prods = [(3, 2, 0), (2, 3, 1), (1, 3, 2), (2, 0, 3), (1, 0, 4)]
                    for hh in range(2):
                        rows = RW[hh]
                        for (li, ri, b) in prods:
                            for c in range(8):
                                mm(bk(b)[rows, c * 64:(c + 1) * 64], til[li][rows, c, cs], til[ri][rows, c, cs], True, True,
                                   [ttil[li][c], ttil[ri][c]], [tbank[b]])
                    for hh in range(2):
                        rows = RW[hh]
                        tt(Abd[0][rows, :, hh * 64:(hh + 1) * 64], v3(bk(0)[rows, :]),
                           cm(CM_MLO, 64, rows).unsqueeze(1).broadcast_to([64, 8, 64]), ALU.mult, [tbank[0], tcm], tAbd[0])
                        tt(Bbd[0][rows, :, hh * 64:(hh + 1) * 64], v3(bk(1)[rows, :]),
                           cm(CM_MUPS, 64, rows).unsqueeze(1).broadcast_to([64, 8, 64]), ALU.mult, [tbank[1], tcm], tBbd[0])
                    for (dst, tdst, moff, b) in ((AkT, tAk, CM_MUPS, 2), (ArbT, tArb, CM_MUPI, 3), (ArkT, tArk, CM_MUPI, 4)):
                        tt(dst, v3(bk(b)), cm(moff, 64).unsqueeze(1).broadcast_to([128, 8, 64]), ALU.mult, [tbank[b], tcm], [tdst])
                    for hh in range(2):
                        rows = RW[hh]
                        for (src, tsrc, b, off) in ((Kt, ttil[1], 5, 0), (Bt, ttil[2], 5, 512), (Vt, ttil[4], 6, 0)):
                            for c in range(8):
                                tr(bk16(b)[rows, off + c * 64:off + (c + 1) * 64], src[rows, c, cs], identb[rows, rows],
                                   [tsrc[c], tcm], [tbank[b]])
                    cp(Kst, v3(bk16(5)[:, 0:512]), [tbank[5]], [tKst], eng="act")
                    cp(Bst, v3(bk16(5)[:, 512:1024]), [tbank[5]], [tBst], eng="act")
                    cp(Vst, v3(bk16(6)[:, 0:512]), [tbank[6]], [tVst], eng="act")
                    cur = 0
                    bT = (0, 3); bA = (1, 4); bB = (2, 7)
                    for lev in range(6):
                        nxt = 1 - cur
                        last = lev == 5
                        for half in range(2):
                            for ci in range(4):
                                c = half * 4 + ci
                                osl = slice(ci * 128, (ci + 1) * 128)
                                if lev > 0:
                                    mm(bk(bT[half])[:, osl], Abd[cur][:, c, :], Ttb[cur][:, c, :], True, True,
                                       [tAbd[cur][half], tTt[cur][half]], [tbank[bT[half]]])
                                if not last:
                                    mm(bk(bA[half])[:, osl], Bbd[cur][:, c, :], Abd[cur][:, c, :], True, True,
                                       [tAbd[cur][half], tBbd[cur][half]], [tbank[bA[half]]])
                                    mm(bk(bB[half])[:, osl], Abd[cur][:, c, :], Bbd[cur][:, c, :], True, True,
                                       [tAbd[cur][half], tBbd[cur][half]], [tbank[bB[half]]])
                        if lev == 0:
                            tt(Ttb[nxt], Bbd[0], idb, ALU.add, tBbd[0] + [tcm], tTt[nxt])
                        for half in range(2):
                            hs = slice(half * 4, half * 4 + 4)
                            if lev > 0:
                                tt(Ttb[nxt][:, hs, :], Ttb[cur][:, hs, :], v3(bk(bT[half]), 4), ALU.add,
                                   [tTt[cur][half], tbank[bT[half]]], [tTt[nxt][half]])
                            if not last:
                                cp(Abd[nxt][:, hs, :], v3(bk(bA[half]), 4), [tbank[bA[half]]], [tAbd[nxt][half]], eng="act")
                                cp(Bbd[nxt][:, hs, :], v3(bk(bB[half]), 4), [tbank[bB[half]]], [tBbd[nxt][half]],
                                   eng="dve" if half == 0 else "act")
                        cur = nxt
                    Tfin = Ttb[cur]; tTfin = tTt[cur]
                    def x_terms(hh):
                        rows = RW[hh]
                        for c in range(8):
                            osl = slice(c * 64, (c + 1) * 64)
                            mm(bk(5)[rows, osl], AkT[rows, c, :], Vst[rows, c, :], c == 0, False, [tAk, tVst], [tbank[5]])
                            mm(bk(5)[rows, osl], At[rows, c, cs], S[rows, c, :], False, True, [ttil[3][c], tS[l]], [tbank[5]])
                    def y1_terms(hh):
                        rows = RW[hh]
                        for c in range(8):
                            osl = slice(c * 64, (c + 1) * 64)
                            mm(bk(6)[rows, osl], ArkT[rows, c, :], Vst[rows, c, :], c == 0, False, [tArk, tVst], [tbank[6]])
                            mm(bk(6)[rows, osl], Rt[rows, c, cs], S[rows, c, :], False, False, [ttil[0][c], tS[l]], [tbank[6]])
                    def s1_terms(hh):
                        rows = RW[hh]
                        for c in range(8):
                            osl = slice(c * 64, (c + 1) * 64)
                            mm(bk(0)[rows, osl], Kst[rows, c, :], Vst[rows, c, :], c == 0, False, [tKst, tVst], [tbank[0]])
                            mm(bk(0)[rows, osl], identb[rows, rows], S[rows, c, :], False, False, [tcm, tS[l]], [tbank[0]])
                    def ys2_terms(hh):
                        rows = RW[hh]
                        for c in range(8):
                            osl = slice(c * 64, (c + 1) * 64)
                            mm(bk(6)[rows, osl], ArbT[rows, c, :], Us[rows, c, :], False, True, [tArb, tUs], [tbank[6]])
                        for c in range(8):
                            osl = slice(c * 64, (c + 1) * 64)
                            mm(bk(0)[rows, osl], Bst[rows, c, :], Us[rows, c, :], False, True, [tBst, tUs], [tbank[0]])
                    x_terms(0); y1_terms(0); s1_terms(0)
                    x_terms(1); y1_terms(1); s1_terms(1)
                    cp(Xs, v3(bk(5)), [tbank[5]], [tXs])
                    for c in range(8):
                        osl = slice(c * 64, (c + 1) * 64)
                        mm(bk(7)[:, osl], Tfin[:, c, :], Xs[:, c, :], True, True, [tTfin[c // 4], tXs], [tbank[7]])
                    cp(Us, v3(bk(7)), [tbank[7]], [tUs], eng="act")
                    ys2_terms(0); ys2_terms(1)
                    tt(S, v3(bk(0)), PC[:, :, q:q + 1].broadcast_to([128, 8, 64]), ALU.mult, [tbank[0], tPC], [tS[l]])
                    act(ysb, bk(6), AF.Copy, [tbank[6]], [tysb])
                    act(ysq, bk(6), AF.Square, [tbank[6]], [tysq])
                    s1 = stat[:, 0:8]; s2 = stat[:, 8:16]; mean = stat[:, 16:24]; msq = stat[:, 24:32]; var = stat[:, 32:40]
                    P.op("dve", lambda e, s1=s1: e.tensor_reduce(out=s1, in_=v3(ysb), axis=AX.X, op=ALU.add),
                         reads=[tysb], writes=[tstat])
                    P.op("dve", lambda e, s2=s2: e.tensor_reduce(out=s2, in_=v3(ysq), axis=AX.X, op=ALU.add),
                         reads=[tysq, tstat], writes=[tstat])
                    tsc(mean, s1, 1.0 / 64, None, ALU.mult, ALU.bypass, [tstat], [tstat])
                    tt(msq, mean, mean, ALU.mult, [tstat], [tstat])
                    stt(var, s2, 1.0 / 64, msq, ALU.mult, ALU.subtract, [tstat], [tstat])
                    act(var, var, AF.Sqrt, [tstat], [tstat], bias=GN_EPS)
                    recip(var, var, [tstat], [tstat])
                    y3 = v3(ysb)
                    tt(y3, y3, mean.unsqueeze(2).broadcast_to([128, 8, 64]), ALU.subtract, [tysb, tstat], [tysb])
                    tt(ynb, y3, var.unsqueeze(2).broadcast_to([128, 8, 64]), ALU.mult, [tysb, tstat], [tynb])
                    for hh in range(2):
                        rows = RW[hh]
                        b = 7 if hh == 0 else 5
                        for c in range(8):
                            tr(bk16(b)[rows, 512 + c * 64:512 + (c + 1) * 64], ynb[rows, c, :], identb[rows, rows], [tynb, tcm], [tbank[b]])
                        cp(M_yb[rows, :, cs], v3(bk16(b)[rows, 512:1024]), [tbank[b]], [tyb_all], eng="act")
                tmpa = [ysb, ysq]
                ttmpa = [tysb, tysq]
                for c in range(8):
                    t_ = tmpa[c % 2]; tt__ = ttmpa[c % 2]
                    act(t_, M_yb[:, c, :], AF.Identity, [tyb_all, tcv], [tt__], bias=cv(l, V_GNB, c), scale=cv(l, V_GNG, c))
                    tt(M_yb[:, c, :], t_, bon[:, c, :], ALU.add, [tt__, tbon[c]], [tybc[c]])
                for mp in range(4):
                    sp_, tsp_ = next_slot(l, ("pj",))
                    spv = sp_[:].rearrange("p (k n) -> p k n", k=8)
                    for mm_ in range(2):
                        m = 2 * mp + mm_
                        b = m % 2
                        for c in range(8):
                            mm(bk(b), spv[:, c, mm_ * 128:(mm_ + 1) * 128], M_yb[:, c, :], c == 0, c == 7, [tsp_, tybc[c]], [tbank[b]])
                        t_ = tmpa[m % 2]; tt__ = ttmpa[m % 2]
                        tt(t_, bk(b), sgb[:, m, :], ALU.mult, [tbank[b], tsgb[m]], [tt__])
                        tt(mer[:, m, :], t_, mer[:, m, :], ALU.add, [tt__], [tmer[m]])
            for mp in range(4):
                sp_, tsp_ = next_slot(l, ("pj",))
                spv = sp_[:].rearrange("p (k n) -> p k n", k=8)
                for mm_ in range(2):
                    m = 2 * mp + mm_
                    b = 2 + m % 2
                    for c in range(8):
                        mm(bk(b), spv[:, c, mm_ * 128:(mm_ + 1) * 128], mer[:, c, :], c == 0, c == 7, [tsp_, tmer[c]], [tbank[b]])
                    tt(x[:, m, :], bk(b), x[:, m, :], ALU.add, [tbank[b]], [tx[m]])

        tyb_all = Trk()
        tQ1g = Trk(); tP1g = Trk()
        tybc = [Trk() for _ in range(8)]

        tiles_per_seq = SEQ // TT
        xt = Uf(0, 4096).rearrange("p (s d) -> p s d", s=4)
        yf = Uf(4096, 4096).rearrange("p (c t) -> p c t", c=8)
        for tile in range(ntiles):
            cur_tile[0] = tile
            P.barrier()
            txt = Trk()
            if tile % tiles_per_seq == 0:
                P.op("dve", lambda e: e.memset(Sst[:].rearrange("p a c n -> p (a c n)"), 0.0), writes=[tS[0], tS[1]])
                P.op("dve", lambda e: e.memset(carry[:].rearrange("p w a c -> p (w a c)"), 0.0),
                     writes=[t for w_ in tcar for l_ in w_ for t in l_])
            P.op("sp", lambda e, tile=tile: e.dma_start(
                out=xt, in_=x_d[tile * TT:(tile + 1) * TT, :].rearrange("(s p) d -> p s d", p=128)),
                writes=[txt], dma_key="xin")
            for c in range(8):
                b = c % 2
                for s in range(4):
                    tr(bk(b)[:, s * 128:(s + 1) * 128], xt[:, s, c * 128:(c + 1) * 128], cm(CM_ID, 128), [txt, tcm], [tbank[b]])
                cp(x[:, c, :], bk(b), [tbank[b]], [tx[c]], eng="act" if c % 2 else "dve")
            for l in range(nlayers):
                spos[0] = 0
                ffn(l, 1)
                if stop_after == ("ffn1", l):
                    break
                mixer(l)
                if stop_after == ("mixer", l):
                    break
                ffn(l, 2)
            P.barrier()
            sqf = [V16(0, 256), V16(256, 256)]; tsqf = [Trk(), Trk()]
            rstd = Vf(512, 512); trs = Trk()
            tyf = [Trk() for _ in range(8)]
            if stop_after is None:
                for c in range(8):
                    act(sqf[c % 2], x[:, c, :], AF.Square, [tx[c]], [tsqf[c % 2]])
                    mm(bk(4), onesmeanb[:], sqf[c % 2], c == 0, c == 7, [tsqf[c % 2]], [tbank[4]])
                act(rstd, bk(4), AF.Ln, [tbank[4]], [trs], bias=NORM_EPS)
                act(rstd, rstd, AF.Exp, [trs], [trs], scale=-0.5)
                for c in range(8):
                    stt(yf[:, c, :], x[:, c, :], cv(0, V_FIN, c), rstd, ALU.mult, ALU.mult, [tx[c], trs, tcv], [tyf[c]])
            else:
                for c in range(8):
                    cp(yf[:, c, :], x[:, c, :], [tx[c]], [tyf[c]])
            txo = Trk()
            for s in range(4):
                for hf in range(2):
                    b = 2 + hf
                    for ci in range(4):
                        c = hf * 4 + ci
                        tr(bk(b)[:, ci * 128:(ci + 1) * 128], yf[:, c, s * 128:(s + 1) * 128], cm(CM_ID, 128), [tyf[c], tcm], [tbank[b]])
                    cp(xt[:, s, hf * 512:(hf + 1) * 512], bk(b), [tbank[b]], [txo], eng="act" if hf else "dve")
            tok = P.op("sp", lambda e, tile=tile: e.dma_start(
                out=y_d[tile * TT:(tile + 1) * TT, :].rearrange("(s p) d -> p s d", p=128), in_=xt),
                reads=[txo], dma_key="xout")
            P.q["sp"].append(([tok], None, None, False))
        P.run(nc)
    return nc


_CACHE = {}


def kernel(**inputs):
    inp = {k: np.asarray(v) for k, v in inputs.items()}
    x = inp["x"].astype(np.float32)
    B = x.shape[0]
    per = B // NCORES
    ntiles = per * SEQ // TT
    wstream = build_stream(inp)
    cvec = build_cvec(inp)
    cmat = build_cmat()
    if "nc" not in _CACHE:
        _CACHE["nc"] = build_program(ntiles)
    nc = _CACHE["nc"]
    in_maps = []
    for i in range(NCORES):
        xs = np.ascontiguousarray(x[i * per:(i + 1) * per].reshape(per * SEQ, D))
        in_maps.append({"x": xs, "wstream": wstream, "cvec": cvec, "cmat": cmat})
    res = run_bass_kernel_spmd(nc, in_maps, core_ids=list(range(NCORES)))
    out = np.concatenate([r["y"].reshape(per, SEQ, D) for r in res.results], axis=0)
    return out.astype(np.float32)
```

```python
import contextlib
import math
import numpy as np
import concourse.bass as bass
import concourse.mybir as mybir
from concourse.bass_utils import run_bass_kernel_spmd

F32 = mybir.dt.float32
BF16 = mybir.dt.bfloat16
ALU = mybir.AluOpType
AF = mybir.ActivationFunctionType
AX = mybir.AxisListType

D = 1024
DFF = 2816
NJ = 22
TT = 512
SEQ = 2048
NCORES = 8
R_RING = 6
NORM_EPS = 1e-6
LN_EPS = 1e-5
GN_EPS = 64e-5
C0 = math.exp(-0.5)
DBG = [None]
STATS = {}

(V_N1, V_NM, V_N2, V_LNG, V_LNB, V_MUR, V_MUK, V_MUV, V_W0, V_A0, V_KK, V_KA, V_RK, V_GNG, V_GNB, V_V0,
 V_FIN, V_MUX, V_OMR, V_OMK, V_OMV, V_OMX, V_OKA) = range(23)
NV = 23 * 8

CM_ID = 0
CM_ONESMEAN = 128
CM_ONESBD = 256
CM_MLO = 384
CM_MUPS = 448
CM_MUPI = 512
CM_SCAN = 576
CM_TRIU = 1088
CM_ONES1 = 1216
NCM = 1344


class Trk:
    __slots__ = ("w", "r", "excl")

    def __init__(self, excl=False):
        self.w = None
        self.r = {}
        self.excl = excl


class Prog:
    ENG = ["pe", "act", "dve", "pool", "sp"]

    def __init__(self):
        self.q = {e: [] for e in self.ENG}
        self.cnt = {e: 0 for e in self.ENG}
        self.seen = {e: {} for e in self.ENG}
        self.dma_cnt = {}

    def _emit(self, eng, fn, deps, dma_key=None):
        sn = self.seen[eng]
        d = {}
        for (k, v) in deps:
            if k == "pe" and eng == "pe":
                continue
            if sn.get(k, 0) < v:
                d[k] = max(d.get(k, 0), v)
        for k, v in d.items():
            sn[k] = v
        if dma_key is None:
            self.cnt[eng] += 1
            tok = (eng, self.cnt[eng])
        else:
            self.dma_cnt[dma_key] = self.dma_cnt.get(dma_key, 0) + 16
            tok = (dma_key, self.dma_cnt[dma_key])
        self.q[eng].append((list(d.items()), fn, tok, dma_key is not None))
        return tok

    def op(self, eng, fn, reads=(), writes=(), dma_key=None, extra=()):
        deps = list(extra)
        for t in reads:
            if t.w is not None:
                deps.append(t.w)
            if t.excl:
                deps.extend((k, v) for k, v in t.r.items() if k != eng)
        for t in writes:
            if t.w is not None:
                deps.append(t.w)
            deps.extend(t.r.items())
        tok = self._emit(eng, fn, deps, dma_key)
        k, v = tok
        for t in reads:
            if t.r.get(k, 0) < v:
                t.r[k] = v
        for t in writes:
            t.w = tok
            t.r = {}
        return tok

    def barrier(self, engines=("pe", "act", "dve")):
        toks = [(e, c) for e, c in self.cnt.items() if c > 0 and e != "sp"]
        for e in engines:
            sn = self.seen[e]
            d = {}
            for (k, v) in toks:
                if k == e:
                    continue
                if sn.get(k, 0) < v:
                    d[k] = v
                    sn[k] = v
            if d:
                self.q[e].append((list(d.items()), None, None, False))

    def run(self, nc):
        keys = sorted(set(self.ENG) | set(self.dma_cnt.keys()))
        with contextlib.ExitStack() as st:
            sems = {k: st.enter_context(nc.semaphore("s_" + k)) for k in keys}
            block = st.enter_context(nc.Block())

            waited = set()
            for name in self.ENG:
                for waits, fn, tok, is_dma in self.q[name]:
                    for kv in waits:
                        waited.add(tuple(kv))

            def mk(name):
                def body(e):
                    pending = 0
                    last_idx = max([i for i, it in enumerate(self.q[name]) if it[1] is not None and not it[3]], default=-1)
                    for idx, (waits, fn, tok, is_dma) in enumerate(self.q[name]):
                        for k, v in waits:
                            e.wait_ge(sems[k], v)
                        if fn is None:
                            continue
                        ins = fn(e)
                        if is_dma:
                            ins.then_inc(sems[tok[0]], 16)
                        elif tuple(tok) in waited or idx == last_idx:
                            ins.then_inc(sems[tok[0]], pending + 1)
                            STATS.setdefault(name, []).append(pending + 1)
                            pending = 0
                        else:
                            pending += 1
                return body

            block.tensor(mk("pe"))
            block.scalar(mk("act"))
            block.vector(mk("dve"))
            block.gpsimd(mk("pool"))
            block.sync(mk("sp"))


def in_chunk_order_a():
    return list(range(24, 32)) + list(range(0, 8)) + list(range(16, 24))


def in_chunk_order_b(layer):
    o = [56]
    if layer > 0:
        o.append(57)
    o += list(range(8, 16))
    for c in range(8):
        o += [32 + c, 40 + c, 48 + c]
    return o


def layer_recipe(layer):
    rec = []
    for which in (1, 2):
        if which == 2:
            ca = in_chunk_order_a()
            for i in range(0, len(ca), 2):
                rec.append(("in", layer, ca[i:i + 2]))
            rec.append(("sgu", layer))
            for mp in range(4):
                rec.append(("pj", layer, "a", mp))
            rec.append(("lora", layer))
            cb = in_chunk_order_b(layer)
            for i in range(0, len(cb), 2):
                rec.append(("in", layer, cb[i:i + 2]))
            for mp in range(4):
                rec.append(("pj", layer, "b", mp))
            for mp in range(4):
                rec.append(("pj", layer, "o", mp))
        f = 1 if which == 1 else 2
        for jg in range(11):
            rec.append(("gu", layer, f, 0, jg))
            rec.append(("gu", layer, f, 1, jg))
        for a in range(2):
            for jq in range(6):
                rec.append(("dn", layer, f, a, jq))
    n_ffn = 22 + 12
    ffn1 = rec[:n_ffn]
    rest = rec[n_ffn:]
    return ffn1 + rest


def _kc(arr):
    n = arr.shape[1]
    return np.ascontiguousarray(arr.reshape(8, 128, n).transpose(1, 0, 2)).reshape(128, 8 * n)


def build_stream(inp):
    slots = []
    for layer in range(2):
        w_in = inp["w_in_first"] if layer == 0 else inp["w_in_rest"][layer - 1]
        for r in layer_recipe(layer):
            s = np.zeros((128, 2048), np.float32)
            kind = r[0]
            if kind == "gu":
                _, l, f, half, jg = r
                w = (inp["ffn1_w_gu"] if f == 1 else inp["ffn2_w_gu"])[l]
                base = half * DFF + jg * 256
                s[:] = _kc(w[:, base:base + 256])
            elif kind == "dn":
                _, l, f, a, jq = r
                w = (inp["ffn1_w_down"] if f == 1 else inp["ffn2_w_down"])[l]
                v = s.reshape(128, 4, 512)
                for i in range(4):
                    j = 4 * jq + i
                    if j < NJ:
                        v[:, i, :] = w[j * 128:(j + 1) * 128, a * 512:(a + 1) * 512]
            elif kind == "in":
                _, l, chunks = r
                v = s.reshape(128, 8, 256)
                for jj, ch in enumerate(chunks):
                    cols = w_in[:, ch * 128:min((ch + 1) * 128, w_in.shape[1])]
                    n = cols.shape[1]
                    v[:, :, jj * 128:jj * 128 + n] = cols.reshape(8, 128, n).transpose(1, 0, 2)
            elif kind == "pj":
                _, l, which, mp = r
                w = {"a": inp["w_proj_a"], "b": inp["w_proj_b"], "o": inp["w_out"]}[which][l]
                s[:] = _kc(w[:, mp * 256:(mp + 1) * 256])
            elif kind == "sgu":
                _, l = r
                ws = inp["sgu_w_s"][l]
                s[:, 0:1024] = ws.transpose(2, 0, 1).reshape(128, 1024)
                s[0, 1024:2048] = inp["sgu_b_s"][l].reshape(1024)
            elif kind == "lora":
                _, l = r
                s[0:64, 0:1024] = inp["rwkv_w2"][l]
                s[64:128, 0:1024] = inp["rwkv_a2"][l]
                if l > 0:
                    s[0:32, 1024:2048] = inp["rwkv_v2"][l - 1]
            slots.append(s)
    return np.stack(slots, 0)


def _vec8(v):
    return np.ascontiguousarray(np.asarray(v, np.float32).reshape(8, 128).T)


def build_cvec(inp):
    out = np.zeros((128, 2, NV), np.float32)
    for l in range(2):
        mu = inp["mu_first"] if l == 0 else inp["mu_rest"][l - 1]
        o = out[:, l, :]

        def put(idx, v):
            o[:, idx * 8:(idx + 1) * 8] = _vec8(v)
        put(V_N1, inp["ffn1_norm"][l]); put(V_NM, inp["mix_norm"][l]); put(V_N2, inp["ffn2_norm"][l])
        put(V_LNG, inp["sgu_ln_g"][l]); put(V_LNB, inp["sgu_ln_b"][l])
        put(V_MUR, mu[0:1024]); put(V_MUK, mu[1024:2048]); put(V_MUV, mu[2048:3072])
        put(V_W0, inp["rwkv_w0"][l]); put(V_A0, inp["rwkv_a0"][l]); put(V_KK, inp["rwkv_k_k"][l])
        put(V_KA, inp["rwkv_k_a"][l]); put(V_RK, inp["rwkv_r_k"][l].reshape(1024))
        put(V_GNG, inp["rwkv_gn_g"][l]); put(V_GNB, inp["rwkv_gn_b"][l])
        if l > 0:
            put(V_V0, inp["rwkv_v0"][l - 1])
        put(V_FIN, inp["final_norm"])
        o[:, V_MUX * 8] = mu[3072:3200]
        if l > 0:
            o[0:32, V_MUX * 8 + 1] = mu[3200:3232]
    return out.reshape(128, 2 * NV)


def build_cmat():
    m = np.zeros((128, NCM), np.float32)
    m[:, CM_ID:CM_ID + 128] = np.eye(128)
    m[:, CM_ONESMEAN:CM_ONESMEAN + 128] = 1.0 / 1024.0
    bd = np.zeros((128, 128), np.float32)
    bd[0:64, 0:64] = 1.0
    bd[64:128, 64:128] = 1.0
    m[:, CM_ONESBD:CM_ONESBD + 128] = bd
    i = (np.arange(128) % 64)[:, None]
    j = np.arange(64)[None, :]
    m[:, CM_MLO:CM_MLO + 64] = (j < i)
    m[:, CM_MUPS:CM_MUPS + 64] = (j > i)
    m[:, CM_MUPI:CM_MUPI + 64] = (j >= i)
    sc = np.ones((512,), np.float32)
    sc[::64] = 0.0
    m[:, CM_SCAN:CM_SCAN + 512] = sc[None, :]
    s = np.arange(128)[:, None]
    t = np.arange(128)[None, :]
    m[:, CM_TRIU:CM_TRIU + 128] = (t >= s)
    m[:, CM_ONES1:CM_ONES1 + 128] = 1.0
    return m


def build_program(ntiles, nlayers=2, stop_after=None):
    ntok = ntiles * TT
    nc = bass.Bass("TRN2", target_bir_lowering=False)
    x_d = nc.dram_tensor("x", [ntok, D], F32, kind="ExternalInput").ap()
    rec = [layer_recipe(l) for l in range(2)]
    nslots = sum(len(r) for r in rec)
    ws_d = nc.dram_tensor("wstream", [nslots, 128, 2048], F32, kind="ExternalInput").ap()
    cv_d = nc.dram_tensor("cvec", [128, 2 * NV], F32, kind="ExternalInput").ap()
    cm_d = nc.dram_tensor("cmat", [128, NCM], F32, kind="ExternalInput").ap()
    y_d = nc.dram_tensor("y", [ntok, D], F32, kind="ExternalOutput").ap()
    dbg16 = nc.dram_tensor("dbg16", [8, 128, 4096], BF16, kind="ExternalOutput").ap() if DBG[0] else None
    dbg32 = nc.dram_tensor("dbg32", [8, 128, 512], F32, kind="ExternalOutput").ap() if DBG[0] else None
    P = Prog()
    layer_base = [0, len(rec[0])]

    with contextlib.ExitStack() as st:
        cnt = [0]

        def sb(shape, dt=F32):
            cnt[0] += 1
            return st.enter_context(nc.sbuf_tensor("sb%d" % cnt[0], shape, dt))

        banks = []
        for i in range(8):
            banks.append(st.enter_context(nc.psum_tensor("bank%d" % i, [128, 512], F32)))
        tbank = [Trk(excl=True) for _ in range(8)]

        def bk(i):
            return banks[i][:]

        def bk16(i):
            return banks[i][:].bitcast(BF16)

        x = sb([128, 8, 512]); tx = [Trk() for _ in range(8)]
        ring = [sb([128, 2048], BF16) for _ in range(R_RING)]
        tring = [Trk() for _ in range(R_RING)]
        cvec = sb([128, 2 * NV]); tcv = Trk()
        cmat = sb([128, NCM]); tcm = Trk()
        identb = sb([128, 128], BF16); onesmeanb = sb([128, 128], BF16); onesbdb = sb([128, 128], BF16)
        ones1b = sb([1, 128], BF16)
        Sst = sb([128, 2, 8, 64], BF16); tS = [Trk(), Trk()]
        carry = sb([128, 2, 2, 26]); tcar = [[[Trk() for _ in range(26)] for _ in range(2)] for _ in range(2)]
        cur_tile = [0]
        vfirst = sb([128, 8, 512], BF16); tvf = [Trk() for _ in range(8)]
        M_h = sb([128, 8, 512], BF16)
        M_sga = sb([128, 8, 512], BF16)
        M_sgb = sb([128, 8, 512], BF16)
        M_mer = sb([128, 8, 512], BF16)
        M_yb = sb([128, 8, 512], BF16)
        M_bon = sb([128, 8, 512], BF16)
        lora = sb([128, 2048], BF16)
        U = sb([128, 10240])
        Vr = sb([128, 13312])

        def cv(l, idx, c=0, rows=slice(0, 128)):
            o = l * NV + idx * 8 + c
            return cvec[rows, o:o + 1]

        def cm(off, n, rows=slice(0, 128)):
            return cmat[rows, off:off + n]

        P.op("sp", lambda e: e.dma_start(out=cvec[:], in_=cv_d), writes=[tcv], dma_key="c0")
        P.op("sp", lambda e: e.dma_start(out=cmat[:], in_=cm_d), writes=[tcm], dma_key="c1")
        for l in range(2):
            for (src, dst) in ((V_MUR, V_OMR), (V_MUK, V_OMK), (V_MUV, V_OMV), (V_MUX, V_OMX), (V_KA, V_OKA)):
                P.op("dve", lambda e, l=l, src=src, dst=dst: e.tensor_scalar(
                    out=cvec[:, l * NV + dst * 8:l * NV + dst * 8 + 8], in0=cvec[:, l * NV + src * 8:l * NV + src * 8 + 8],
                    scalar1=-1.0, scalar2=1.0, op0=ALU.mult, op1=ALU.add), reads=[], writes=[tcv])
        P.op("dve", lambda e: e.tensor_copy(out=identb[:], in_=cm(CM_ID, 128)), reads=[tcm], writes=[tcm])
        P.op("dve", lambda e: e.tensor_copy(out=onesmeanb[:], in_=cm(CM_ONESMEAN, 128)), writes=[tcm])
        P.op("dve", lambda e: e.tensor_copy(out=onesbdb[:], in_=cm(CM_ONESBD, 128)), writes=[tcm])
        P.op("dve", lambda e: e.tensor_copy(out=ones1b[:], in_=cmat[0:1, CM_ONES1:CM_ONES1 + 128]), writes=[tcm])
        P.barrier()

        spos = [0]

        def next_slot(layer, expect):
            li = spos[0]
            assert rec[layer][li][0] == expect[0], (rec[layer][li], expect)
            spos[0] += 1
            gi = layer_base[layer] + li
            nslot[0] += 1
            s = nslot[0] % R_RING
            P.op("pool", lambda e, s=s, gi=gi: e.dma_start(out=ring[s][:], in_=ws_d[gi]), writes=[tring[s]],
                 dma_key="w%d" % s)
            return ring[s], tring[s]

        nslot = [0]

        def mm(out, lhsT, rhs, start, stop, reads, writes):
            P.op("pe", lambda e: e.matmul(out, lhsT=lhsT, rhs=rhs, start=start, stop=stop), reads=reads, writes=writes)

        def tr(out, in_, ident, reads, writes):
            P.op("pe", lambda e: e.transpose(out, in_, ident), reads=reads, writes=writes)

        def act(out, in_, func, reads, writes, bias=None, scale=None):
            kw = {}
            if bias is not None:
                kw["bias"] = bias
            if scale is not None:
                kw["scale"] = scale
            P.op("act", lambda e: e.activation(out=out, in_=in_, func=func, **kw), reads=reads, writes=writes)

        def tt(out, in0, in1, op, reads, writes, eng="dve"):
            P.op(eng, lambda e: e.tensor_tensor(out=out, in0=in0, in1=in1, op=op), reads=reads, writes=writes)

        def stt(out, in0, scalar, in1, op0, op1, reads, writes):
            P.op("dve", lambda e: e.scalar_tensor_tensor(out=out, in0=in0, scalar=scalar, in1=in1, op0=op0, op1=op1),
                 reads=reads, writes=writes)

        def tsc(out, in0, s1, s2, op0, op1, reads, writes, eng="dve"):
            P.op(eng, lambda e: e.tensor_scalar(out=out, in0=in0, scalar1=s1, scalar2=s2, op0=op0, op1=op1),
                 reads=reads, writes=writes)

        def cp(out, in_, reads, writes, eng="dve"):
            if eng == "act":
                P.op(eng, lambda e: e.activation(out=out, in_=in_, func=AF.Copy), reads=reads, writes=writes)
            else:
                P.op(eng, lambda e: e.tensor_copy(out=out, in_=in_), reads=reads, writes=writes)

        def recip(out, in_, reads, writes):
            P.op("dve", lambda e: e.reciprocal(out=out, in_=in_), reads=reads, writes=writes)

        def Uf(off, n):
            return U[:, off:off + n]

        def U16(off, n):
            return U[:, off:off + n].bitcast(BF16)

        def Vf(off, n):
            return Vr[:, off:off + n]

        def V16(off, n):
            return Vr[:, off:off + n].bitcast(BF16)

        def rmsnorm(gidx, l, h, th, tmpbase=0):
            sq = [V16(tmpbase, 256), V16(tmpbase + 256, 256)]
            tsq = [Trk(), Trk()]
            rstd = Vf(tmpbase + 512, 512); trs = Trk()
            for c in range(8):
                act(sq[c % 2], x[:, c, :], AF.Square, [tx[c]], [tsq[c % 2]])
                mm(bk(4), onesmeanb[:], sq[c % 2], c == 0, c == 7, [tsq[c % 2]], [tbank[4]])
            act(rstd, bk(4), AF.Ln, [tbank[4]], [trs], bias=NORM_EPS)
            act(rstd, rstd, AF.Exp, [trs], [trs], scale=-0.5)
            for c in range(8):
                stt(h[:, c, :], x[:, c, :], cv(l, gidx, c), rstd, ALU.mult, ALU.mult, [tx[c], trs, tcv], [th[c]])

        def ffn(l, f):
            P.barrier()
            h = U16(0, 2048).rearrange("p (c t) -> p c t", c=8)
            g = U16(2048, 5632).rearrange("p (j t) -> p j t", j=NJ)
            th = [Trk() for _ in range(8)]; tg = [Trk() for _ in range(NJ)]
            sgt = [Uf(7680, 512), Uf(8192, 512)]; tsg = [Trk(), Trk()]
            rmsnorm(V_N1 if f == 1 else V_N2, l, h, th)
            for jg in range(11):
                sg_, tsg_ = next_slot(l, ("gu",))
                su_, tsu_ = next_slot(l, ("gu",))
                sgv = sg_[:].rearrange("p (k n) -> p k n", k=8)
                suv = su_[:].rearrange("p (k n) -> p k n", k=8)
                for jj in range(2):
                    j = 2 * jg + jj
                    bg = j % 2; bu = 2 + j % 2
                    for k in range(8):
                        mm(bk(bg), sgv[:, k, jj * 128:(jj + 1) * 128], h[:, k, :], k == 0, k == 7, [tsg_, th[k]], [tbank[bg]])
                    for k in range(8):
                        mm(bk(bu), suv[:, k, jj * 128:(jj + 1) * 128], h[:, k, :], k == 0, k == 7, [tsu_, th[k]], [tbank[bu]])
                    act(sgt[j % 2], bk(bg), AF.Silu, [tbank[bg]], [tsg[j % 2]])
                    tt(g[:, j, :], sgt[j % 2], bk(bu), ALU.mult, [tsg[j % 2], tbank[bu]], [tg[j]])
            for a in range(2):
                for jq in range(6):
                    sd_, tsd_ = next_slot(l, ("dn",))
                    sdv = sd_[:].rearrange("p (i n) -> p i n", i=4)
                    for i in range(4):
                        j = 4 * jq + i
                        if j >= NJ:
                            continue
                        for m in range(4):
                            mm(bk(4 + m), sdv[:, i, m * 128:(m + 1) * 128], g[:, j, :], j == 0, j == NJ - 1,
                               [tsd_, tg[j]], [tbank[4 + m]])
                for m in range(4):
                    c = a * 4 + m
                    stt(x[:, c, :], bk(4 + m), 0.5, x[:, c, :], ALU.mult, ALU.add, [tbank[4 + m]], [tx[c]])

        def inproj(slotv, tslot, jj, h, th, b, mcols=128):
            for k in range(8):
                mm(bk(b)[0:mcols, :], slotv[:, k, jj * 128:jj * 128 + mcols], h[:, k, :], k == 0, k == 7,
                   [tslot, th[k]], [tbank[b]])

        class InStream:
            def __init__(self, l, order, h, th):
                self.l = l; self.order = order; self.h = h; self.th = th; self.i = 0
                self.slot = None

            def next(self, mcols=128):
                if self.i % 2 == 0:
                    s_, t_ = next_slot(self.l, ("in",))
                    self.slot = (s_[:].rearrange("p (k n) -> p k n", k=8), t_)
                b = self.i % 2
                inproj(self.slot[0], self.slot[1], self.i % 2, self.h, self.th, b, mcols)
                self.i += 1
                return b

        def token_shift(b, l, muidx, omidx, c, cidx, out, tout, rows=slice(0, 128)):
            pw = cur_tile[0] % 2; pr = 1 - pw
            act(out, bk(b)[rows, :], AF.Copy, [tbank[b], tcv], [tout], scale=cv(l, omidx, c, rows))
            cp(carry[rows, pw, l, cidx:cidx + 1], bk(b)[rows, 511:512], [tbank[b]], [tcar[pw][l][cidx]], eng="act")
            stt(out[:, 1:512], bk(b)[rows, 0:511], cv(l, muidx, c, rows), out[:, 1:512], ALU.mult, ALU.add,
                [tbank[b], tcv], [tout])
            stt(out[:, 0:1], carry[rows, pr, l, cidx:cidx + 1], cv(l, muidx, c, rows), out[:, 0:1], ALU.mult, ALU.add,
                [tcar[pr][l][cidx], tcv], [tout])

        def mixer(l):
            P.barrier()
            h = M_h; th = [Trk() for _ in range(8)]
            rmsnorm(V_NM, l, h, th)
            sga = M_sga; tsga = [Trk() for _ in range(8)]
            sgb = M_sgb; tsgb = [Trk() for _ in range(8)]
            mer = M_mer; tmer = [Trk() for _ in range(8)]
            ua = U16(0, 2048).rearrange("p (c t) -> p c t", c=8); tua = [Trk() for _ in range(8)]
            gv = Uf(2048, 4096).rearrange("p (c t) -> p c t", c=8); tgv = [Trk() for _ in range(8)]
            vn = U16(6144, 2048).rearrange("p (c t) -> p c t", c=8); tvn = [Trk() for _ in range(8)]
            vnT = U16(8192, 2048).rearrange("p (c t) -> p c t", c=8); tvnT = [Trk() for _ in range(8)]
            ins = InStream(l, in_chunk_order_a(), h, th)
            sq16 = [V16(1024, 256), V16(1280, 256)]; tsq16 = [Trk(), Trk()]
            gvb = [V16(1536, 256), V16(1792, 256)]; tgvb = [Trk(), Trk()]
            for c in range(8):
                b = ins.next()
                act(gv[:, c, :], bk(b), AF.Gelu_apprx_tanh, [tbank[b]], [tgv[c]])
                act(sq16[c % 2], gv[:, c, :], AF.Square, [tgv[c]], [tsq16[c % 2]])
                cp(gvb[c % 2], gv[:, c, :], [tgv[c]], [tgvb[c % 2]])
                mm(bk(4), onesmeanb[:], gvb[c % 2], c == 0, c == 7, [tgvb[c % 2]], [tbank[4]])
                mm(bk(5), onesmeanb[:], sq16[c % 2], c == 0, c == 7, [tsq16[c % 2]], [tbank[5]])
            mean = Vf(2048, 512); tmean = Trk()
            var = Vf(2560, 512); tvar = Trk()
            act(mean, bk(4), AF.Copy, [tbank[4]], [tmean])
            tt(var, mean, mean, ALU.mult, [tmean], [tvar])
            tt(var, bk(5), var, ALU.subtract, [tbank[5], tvar], [tvar])
            act(var, var, AF.Ln, [tvar], [tvar], bias=LN_EPS)
            act(var, var, AF.Exp, [tvar], [tvar], scale=-0.5)
            tmpl = [Vf(3072, 512), Vf(3584, 512)]; ttmpl = [Trk(), Trk()]
            for c in range(8):
                t_ = tmpl[c % 2]; tt_ = ttmpl[c % 2]
                tt(t_, gv[:, c, :], mean, ALU.subtract, [tgv[c], tmean], [tt_])
                tt(t_, t_, var, ALU.mult, [tt_, tvar], [tt_])
                act(vn[:, c, :], t_, AF.Identity, [tt_, tcv], [tvn[c]], bias=cv(l, V_LNB, c), scale=cv(l, V_LNG, c))
                b = ins.next()
                act(sga[:, c, :], bk(b), AF.Sigmoid, [tbank[b]], [tsga[c]])
            for c in range(8):
                b = ins.next()
                act(ua[:, c, :], bk(b), AF.Gelu_apprx_tanh, [tbank[b]], [tua[c]])
            ssg, tssg = next_slot(l, ("sgu",))
            wsT = V16(4096, 512).rearrange("p (g t) -> p g t", g=8); twsT = Trk()
            tt(wsT, ssg[:, 0:1024].rearrange("p (g t) -> p g t", g=8),
               cm(CM_TRIU, 128).unsqueeze(1).broadcast_to([128, 8, 128]), ALU.mult, [tssg, tcm], [twsT])
            for c in range(8):
                b = 6 + (c // 2) % 2
                off = (c % 2) * 512
                for cc in range(4):
                    tr(bk16(b)[:, off + cc * 128:off + (cc + 1) * 128], vn[:, c, cc * 128:(cc + 1) * 128], identb[:],
                       [tvn[c], tcm], [tbank[b]])
                cp(vnT[:, c, :], bk16(b)[:, off:off + 512], [tbank[b]], [tvnT[c]], eng="act" if c % 2 else "dve")
            for c in range(8):
                b = 2 + c % 2
                for cc in range(4):
                    mm(bk(b)[:, cc * 128:(cc + 1) * 128], vnT[:, c, cc * 128:(cc + 1) * 128], wsT[:, c, :], True, False,
                       [tvnT[c], twsT], [tbank[b]])
                    mm(bk(b)[:, cc * 128:(cc + 1) * 128], ones1b[0:1, :], ssg[0:1, 1024 + c * 128:1024 + (c + 1) * 128],
                       False, True, [tssg, tcm], [tbank[b]])
                tt(ua[:, c, :], bk(b), ua[:, c, :], ALU.mult, [tbank[b]], [tua[c]])
            for mp in range(4):
                sp_, tsp_ = next_slot(l, ("pj",))
                spv = sp_[:].rearrange("p (k n) -> p k n", k=8)
                for mm_ in range(2):
                    m = 2 * mp + mm_
                    b = m % 2
                    for c in range(8):
                        mm(bk(b), spv[:, c, mm_ * 128:(mm_ + 1) * 128], ua[:, c, :], c == 0, c == 7, [tsp_, tua[c]], [tbank[b]])
                    tt(mer[:, m, :], bk(b), sga[:, m, :], ALU.mult, [tbank[b], tsga[m]], [tmer[m]])
            if DBG[0] == 'sgu':
                spos[0] += 1 + (len(in_chunk_order_b(l)) + 1) // 2 + 4
            if DBG[0] != 'sgu':
                P.barrier()
                til = [U16(i * 2048, 2048).rearrange("p (c t) -> p c t", c=8) for i in range(5)]
                ttil = [[Trk() for _ in range(8)] for _ in range(5)]
                Rt, Kt, Bt, At, Vt = til
                bon = M_bon; tbon = [Trk() for _ in range(8)]
                TA = [Vf(i * 512, 512) for i in range(11)]; tTA = [Trk() for _ in range(11)]
                TB = [Vf(7680 + i * 512, 512) for i in range(11)]; tTB = [Trk() for _ in range(11)]
                T = TA; tT = tTA
                sqb = V16(5632, 256); tsqb = Trk()
                rkb = V16(5888, 256); trkb = Trk()
                twad = V16(6144, 256); ttw = Trk()
                vres = V16(6400, 256); tvres = Trk()
                PC = Vf(6656, 64).rearrange("p (c q) -> p c q", c=8); tPC = Trk()
                slo_r, tslo_r = next_slot(l, ("lora",))
                slo = lora; tslo = Trk()
                cp(lora[:], slo_r[:], [tslo_r], [tslo])
                ins = InStream(l, in_chunk_order_b(l), h, th)
                b = ins.next()
                token_shift(b, l, V_MUX, V_OMX, 0, 24, T[0], tT[0])
                act(twad[0:64, :], T[0][0:64, :], AF.Tanh, [tT[0]], [ttw])
                act(twad[64:128, :], T[0][64:128, :], AF.Copy, [tT[0]], [ttw])
                if l > 0:
                    b = ins.next(mcols=32)
                    token_shift(b, l, V_MUX, V_OMX, 1, 25, T[1][0:32, :], tT[1], rows=slice(0, 32))
                    cp(vres[0:32, :], T[1][0:32, :], [tT[1]], [tvres])
                for c in range(8):
                    b = ins.next()
                    act(sgb[:, c, :], bk(b), AF.Sigmoid, [tbank[b]], [tsgb[c]])
                def stageA0(c):
                        T, tT = (TA, tTA) if c % 2 == 0 else (TB, tTB)
                        rb, kb, vb = T[0], T[1], T[2]
                        b = ins.next(); token_shift(b, l, V_MUR, V_OMR, c, c, rb, tT[0])
                        b = ins.next(); token_shift(b, l, V_MUK, V_OMK, c, 8 + c, kb, tT[1])
                        b = ins.next(); token_shift(b, l, V_MUV, V_OMV, c, 16 + c, vb, tT[2])
                def stageA1(c):
                        T, tT = (TA, tTA) if c % 2 == 0 else (TB, tTB)
                        rb, kb, vb = T[0], T[1], T[2]
                        mm(bk(2), slo[0:64, c * 128:(c + 1) * 128], twad[0:64, :], True, True, [tslo, ttw], [tbank[2]])
                        mm(bk(3), slo[64:128, c * 128:(c + 1) * 128], twad[64:128, :], True, True, [tslo, ttw], [tbank[3]])
                        act(T[3], bk(2), AF.Sigmoid, [tbank[2], tcv], [tT[3]], bias=cv(l, V_W0, c))
                        act(T[4], bk(3), AF.Sigmoid, [tbank[3], tcv], [tT[4]], bias=cv(l, V_A0, c))
                        P.op("dve", lambda e, o_=T[5], i_=T[3]: e.tensor_tensor_scan(out=o_, data0=cm(CM_SCAN, 512), data1=i_, initial=0.0,
                                                                   op0=ALU.mult, op1=ALU.add), reads=[tT[3], tcm], writes=[tT[5]])
                        tt(T[6], T[5], T[3], ALU.subtract, [tT[5], tT[3]], [tT[6]])
                        act(T[7], T[5], AF.Exp, [tT[5]], [tT[7]], scale=-C0)
                        act(T[8], T[5], AF.Exp, [tT[5]], [tT[8]], scale=C0)
                        act(T[6], T[6], AF.Exp, [tT[6]], [tT[6]], scale=-C0)
                        cp(PC[:, c, :], T[7].rearrange("p (q t) -> p q t", q=8)[:, :, 63], [tT[7]], [tPC])
                        act(T[9], kb, AF.Copy, [tT[1], tcv], [tT[9]], scale=cv(l, V_KK, c))
                        act(sqb, T[9], AF.Square, [tT[9]], [tsqb])
                        mm(bk(4), onesbdb[:], sqb, True, True, [tsqb, tcm], [tbank[4]])
                        tsc(T[10], bk(4), 1e-18, None, ALU.max, ALU.bypass, [tbank[4]], [tT[10]])
                        act(T[10], T[10], AF.Ln, [tT[10]], [tT[10]])
                        act(T[10], T[10], AF.Exp, [tT[10]], [tT[10]], scale=-0.5)
                        tt(T[9], T[9], T[10], ALU.mult, [tT[9], tT[10]], [tT[9]])
                        if l > 0:
                            mm(bk(5)[:, :], slo[0:32, 1024 + c * 128:1024 + (c + 1) * 128], vres[0:32, :], True, True,
                               [tslo, tvres], [tbank[5]])
                            act(T[5], bk(5), AF.Sigmoid, [tbank[5], tcv], [tT[5]], bias=cv(l, V_V0, c))
                def stageB(c):
                        T, tT = (TA, tTA) if c % 2 == 0 else (TB, tTB)
                        rb, kb, vb = T[0], T[1], T[2]
                        tsc(T[3], T[4], cv(l, V_KA, c), cv(l, V_OKA, c), ALU.mult, ALU.add, [tT[4], tcv], [tT[3]])
                        tt(T[3], kb, T[3], ALU.mult, [tT[1], tT[3]], [tT[3]])
                        tt(T[10], T[9], T[4], ALU.mult, [tT[9], tT[4]], [tT[10]])
                        if l > 0:
                            tt(T[4], vfirst[:, c, :], vb, ALU.subtract, [tvf[c], tT[2]], [tT[4]])
                            tt(T[4], T[4], T[5], ALU.mult, [tT[4], tT[5]], [tT[4]])
                            tt(vb, vb, T[4], ALU.add, [tT[2], tT[4]], [tT[2]])
                        else:
                            cp(vfirst[:, c, :], vb, [tT[2]], [tvf[c]], eng="act")
                        stt(M_yb[:, c, :], rb, cv(l, V_RK, c), T[3], ALU.mult, ALU.mult, [tT[0], tT[3], tcv], [trk8[c]])
                        tt(Rt[:, c, :], rb, T[7], ALU.mult, [tT[0], tT[7]], [ttil[0][c]])
                        tt(Kt[:, c, :], T[3], T[8], ALU.mult, [tT[3], tT[8]], [ttil[1][c]])
                        tt(Bt[:, c, :], T[10], T[8], ALU.mult, [tT[10], tT[8]], [ttil[2][c]])
                        stt(At[:, c, :], T[9], -1.0, T[6], ALU.mult, ALU.mult, [tT[9], tT[6]], [ttil[3][c]])
                        cp(Vt[:, c, :], vb, [tT[2]], [ttil[4][c]], eng="act")
                trk8 = [Trk() for _ in range(8)]
                stageA0(0)
                for c in range(8):
                    if c >= 1:
                        stageB(c - 1)
                    if c + 1 <= 7:
                        stageA0(c + 1)
                    stageA1(c)
                stageB(7)
                for c in range(8):
                    b = 6 if c % 2 == 0 else 2
                    mm(bk(b), onesbdb[:], M_yb[:, c, :], True, True, [trk8[c], tcm], [tbank[b]])
                    tt(bon[:, c, :], bk(b), Vt[:, c, :], ALU.mult, [tbank[b], ttil[4][c]], [tbon[c]])
            if DBG[0] == 'prep':
                spos[0] += 4
                for i in range(5):
                    tok = P.op("sp", lambda e, i=i: e.dma_start(out=dbg16[i], in_=U[:, i * 2048:(i + 1) * 2048].bitcast(BF16)),
                               reads=ttil[i], dma_key="dbg")
                    P.q["sp"].append(([tok], None, None, False))
                for i in range(8):
                    tok = P.op("sp", lambda e, i=i: e.dma_start(out=dbg32[i], in_=T[3 + i]), reads=[tT[3 + i]], dma_key="dbg")
                    P.q["sp"].append(([tok], None, None, False))
            if DBG[0] not in ('sgu', 'prep'):
                P.barrier()
                def bd(off):
                    return V16(off, 512).rearrange("p (c n) -> p c n", c=8)
                def st64(off):
                    return V16(off, 256).rearrange("p (c n) -> p c n", c=8)
                Abd = [bd(0), bd(512)]; Bbd = [bd(1024), bd(1536)]; Ttb = [bd(2048), bd(2560)]
                tAbd = [[Trk(), Trk()], [Trk(), Trk()]]; tBbd = [[Trk(), Trk()], [Trk(), Trk()]]
                tTt = [[Trk(), Trk()], [Trk(), Trk()]]
                AkT = st64(3072); ArbT = st64(3328); ArkT = st64(3584)
                Kst = st64(3840); Bst = st64(4096); Vst = st64(4352); Xs = st64(4608); Us = st64(4864); ynb = st64(5120)
                tAk, tArb, tArk, tKst, tBst, tVst, tXs, tUs, tynb = [Trk() for _ in range(9)]
                ysb = Vf(6144, 512); tysb = Trk()
                stat = Vf(6720, 64); tstat = Trk()
                ysq = Vf(6784, 512); tysq = Trk()
                P.op("dve", lambda e: e.memset(Abd[0], 0.0), writes=tAbd[0])
                P.op("dve", lambda e: e.memset(Bbd[0], 0.0), writes=tBbd[0])
                S = Sst[:, l, :, :]
                R0 = slice(0, 64); R1 = slice(64, 128)
                RW = (R0, R1)
                idb = identb[:].unsqueeze(1).broadcast_to([128, 8, 128])
                v3 = lambda ap, n=8: ap.rearrange("p (c n) -> p c n", c=n)
                for q in range(8):
                    cs = slice(q * 64, (q + 1) * 64)
                    prods = [(3, 2, 0), (2, 3, 1), (1, 3, 2), (2, 0, 3), (1, 0, 4)]
                    for hh in range(2):
                        rows = RW[hh]
                        for (li, ri, b) in prods:
                            for c in range(8):
                                mm(bk(b)[rows, c * 64:(c + 1) * 64], til[li][rows, c, cs], til[ri][rows, c, cs], True, True,
                                   [ttil[li][c], ttil[ri][c]], [tbank[b]])
                    for hh in range(2):
                        rows = RW[hh]
                        tt(Abd[0][rows, :, hh * 64:(hh + 1) * 64], v3(bk(0)[rows, :]),
                           cm(CM_MLO, 64, rows).unsqueeze(1).broadcast_to([64, 8, 64]), ALU.mult, [tbank[0], tcm], tAbd[0])
                        tt(Bbd[0][rows, :, hh * 64:(hh + 1) * 64], v3(bk(1)[rows, :]),
                           cm(CM_MUPS, 64, rows).unsqueeze(1).broadcast_to([64, 8, 64]), ALU.mult, [tbank[1], tcm], tBbd[0])
                    for (dst, tdst, moff, b) in ((AkT, tAk, CM_MUPS, 2), (ArbT, tArb, CM_MUPI, 3), (ArkT, tArk, CM_MUPI, 4)):
                        tt(dst, v3(bk(b)), cm(moff, 64).unsqueeze(1).broadcast_to([128, 8, 64]), ALU.mult, [tbank[b], tcm], [tdst])
                    for hh in range(2):
                        rows = RW[hh]
                        for (src, tsrc, b, off) in ((Kt, ttil[1], 5, 0), (Bt, ttil[2], 5, 512), (Vt, ttil[4], 6, 0)):
                            for c in range(8):
                                tr(bk16(b)[rows, off + c * 64:off + (c + 1) * 64], src[rows, c, cs], identb[rows, rows],
                                   [tsrc[c], tcm], [tbank[b]])
                    cp(Kst, v3(bk16(5)[:, 0:512]), [tbank[5]], [tKst], eng="act")
                    cp(Bst, v3(bk16(5)[:, 512:1024]), [tbank[5]], [tBst], eng="act")
                    cp(Vst, v3(bk16(6)[:, 0:512]), [tbank[6]], [tVst], eng="act")
                    def x_terms(hh):
                        rows = RW[hh]
                        for c in range(8):
                            osl = slice(c * 64, (c + 1) * 64)
                            mm(bk(5)[rows, osl], AkT[rows, c, :], Vst[rows, c, :], c == 0, False, [tAk, tVst], [tbank[5]])
                            mm(bk(5)[rows, osl], At[rows, c, cs], S[rows, c, :], False, True, [ttil[3][c], tS[l]], [tbank[5]])
                    def y1_terms(hh):
                        rows = RW[hh]
                        for c in range(8):
                            osl = slice(c * 64, (c + 1) * 64)
                            mm(bk(6)[rows, osl], ArkT[rows, c, :], Vst[rows, c, :], c == 0, False, [tArk, tVst], [tbank[6]])
                            mm(bk(6)[rows, osl], Rt[rows, c, cs], S[rows, c, :], False, False, [ttil[0][c], tS[l]], [tbank[6]])
                    def s1_terms(hh):
                        rows = RW[hh]
                        for c in range(8):
                            osl = slice(c * 64, (c + 1) * 64)
                            mm(bk(1)[rows, osl], Kst[rows, c, :], Vst[rows, c, :], c == 0, False, [tKst, tVst], [tbank[1]])
                            mm(bk(1)[rows, osl], identb[rows, rows], S[rows, c, :], False, False, [tcm, tS[l]], [tbank[1]])
                    def ys2_terms(hh):
                        rows = RW[hh]
                        for c in range(8):
                            osl = slice(c * 64, (c + 1) * 64)
                            mm(bk(6)[rows, osl], ArbT[rows, c, :], Us[rows, c, :], False, True, [tArb, tUs], [tbank[6]])
                        for c in range(8):
                            osl = slice(c * 64, (c + 1) * 64)
                            mm(bk(1)[rows, osl], Bst[rows, c, :], Us[rows, c, :], False, True, [tBst, tUs], [tbank[1]])
                    cur = 0
                    bT = (0, 3); bA = (1, 4); bB = (2, 7)
                    for lev in range(6):
                        nxt = 1 - cur
                        last = lev == 5
                        for half in range(2):
                            for ci in range(4):
                                c = half * 4 + ci
                                osl = slice(ci * 128, (ci + 1) * 128)
                                if lev > 0:
                                    mm(bk(bT[half])[:, osl], Abd[cur][:, c, :], Ttb[cur][:, c, :], True, True,
                                       [tAbd[cur][half], tTt[cur][half]], [tbank[bT[half]]])
                                if not last:
                                    mm(bk(bA[half])[:, osl], Bbd[cur][:, c, :], Abd[cur][:, c, :], True, True,
                                       [tAbd[cur][half], tBbd[cur][half]], [tbank[bA[half]]])
                                    mm(bk(bB[half])[:, osl], Abd[cur][:, c, :], Bbd[cur][:, c, :], True, True,
                                       [tAbd[cur][half], tBbd[cur][half]], [tbank[bB[half]]])
                        fill = {0: (x_terms, 0), 1: (y1_terms, 0), 2: (x_terms, 1), 3: (y1_terms, 1), 5: (s1_terms, 0)}.get(lev)
                        if fill is not None:
                            fill[0](fill[1])
                        if lev == 0:
                            tt(Ttb[nxt], Bbd[0], idb, ALU.add, tBbd[0] + [tcm], tTt[nxt])
                        for half in range(2):
                            hs = slice(half * 4, half * 4 + 4)
                            if lev > 0:
                                tt(Ttb[nxt][:, hs, :], Ttb[cur][:, hs, :], v3(bk(bT[half]), 4), ALU.add,
                                   [tTt[cur][half], tbank[bT[half]]], [tTt[nxt][half]])
                            if not last:
                                cp(Abd[nxt][:, hs, :], v3(bk(bA[half]), 4), [tbank[bA[half]]], [tAbd[nxt][half]], eng="act")
                                cp(Bbd[nxt][:, hs, :], v3(bk(bB[half]), 4), [tbank[bB[half]]], [tBbd[nxt][half]],
                                   eng="dve" if half == 0 else "act")
                        cur = nxt
                    Tfin = Ttb[cur]; tTfin = tTt[cur]
                    cp(Xs, v3(bk(5)), [tbank[5]], [tXs])
                    for c in range(8):
                        osl = slice(c * 64, (c + 1) * 64)
                        mm(bk(7)[:, osl], Tfin[:, c, :], Xs[:, c, :], True, True, [tTfin[c // 4], tXs], [tbank[7]])
                    s1_terms(1)
                    cp(Us, v3(bk(7)), [tbank[7]], [tUs], eng="act")
                    ys2_terms(0); ys2_terms(1)
                    tt(S, v3(bk(1)), PC[:, :, q:q + 1].broadcast_to([128, 8, 64]), ALU.mult, [tbank[1], tPC], [tS[l]])
                    act(ysb, bk(6), AF.Copy, [tbank[6]], [tysb])
                    act(ysq, bk(6), AF.Square, [tbank[6]], [tysq])
                    s1 = stat[:, 0:8]; s2 = stat[:, 8:16]; mean = stat[:, 16:24]; msq = stat[:, 24:32]; var = stat[:, 32:40]
                    P.op("dve", lambda e, s1=s1: e.tensor_reduce(out=s1, in_=v3(ysb), axis=AX.X, op=ALU.add),
                         reads=[tysb], writes=[tstat])
                    P.op("dve", lambda e, s2=s2: e.tensor_reduce(out=s2, in_=v3(ysq), axis=AX.X, op=ALU.add),
                         reads=[tysq, tstat], writes=[tstat])
                    tsc(mean, s1, 1.0 / 64, None, ALU.mult, ALU.bypass, [tstat], [tstat])
                    tt(msq, mean, mean, ALU.mult, [tstat], [tstat])
                    stt(var, s2, 1.0 / 64, msq, ALU.mult, ALU.subtract, [tstat], [tstat])
                    act(var, var, AF.Sqrt, [tstat], [tstat], bias=GN_EPS)
                    recip(var, var, [tstat], [tstat])
                    y3 = v3(ysb)
                    tt(y3, y3, mean.unsqueeze(2).broadcast_to([128, 8, 64]), ALU.subtract, [tysb, tstat], [tysb])
                    tt(ynb, y3, var.unsqueeze(2).broadcast_to([128, 8, 64]), ALU.mult, [tysb, tstat], [tynb])
                    for hh in range(2):
                        rows = RW[hh]
                        b = 7 if hh == 0 else 5
                        for c in range(8):
                            tr(bk16(b)[rows, 512 + c * 64:512 + (c + 1) * 64], ynb[rows, c, :], identb[rows, rows], [tynb, tcm], [tbank[b]])
                        cp(M_yb[rows, :, cs], v3(bk16(b)[rows, 512:1024]), [tbank[b]], [tyb_all], eng="act")
                tmpa = [ysb, ysq]
                ttmpa = [tysb, tysq]
                for c in range(8):
                    t_ = tmpa[c % 2]; tt__ = ttmpa[c % 2]
                    act(t_, M_yb[:, c, :], AF.Identity, [tyb_all, tcv], [tt__], bias=cv(l, V_GNB, c), scale=cv(l, V_GNG, c))
                    tt(M_yb[:, c, :], t_, bon[:, c, :], ALU.add, [tt__, tbon[c]], [tybc[c]])
                for mp in range(4):
                    sp_, tsp_ = next_slot(l, ("pj",))
                    spv = sp_[:].rearrange("p (k n) -> p k n", k=8)
                    for mm_ in range(2):
                        m = 2 * mp + mm_
                        b = m % 2
                        for c in range(8):
                            mm(bk(b), spv[:, c, mm_ * 128:(mm_ + 1) * 128], M_yb[:, c, :], c == 0, c == 7, [tsp_, tybc[c]], [tbank[b]])
                        t_ = tmpa[m % 2]; tt__ = ttmpa[m % 2]
                        tt(t_, bk(b), sgb[:, m, :], ALU.mult, [tbank[b], tsgb[m]], [tt__])
                        tt(mer[:, m, :], t_, mer[:, m, :], ALU.add, [tt__], [tmer[m]])
            for mp in range(4):
                sp_, tsp_ = next_slot(l, ("pj",))
                spv = sp_[:].rearrange("p (k n) -> p k n", k=8)
                for mm_ in range(2):
                    m = 2 * mp + mm_
                    b = 2 + m % 2
                    for c in range(8):
                        mm(bk(b), spv[:, c, mm_ * 128:(mm_ + 1) * 128], mer[:, c, :], c == 0, c == 7, [tsp_, tmer[c]], [tbank[b]])
                    tt(x[:, m, :], bk(b), x[:, m, :], ALU.add, [tbank[b]], [tx[m]])

        tyb_all = Trk()
        tQ1g = Trk(); tP1g = Trk()
        tybc = [Trk() for _ in range(8)]

        tiles_per_seq = SEQ // TT
        xt = Uf(0, 4096).rearrange("p (s d) -> p s d", s=4)
        yf = Uf(4096, 4096).rearrange("p (c t) -> p c t", c=8)
        for tile in range(ntiles):
            cur_tile[0] = tile
            P.barrier()
            txt = Trk()
            if tile % tiles_per_seq == 0:
                P.op("dve", lambda e: e.memset(Sst[:].rearrange("p a c n -> p (a c n)"), 0.0), writes=[tS[0], tS[1]])
                P.op("dve", lambda e: e.memset(carry[:].rearrange("p w a c -> p (w a c)"), 0.0),
                     writes=[t for w_ in tcar for l_ in w_ for t in l_])
            P.op("sp", lambda e, tile=tile: e.dma_start(
                out=xt, in_=x_d[tile * TT:(tile + 1) * TT, :].rearrange("(s p) d -> p s d", p=128)),
                writes=[txt], dma_key="xin")
            for c in range(8):
                b = c % 2
                for s in range(4):
                    tr(bk(b)[:, s * 128:(s + 1) * 128], xt[:, s, c * 128:(c + 1) * 128], cm(CM_ID, 128), [txt, tcm], [tbank[b]])
                cp(x[:, c, :], bk(b), [tbank[b]], [tx[c]], eng="act" if c % 2 else "dve")
            for l in range(nlayers):
                spos[0] = 0
                ffn(l, 1)
                if stop_after == ("ffn1", l):
                    break
                mixer(l)
                if stop_after == ("mixer", l):
                    break
                ffn(l, 2)
            P.barrier()
            sqf = [V16(0, 256), V16(256, 256)]; tsqf = [Trk(), Trk()]
            rstd = Vf(512, 512); trs = Trk()
            tyf = [Trk() for _ in range(8)]
            if stop_after is None:
                for c in range(8):
                    act(sqf[c % 2], x[:, c, :], AF.Square, [tx[c]], [tsqf[c % 2]])
                    mm(bk(4), onesmeanb[:], sqf[c % 2], c == 0, c == 7, [tsqf[c % 2]], [tbank[4]])
                act(rstd, bk(4), AF.Ln, [tbank[4]], [trs], bias=NORM_EPS)
                act(rstd, rstd, AF.Exp, [trs], [trs], scale=-0.5)
                for c in range(8):
                    stt(yf[:, c, :], x[:, c, :], cv(0, V_FIN, c), rstd, ALU.mult, ALU.mult, [tx[c], trs, tcv], [tyf[c]])
            else:
                for c in range(8):
                    cp(yf[:, c, :], x[:, c, :], [tx[c]], [tyf[c]])
            txo = Trk()
            for s in range(4):
                for hf in range(2):
                    b = 2 + hf
                    for ci in range(4):
                        c = hf * 4 + ci
                        tr(bk(b)[:, ci * 128:(ci + 1) * 128], yf[:, c, s * 128:(s + 1) * 128], cm(CM_ID, 128), [tyf[c], tcm], [tbank[b]])
                    cp(xt[:, s, hf * 512:(hf + 1) * 512], bk(b), [tbank[b]], [txo], eng="act" if hf else "dve")
            tok = P.op("sp", lambda e, tile=tile: e.dma_start(
                out=y_d[tile * TT:(tile + 1) * TT, :].rearrange("(s p) d -> p s d", p=128), in_=xt),
                reads=[txo], dma_key="xout")
            P.q["sp"].append(([tok], None, None, False))
        P.run(nc)
    return nc


_CACHE = {}


def kernel(**inputs):
    inp = {k: np.asarray(v) for k, v in inputs.items()}
    x = inp["x"].astype(np.float32)
    B = x.shape[0]
    per = B // NCORES
    ntiles = per * SEQ // TT
    wstream = build_stream(inp)
    cvec = build_cvec(inp)
    cmat = build_cmat()
    if "nc" not in _CACHE:
        _CACHE["nc"] = build_program(ntiles)
    nc = _CACHE["nc"]
    in_maps = []
    for i in range(NCORES):
        xs = np.ascontiguousarray(x[i * per:(i + 1) * per].reshape(per * SEQ, D))
        in_maps.append({"x": xs, "wstream": wstream, "cvec": cvec, "cmat": cmat})
    res = run_bass_kernel_spmd(nc, in_maps, core_ids=list(range(NCORES)))
    out = np.concatenate([r["y"].reshape(per, SEQ, D) for r in res.results], axis=0)
    return out.astype(np.float32)
```

```python
import contextlib
import math
import numpy as np
import concourse.bass as bass
import concourse.mybir as mybir
from concourse.bass_utils import run_bass_kernel_spmd

F32 = mybir.dt.float32
BF16 = mybir.dt.bfloat16
ALU = mybir.AluOpType
AF = mybir.ActivationFunctionType
AX = mybir.AxisListType

D = 1024
DFF = 2816
NJ = 22
TT = 512
SEQ = 2048
NCORES = 8
R_RING = 6
NORM_EPS = 1e-6
LN_EPS = 1e-5
GN_EPS = 64e-5
C0 = math.exp(-0.5)
DBG = [None]
STATS = {}

(V_N1, V_NM, V_N2, V_LNG, V_LNB, V_MUR, V_MUK, V_MUV, V_W0, V_A0, V_KK, V_KA, V_RK, V_GNG, V_GNB, V_V0,
 V_FIN, V_MUX, V_OMR, V_OMK, V_OMV, V_OMX, V_OKA) = range(23)
NV = 23 * 8

CM_ID = 0
CM_ONESMEAN = 128
CM_ONESBD = 256
CM_MLO = 384
CM_MUPS = 448
CM_MUPI = 512
CM_SCAN = 576
CM_TRIU = 1088
CM_ONES1 = 1216
NCM = 1344


class Trk:
    __slots__ = ("w", "r", "excl")

    def __init__(self, excl=False):
        self.w = None
        self.r = {}
        self.excl = excl


class Prog:
    ENG = ["pe", "act", "dve", "pool", "sp"]

    def __init__(self):
        self.q = {e: [] for e in self.ENG}
        self.cnt = {e: 0 for e in self.ENG}
        self.seen = {e: {} for e in self.ENG}
        self.dma_cnt = {}

    def _emit(self, eng, fn, deps, dma_key=None):
        sn = self.seen[eng]
        d = {}
        for (k, v) in deps:
            if k == "pe" and eng == "pe":
                continue
            if sn.get(k, 0) < v:
                d[k] = max(d.get(k, 0), v)
        for k, v in d.items():
            sn[k] = v
        if dma_key is None:
            self.cnt[eng] += 1
            tok = (eng, self.cnt[eng])
        else:
            self.dma_cnt[dma_key] = self.dma_cnt.get(dma_key, 0) + 16
            tok = (dma_key, self.dma_cnt[dma_key])
        self.q[eng].append((list(d.items()), fn, tok, dma_key is not None))
        return tok

    def op(self, eng, fn, reads=(), writes=(), dma_key=None, extra=()):
        deps = list(extra)
        for t in reads:
            if t.w is not None:
                deps.append(t.w)
            if t.excl:
                deps.extend((k, v) for k, v in t.r.items() if k != eng)
        for t in writes:
            if t.w is not None:
                deps.append(t.w)
            deps.extend(t.r.items())
        tok = self._emit(eng, fn, deps, dma_key)
        k, v = tok
        for t in reads:
            if t.r.get(k, 0) < v:
                t.r[k] = v
        for t in writes:
            t.w = tok
            t.r = {}
        return tok

    def barrier(self, engines=("pe", "act", "dve")):
        toks = [(e, c) for e, c in self.cnt.items() if c > 0 and e != "sp"]
        for e in engines:
            sn = self.seen[e]
            d = {}
            for (k, v) in toks:
                if k == e:
                    continue
                if sn.get(k, 0) < v:
                    d[k] = v
                    sn[k] = v
            if d:
                self.q[e].append((list(d.items()), None, None, False))

    def run(self, nc):
        keys = sorted(set(self.ENG) | set(self.dma_cnt.keys()))
        with contextlib.ExitStack() as st:
            sems = {k: st.enter_context(nc.semaphore("s_" + k)) for k in keys}
            block = st.enter_context(nc.Block())

            waited = set()
            for name in self.ENG:
                for waits, fn, tok, is_dma in self.q[name]:
                    for kv in waits:
                        waited.add(tuple(kv))

            def mk(name):
                def body(e):
                    pending = 0
                    last_idx = max([i for i, it in enumerate(self.q[name]) if it[1] is not None and not it[3]], default=-1)
                    for idx, (waits, fn, tok, is_dma) in enumerate(self.q[name]):
                        for k, v in waits:
                            e.wait_ge(sems[k], v)
                        if fn is None:
                            continue
                        ins = fn(e)
                        if is_dma:
                            ins.then_inc(sems[tok[0]], 16)
                        elif tuple(tok) in waited or idx == last_idx:
                            ins.then_inc(sems[tok[0]], pending + 1)
                            STATS.setdefault(name, []).append(pending + 1)
                            pending = 0
                        else:
                            pending += 1
                return body

            block.tensor(mk("pe"))
            block.scalar(mk("act"))
            block.vector(mk("dve"))
            block.gpsimd(mk("pool"))
            block.sync(mk("sp"))


def in_chunk_order_a():
    return list(range(24, 32)) + list(range(0, 8)) + list(range(16, 24))


def in_chunk_order_b(layer):
    o = [56]
    if layer > 0:
        o.append(57)
    o += list(range(8, 16))
    for c in range(8):
        o += [32 + c, 40 + c, 48 + c]
    return o


def layer_recipe(layer):
    rec = []
    for which in (1, 2):
        if which == 2:
            ca = in_chunk_order_a()
            for i in range(0, len(ca), 2):
                rec.append(("in", layer, ca[i:i + 2]))
            rec.append(("sgu", layer))
            for mp in range(4):
                rec.append(("pj", layer, "a", mp))
            rec.append(("lora", layer))
            cb = in_chunk_order_b(layer)
            for i in range(0, len(cb), 2):
                rec.append(("in", layer, cb[i:i + 2]))
            for mp in range(4):
                rec.append(("pj", layer, "b", mp))
            for mp in range(4):
                rec.append(("pj", layer, "o", mp))
        f = 1 if which == 1 else 2
        for jg in range(11):
            rec.append(("gu", layer, f, 0, jg))
            rec.append(("gu", layer, f, 1, jg))
        for a in range(2):
            for jq in range(6):
                rec.append(("dn", layer, f, a, jq))
    n_ffn = 22 + 12
    ffn1 = rec[:n_ffn]
    rest = rec[n_ffn:]
    return ffn1 + rest


def _kc(arr):
    n = arr.shape[1]
    return np.ascontiguousarray(arr.reshape(8, 128, n).transpose(1, 0, 2)).reshape(128, 8 * n)


def build_stream(inp):
    slots = []
    for layer in range(2):
        w_in = inp["w_in_first"] if layer == 0 else inp["w_in_rest"][layer - 1]
        for r in layer_recipe(layer):
            s = np.zeros((128, 2048), np.float32)
            kind = r[0]
            if kind == "gu":
                _, l, f, half, jg = r
                w = (inp["ffn1_w_gu"] if f == 1 else inp["ffn2_w_gu"])[l]
                base = half * DFF + jg * 256
                s[:] = _kc(w[:, base:base + 256])
            elif kind == "dn":
                _, l, f, a, jq = r
                w = (inp["ffn1_w_down"] if f == 1 else inp["ffn2_w_down"])[l]
                v = s.reshape(128, 4, 512)
                for i in range(4):
                    j = 4 * jq + i
                    if j < NJ:
                        v[:, i, :] = w[j * 128:(j + 1) * 128, a * 512:(a + 1) * 512]
            elif kind == "in":
                _, l, chunks = r
                v = s.reshape(128, 8, 256)
                for jj, ch in enumerate(chunks):
                    cols = w_in[:, ch * 128:min((ch + 1) * 128, w_in.shape[1])]
                    n = cols.shape[1]
                    v[:, :, jj * 128:jj * 128 + n] = cols.reshape(8, 128, n).transpose(1, 0, 2)
            elif kind == "pj":
                _, l, which, mp = r
                w = {"a": inp["w_proj_a"], "b": inp["w_proj_b"], "o": inp["w_out"]}[which][l]
                s[:] = _kc(w[:, mp * 256:(mp + 1) * 256])
            elif kind == "sgu":
                _, l = r
                ws = inp["sgu_w_s"][l]
                s[:, 0:1024] = ws.transpose(2, 0, 1).reshape(128, 1024)
                s[0, 1024:2048] = inp["sgu_b_s"][l].reshape(1024)
            elif kind == "lora":
                _, l = r
                s[0:64, 0:1024] = inp["rwkv_w2"][l]
                s[64:128, 0:1024] = inp["rwkv_a2"][l]
                if l > 0:
                    s[0:32, 1024:2048] = inp["rwkv_v2"][l - 1]
            slots.append(s)
    return np.stack(slots, 0)


def _vec8(v):
    return np.ascontiguousarray(np.asarray(v, np.float32).reshape(8, 128).T)


def build_cvec(inp):
    out = np.zeros((128, 2, NV), np.float32)
    for l in range(2):
        mu = inp["mu_first"] if l == 0 else inp["mu_rest"][l - 1]
        o = out[:, l, :]

        def put(idx, v):
            o[:, idx * 8:(idx + 1) * 8] = _vec8(v)
        put(V_N1, inp["ffn1_norm"][l]); put(V_NM, inp["mix_norm"][l]); put(V_N2, inp["ffn2_norm"][l])
        put(V_LNG, inp["sgu_ln_g"][l]); put(V_LNB, inp["sgu_ln_b"][l])
        put(V_MUR, mu[0:1024]); put(V_MUK, mu[1024:2048]); put(V_MUV, mu[2048:3072])
        put(V_W0, inp["rwkv_w0"][l]); put(V_A0, inp["rwkv_a0"][l]); put(V_KK, inp["rwkv_k_k"][l])
        put(V_KA, inp["rwkv_k_a"][l]); put(V_RK, inp["rwkv_r_k"][l].reshape(1024))
        put(V_GNG, inp["rwkv_gn_g"][l]); put(V_GNB, inp["rwkv_gn_b"][l])
        if l > 0:
            put(V_V0, inp["rwkv_v0"][l - 1])
        put(V_FIN, inp["final_norm"])
        o[:, V_MUX * 8] = mu[3072:3200]
        if l > 0:
            o[0:32, V_MUX * 8 + 1] = mu[3200:3232]
    return out.reshape(128, 2 * NV)


def build_cmat():
    m = np.zeros((128, NCM), np.float32)
    m[:, CM_ID:CM_ID + 128] = np.eye(128)
    m[:, CM_ONESMEAN:CM_ONESMEAN + 128] = 1.0 / 1024.0
    bd = np.zeros((128, 128), np.float32)
    bd[0:64, 0:64] = 1.0
    bd[64:128, 64:128] = 1.0
    m[:, CM_ONESBD:CM_ONESBD + 128] = bd
    i = (np.arange(128) % 64)[:, None]
    j = np.arange(64)[None, :]
    m[:, CM_MLO:CM_MLO + 64] = (j < i)
    m[:, CM_MUPS:CM_MUPS + 64] = (j > i)
    m[:, CM_MUPI:CM_MUPI + 64] = (j >= i)
    sc = np.ones((512,), np.float32)
    sc[::64] = 0.0
    m[:, CM_SCAN:CM_SCAN + 512] = sc[None, :]
    s = np.arange(128)[:, None]
    t = np.arange(128)[None, :]
    m[:, CM_TRIU:CM_TRIU + 128] = (t >= s)
    m[:, CM_ONES1:CM_ONES1 + 128] = 1.0
    return m


def build_program(ntiles, nlayers=2, stop_after=None):
    ntok = ntiles * TT
    nc = bass.Bass("TRN2", target_bir_lowering=False)
    x_d = nc.dram_tensor("x", [ntok, D], F32, kind="ExternalInput").ap()
    rec = [layer_recipe(l) for l in range(2)]
    nslots = sum(len(r) for r in rec)
    ws_d = nc.dram_tensor("wstream", [nslots, 128, 2048], F32, kind="ExternalInput").ap()
    cv_d = nc.dram_tensor("cvec", [128, 2 * NV], F32, kind="ExternalInput").ap()
    cm_d = nc.dram_tensor("cmat", [128, NCM], F32, kind="ExternalInput").ap()
    y_d = nc.dram_tensor("y", [ntok, D], F32, kind="ExternalOutput").ap()
    dbg16 = nc.dram_tensor("dbg16", [8, 128, 4096], BF16, kind="ExternalOutput").ap() if DBG[0] else None
    dbg32 = nc.dram_tensor("dbg32", [8, 128, 512], F32, kind="ExternalOutput").ap() if DBG[0] else None
    P = Prog()
    layer_base = [0, len(rec[0])]

    with contextlib.ExitStack() as st:
        cnt = [0]

        def sb(shape, dt=F32):
            cnt[0] += 1
            return st.enter_context(nc.sbuf_tensor("sb%d" % cnt[0], shape, dt))

        banks = []
        for i in range(8):
            banks.append(st.enter_context(nc.psum_tensor("bank%d" % i, [128, 512], F32)))
        tbank = [Trk(excl=True) for _ in range(8)]

        def bk(i):
            return banks[i][:]

        def bk16(i):
            return banks[i][:].bitcast(BF16)

        x = sb([128, 8, 512]); tx = [Trk() for _ in range(8)]
        ring = [sb([128, 2048], BF16) for _ in range(R_RING)]
        tring = [Trk() for _ in range(R_RING)]
        cvec = sb([128, 2 * NV]); tcv = Trk()
        cmat = sb([128, NCM]); tcm = Trk()
        identb = sb([128, 128], BF16); onesmeanb = sb([128, 128], BF16); onesbdb = sb([128, 128], BF16)
        ones1b = sb([1, 128], BF16)
        Sst = sb([128, 2, 8, 64], BF16); tS = [Trk(), Trk()]
        carry = sb([128, 2, 2, 26]); tcar = [[[Trk() for _ in range(26)] for _ in range(2)] for _ in range(2)]
        cur_tile = [0]
        vfirst = sb([128, 8, 512], BF16); tvf = [Trk() for _ in range(8)]
        M_h = sb([128, 8, 512], BF16)
        M_sga = sb([128, 8, 512], BF16)
        M_sgb = sb([128, 8, 512], BF16)
        M_mer = sb([128, 8, 512], BF16)
        M_yb = sb([128, 8, 512], BF16)
        M_bon = sb([128, 8, 512], BF16)
        lora = sb([128, 2048], BF16)
        U = sb([128, 10240])
        Vr = sb([128, 13312])

        def cv(l, idx, c=0, rows=slice(0, 128)):
            o = l * NV + idx * 8 + c
            return cvec[rows, o:o + 1]

        def cm(off, n, rows=slice(0, 128)):
            return cmat[rows, off:off + n]

        P.op("sp", lambda e: e.dma_start(out=cvec[:], in_=cv_d), writes=[tcv], dma_key="c0")
        P.op("sp", lambda e: e.dma_start(out=cmat[:], in_=cm_d), writes=[tcm], dma_key="c1")
        for l in range(2):
            for (src, dst) in ((V_MUR, V_OMR), (V_MUK, V_OMK), (V_MUV, V_OMV), (V_MUX, V_OMX), (V_KA, V_OKA)):
                P.op("dve", lambda e, l=l, src=src, dst=dst: e.tensor_scalar(
                    out=cvec[:, l * NV + dst * 8:l * NV + dst * 8 + 8], in0=cvec[:, l * NV + src * 8:l * NV + src * 8 + 8],
                    scalar1=-1.0, scalar2=1.0, op0=ALU.mult, op1=ALU.add), reads=[], writes=[tcv])
        P.op("dve", lambda e: e.tensor_copy(out=identb[:], in_=cm(CM_ID, 128)), reads=[tcm], writes=[tcm])
        P.op("dve", lambda e: e.tensor_copy(out=onesmeanb[:], in_=cm(CM_ONESMEAN, 128)), writes=[tcm])
        P.op("dve", lambda e: e.tensor_copy(out=onesbdb[:], in_=cm(CM_ONESBD, 128)), writes=[tcm])
        P.op("dve", lambda e: e.tensor_copy(out=ones1b[:], in_=cmat[0:1, CM_ONES1:CM_ONES1 + 128]), writes=[tcm])
        P.barrier()

        spos = [0]

        def next_slot(layer, expect):
            li = spos[0]
            assert rec[layer][li][0] == expect[0], (rec[layer][li], expect)
            spos[0] += 1
            gi = layer_base[layer] + li
            nslot[0] += 1
            s = nslot[0] % R_RING
            P.op("pool", lambda e, s=s, gi=gi: e.dma_start(out=ring[s][:], in_=ws_d[gi]), writes=[tring[s]],
                 dma_key="w%d" % s)
            return ring[s], tring[s]

        nslot = [0]

        def mm(out, lhsT, rhs, start, stop, reads, writes):
            P.op("pe", lambda e: e.matmul(out, lhsT=lhsT, rhs=rhs, start=start, stop=stop), reads=reads, writes=writes)

        def tr(out, in_, ident, reads, writes):
            P.op("pe", lambda e: e.transpose(out, in_, ident), reads=reads, writes=writes)

        def act(out, in_, func, reads, writes, bias=None, scale=None):
            kw = {}
            if bias is not None:
                kw["bias"] = bias
            if scale is not None:
                kw["scale"] = scale
            P.op("act", lambda e: e.activation(out=out, in_=in_, func=func, **kw), reads=reads, writes=writes)

        def tt(out, in0, in1, op, reads, writes, eng="dve"):
            P.op(eng, lambda e: e.tensor_tensor(out=out, in0=in0, in1=in1, op=op), reads=reads, writes=writes)

        def stt(out, in0, scalar, in1, op0, op1, reads, writes):
            P.op("dve", lambda e: e.scalar_tensor_tensor(out=out, in0=in0, scalar=scalar, in1=in1, op0=op0, op1=op1),
                 reads=reads, writes=writes)

        def tsc(out, in0, s1, s2, op0, op1, reads, writes, eng="dve"):
            P.op(eng, lambda e: e.tensor_scalar(out=out, in0=in0, scalar1=s1, scalar2=s2, op0=op0, op1=op1),
                 reads=reads, writes=writes)

        def cp(out, in_, reads, writes, eng="dve"):
            if eng == "act":
                P.op(eng, lambda e: e.activation(out=out, in_=in_, func=AF.Copy), reads=reads, writes=writes)
            else:
                P.op(eng, lambda e: e.tensor_copy(out=out, in_=in_), reads=reads, writes=writes)

        def recip(out, in_, reads, writes):
            P.op("dve", lambda e: e.reciprocal(out=out, in_=in_), reads=reads, writes=writes)

        def Uf(off, n):
            return U[:, off:off + n]

        def U16(off, n):
            return U[:, off:off + n].bitcast(BF16)

        def Vf(off, n):
            return Vr[:, off:off + n]

        def V16(off, n):
            return Vr[:, off:off + n].bitcast(BF16)

        def rmsnorm(gidx, l, h, th, tmpbase=0):
            sq = [V16(tmpbase, 256), V16(tmpbase + 256, 256)]
            tsq = [Trk(), Trk()]
            rstd = Vf(tmpbase + 512, 512); trs = Trk()
            for c in range(8):
                act(sq[c % 2], x[:, c, :], AF.Square, [tx[c]], [tsq[c % 2]])
                mm(bk(4), onesmeanb[:], sq[c % 2], c == 0, c == 7, [tsq[c % 2]], [tbank[4]])
            act(rstd, bk(4), AF.Ln, [tbank[4]], [trs], bias=NORM_EPS)
            act(rstd, rstd, AF.Exp, [trs], [trs], scale=-0.5)
            for c in range(8):
                stt(h[:, c, :], x[:, c, :], cv(l, gidx, c), rstd, ALU.mult, ALU.mult, [tx[c], trs, tcv], [th[c]])

        def ffn(l, f):
            P.barrier()
            h = U16(0, 2048).rearrange("p (c t) -> p c t", c=8)
            g = U16(2048, 5632).rearrange("p (j t) -> p j t", j=NJ)
            th = [Trk() for _ in range(8)]; tg = [Trk() for _ in range(NJ)]
            sgt = [Uf(7680, 512), Uf(8192, 512)]; tsg = [Trk(), Trk()]
            rmsnorm(V_N1 if f == 1 else V_N2, l, h, th)
            for jg in range(11):
                sg_, tsg_ = next_slot(l, ("gu",))
                su_, tsu_ = next_slot(l, ("gu",))
                sgv = sg_[:].rearrange("p (k n) -> p k n", k=8)
                suv = su_[:].rearrange("p (k n) -> p k n", k=8)
                for jj in range(2):
                    j = 2 * jg + jj
                    bg = j % 2; bu = 2 + j % 2
                    for k in range(8):
                        mm(bk(bg), sgv[:, k, jj * 128:(jj + 1) * 128], h[:, k, :], k == 0, k == 7, [tsg_, th[k]], [tbank[bg]])
                    for k in range(8):
                        mm(bk(bu), suv[:, k, jj * 128:(jj + 1) * 128], h[:, k, :], k == 0, k == 7, [tsu_, th[k]], [tbank[bu]])
                    act(sgt[j % 2], bk(bg), AF.Silu, [tbank[bg]], [tsg[j % 2]])
                    tt(g[:, j, :], sgt[j % 2], bk(bu), ALU.mult, [tsg[j % 2], tbank[bu]], [tg[j]])
            for a in range(2):
                for jq in range(6):
                    sd_, tsd_ = next_slot(l, ("dn",))
                    sdv = sd_[:].rearrange("p (i n) -> p i n", i=4)
                    for i in range(4):
                        j = 4 * jq + i
                        if j >= NJ:
                            continue
                        for m in range(4):
                            bb = (4 if a == 0 else 0) + m
                            mm(bk(bb), sdv[:, i, m * 128:(m + 1) * 128], g[:, j, :], j == 0, j == NJ - 1,
                               [tsd_, tg[j]], [tbank[bb]])
                for m in range(4):
                    c = a * 4 + m
                    bb = (4 if a == 0 else 0) + m
                    stt(x[:, c, :], bk(bb), 0.5, x[:, c, :], ALU.mult, ALU.add, [tbank[bb]], [tx[c]])

        def inproj(slotv, tslot, jj, h, th, b, mcols=128):
            for k in range(8):
                mm(bk(b)[0:mcols, :], slotv[:, k, jj * 128:jj * 128 + mcols], h[:, k, :], k == 0, k == 7,
                   [tslot, th[k]], [tbank[b]])

        class InStream:
            def __init__(self, l, order, h, th):
                self.l = l; self.order = order; self.h = h; self.th = th; self.i = 0
                self.slot = None

            def next(self, mcols=128):
                if self.i % 2 == 0:
                    s_, t_ = next_slot(self.l, ("in",))
                    self.slot = (s_[:].rearrange("p (k n) -> p k n", k=8), t_)
                b = self.i % 2
                inproj(self.slot[0], self.slot[1], self.i % 2, self.h, self.th, b, mcols)
                self.i += 1
                return b

        def token_shift(b, l, muidx, omidx, c, cidx, out, tout, rows=slice(0, 128)):
            pw = cur_tile[0] % 2; pr = 1 - pw
            act(out, bk(b)[rows, :], AF.Copy, [tbank[b], tcv], [tout], scale=cv(l, omidx, c, rows))
            cp(carry[rows, pw, l, cidx:cidx + 1], bk(b)[rows, 511:512], [tbank[b]], [tcar[pw][l][cidx]], eng="act")
            stt(out[:, 1:512], bk(b)[rows, 0:511], cv(l, muidx, c, rows), out[:, 1:512], ALU.mult, ALU.add,
                [tbank[b], tcv], [tout])
            stt(out[:, 0:1], carry[rows, pr, l, cidx:cidx + 1], cv(l, muidx, c, rows), out[:, 0:1], ALU.mult, ALU.add,
                [tcar[pr][l][cidx], tcv], [tout])

        def mixer(l):
            P.barrier()
            h = M_h; th = [Trk() for _ in range(8)]
            rmsnorm(V_NM, l, h, th)
            sga = M_sga; tsga = [Trk() for _ in range(8)]
            sgb = M_sgb; tsgb = [Trk() for _ in range(8)]
            mer = M_mer; tmer = [Trk() for _ in range(8)]
            ua = U16(0, 2048).rearrange("p (c t) -> p c t", c=8); tua = [Trk() for _ in range(8)]
            gv = Uf(2048, 4096).rearrange("p (c t) -> p c t", c=8); tgv = [Trk() for _ in range(8)]
            vn = U16(6144, 2048).rearrange("p (c t) -> p c t", c=8); tvn = [Trk() for _ in range(8)]
            vnT = U16(8192, 2048).rearrange("p (c t) -> p c t", c=8); tvnT = [Trk() for _ in range(8)]
            ins = InStream(l, in_chunk_order_a(), h, th)
            sq16 = [V16(1024, 256), V16(1280, 256)]; tsq16 = [Trk(), Trk()]
            gvb = [V16(1536, 256), V16(1792, 256)]; tgvb = [Trk(), Trk()]
            for c in range(8):
                b = ins.next()
                act(gv[:, c, :], bk(b), AF.Gelu_apprx_tanh, [tbank[b]], [tgv[c]])
                act(sq16[c % 2], gv[:, c, :], AF.Square, [tgv[c]], [tsq16[c % 2]])
                cp(gvb[c % 2], gv[:, c, :], [tgv[c]], [tgvb[c % 2]])
                mm(bk(4), onesmeanb[:], gvb[c % 2], c == 0, c == 7, [tgvb[c % 2]], [tbank[4]])
                mm(bk(5), onesmeanb[:], sq16[c % 2], c == 0, c == 7, [tsq16[c % 2]], [tbank[5]])
            mean = Vf(2048, 512); tmean = Trk()
            var = Vf(2560, 512); tvar = Trk()
            act(mean, bk(4), AF.Copy, [tbank[4]], [tmean])
            tt(var, mean, mean, ALU.mult, [tmean], [tvar])
            tt(var, bk(5), var, ALU.subtract, [tbank[5], tvar], [tvar])
            act(var, var, AF.Ln, [tvar], [tvar], bias=LN_EPS)
            act(var, var, AF.Exp, [tvar], [tvar], scale=-0.5)
            tmpl = [Vf(3072, 512), Vf(3584, 512)]; ttmpl = [Trk(), Trk()]
            for c in range(8):
                t_ = tmpl[c % 2]; tt_ = ttmpl[c % 2]
                tt(t_, gv[:, c, :], mean, ALU.subtract, [tgv[c], tmean], [tt_])
                tt(t_, t_, var, ALU.mult, [tt_, tvar], [tt_])
                act(vn[:, c, :], t_, AF.Identity, [tt_, tcv], [tvn[c]], bias=cv(l, V_LNB, c), scale=cv(l, V_LNG, c))
                b = ins.next()
                act(sga[:, c, :], bk(b), AF.Sigmoid, [tbank[b]], [tsga[c]])
            for c in range(8):
                b = ins.next()
                act(ua[:, c, :], bk(b), AF.Gelu_apprx_tanh, [tbank[b]], [tua[c]])
            ssg, tssg = next_slot(l, ("sgu",))
            wsT = V16(4096, 512).rearrange("p (g t) -> p g t", g=8); twsT = Trk()
            tt(wsT, ssg[:, 0:1024].rearrange("p (g t) -> p g t", g=8),
               cm(CM_TRIU, 128).unsqueeze(1).broadcast_to([128, 8, 128]), ALU.mult, [tssg, tcm], [twsT])
            for c in range(8):
                b = 6 + (c // 2) % 2
                off = (c % 2) * 512
                for cc in range(4):
                    tr(bk16(b)[:, off + cc * 128:off + (cc + 1) * 128], vn[:, c, cc * 128:(cc + 1) * 128], identb[:],
                       [tvn[c], tcm], [tbank[b]])
                cp(vnT[:, c, :], bk16(b)[:, off:off + 512], [tbank[b]], [tvnT[c]], eng="act" if c % 2 else "dve")
            for c in range(8):
                b = 2 + c % 2
                for cc in range(4):
                    mm(bk(b)[:, cc * 128:(cc + 1) * 128], vnT[:, c, cc * 128:(cc + 1) * 128], wsT[:, c, :], True, False,
                       [tvnT[c], twsT], [tbank[b]])
                    mm(bk(b)[:, cc * 128:(cc + 1) * 128], ones1b[0:1, :], ssg[0:1, 1024 + c * 128:1024 + (c + 1) * 128],
                       False, True, [tssg, tcm], [tbank[b]])
                tt(ua[:, c, :], bk(b), ua[:, c, :], ALU.mult, [tbank[b]], [tua[c]])
            for mp in range(4):
                sp_, tsp_ = next_slot(l, ("pj",))
                spv = sp_[:].rearrange("p (k n) -> p k n", k=8)
                for mm_ in range(2):
                    m = 2 * mp + mm_
                    b = m % 2
                    for c in range(8):
                        mm(bk(b), spv[:, c, mm_ * 128:(mm_ + 1) * 128], ua[:, c, :], c == 0, c == 7, [tsp_, tua[c]], [tbank[b]])
                    tt(mer[:, m, :], bk(b), sga[:, m, :], ALU.mult, [tbank[b], tsga[m]], [tmer[m]])
            if DBG[0] == 'sgu':
                spos[0] += 1 + (len(in_chunk_order_b(l)) + 1) // 2 + 4
            if DBG[0] != 'sgu':
                P.barrier()
                til = [U16(i * 2048, 2048).rearrange("p (c t) -> p c t", c=8) for i in range(5)]
                ttil = [[Trk() for _ in range(8)] for _ in range(5)]
                Rt, Kt, Bt, At, Vt = til
                bon = M_bon; tbon = [Trk() for _ in range(8)]
                TA = [Vf(i * 512, 512) for i in range(11)]; tTA = [Trk() for _ in range(11)]
                TB = [Vf(7680 + i * 512, 512) for i in range(11)]; tTB = [Trk() for _ in range(11)]
                T = TA; tT = tTA
                sqb = V16(5632, 256); tsqb = Trk()
                rkb = V16(5888, 256); trkb = Trk()
                twad = V16(6144, 256); ttw = Trk()
                vres = V16(6400, 256); tvres = Trk()
                PC = Vf(6656, 64).rearrange("p (c q) -> p c q", c=8); tPC = Trk()
                slo_r, tslo_r = next_slot(l, ("lora",))
                slo = lora; tslo = Trk()
                cp(lora[:], slo_r[:], [tslo_r], [tslo])
                ins = InStream(l, in_chunk_order_b(l), h, th)
                b = ins.next()
                token_shift(b, l, V_MUX, V_OMX, 0, 24, T[0], tT[0])
                act(twad[0:64, :], T[0][0:64, :], AF.Tanh, [tT[0]], [ttw])
                act(twad[64:128, :], T[0][64:128, :], AF.Copy, [tT[0]], [ttw])
                if l > 0:
                    b = ins.next(mcols=32)
                    token_shift(b, l, V_MUX, V_OMX, 1, 25, T[1][0:32, :], tT[1], rows=slice(0, 32))
                    cp(vres[0:32, :], T[1][0:32, :], [tT[1]], [tvres])
                for c in range(8):
                    b = ins.next()
                    act(sgb[:, c, :], bk(b), AF.Sigmoid, [tbank[b]], [tsgb[c]])
                def stageA0(c):
                        T, tT = (TA, tTA) if c % 2 == 0 else (TB, tTB)
                        rb, kb, vb = T[0], T[1], T[2]
                        b = ins.next(); token_shift(b, l, V_MUR, V_OMR, c, c, rb, tT[0])
                        b = ins.next(); token_shift(b, l, V_MUK, V_OMK, c, 8 + c, kb, tT[1])
                        b = ins.next(); token_shift(b, l, V_MUV, V_OMV, c, 16 + c, vb, tT[2])
                def stageA1(c):
                        T, tT = (TA, tTA) if c % 2 == 0 else (TB, tTB)
                        rb, kb, vb = T[0], T[1], T[2]
                        mm(bk(2), slo[0:64, c * 128:(c + 1) * 128], twad[0:64, :], True, True, [tslo, ttw], [tbank[2]])
                        mm(bk(3), slo[64:128, c * 128:(c + 1) * 128], twad[64:128, :], True, True, [tslo, ttw], [tbank[3]])
                        act(T[3], bk(2), AF.Sigmoid, [tbank[2], tcv], [tT[3]], bias=cv(l, V_W0, c))
                        act(T[4], bk(3), AF.Sigmoid, [tbank[3], tcv], [tT[4]], bias=cv(l, V_A0, c))
                        P.op("dve", lambda e, o_=T[5], i_=T[3]: e.tensor_tensor_scan(out=o_, data0=cm(CM_SCAN, 512), data1=i_, initial=0.0,
                                                                   op0=ALU.mult, op1=ALU.add), reads=[tT[3], tcm], writes=[tT[5]])
                        tt(T[6], T[5], T[3], ALU.subtract, [tT[5], tT[3]], [tT[6]])
                        act(T[7], T[5], AF.Exp, [tT[5]], [tT[7]], scale=-C0)
                        act(T[8], T[5], AF.Exp, [tT[5]], [tT[8]], scale=C0)
                        act(T[6], T[6], AF.Exp, [tT[6]], [tT[6]], scale=-C0)
                        cp(PC[:, c, :], T[7].rearrange("p (q t) -> p q t", q=8)[:, :, 63], [tT[7]], [tPC])
                        act(sqb, kb, AF.Square, [tT[1], tcv], [tsqb], scale=cv(l, V_KK, c))
                        mm(bk(4), onesbdb[:], sqb, True, True, [tsqb, tcm], [tbank[4]])
                        tsc(T[10], bk(4), 1e-18, None, ALU.max, ALU.bypass, [tbank[4]], [tT[10]])
                        act(T[10], T[10], AF.Ln, [tT[10]], [tT[10]])
                        act(T[10], T[10], AF.Exp, [tT[10]], [tT[10]], scale=-0.5)
                        stt(T[9], kb, cv(l, V_KK, c), T[10], ALU.mult, ALU.mult, [tT[1], tT[10], tcv], [tT[9]])
                        if l > 0:
                            mm(bk(5)[:, :], slo[0:32, 1024 + c * 128:1024 + (c + 1) * 128], vres[0:32, :], True, True,
                               [tslo, tvres], [tbank[5]])
                            act(T[5], bk(5), AF.Sigmoid, [tbank[5], tcv], [tT[5]], bias=cv(l, V_V0, c))
                def stageB(c):
                        T, tT = (TA, tTA) if c % 2 == 0 else (TB, tTB)
                        rb, kb, vb = T[0], T[1], T[2]
                        tsc(T[3], T[4], cv(l, V_KA, c), cv(l, V_OKA, c), ALU.mult, ALU.add, [tT[4], tcv], [tT[3]])
                        tt(T[3], kb, T[3], ALU.mult, [tT[1], tT[3]], [tT[3]])
                        tt(T[10], T[9], T[4], ALU.mult, [tT[9], tT[4]], [tT[10]])
                        if l > 0:
                            tt(T[4], vfirst[:, c, :], vb, ALU.subtract, [tvf[c], tT[2]], [tT[4]])
                            tt(T[4], T[4], T[5], ALU.mult, [tT[4], tT[5]], [tT[4]])
                            tt(vb, vb, T[4], ALU.add, [tT[2], tT[4]], [tT[2]])
                        else:
                            cp(vfirst[:, c, :], vb, [tT[2]], [tvf[c]], eng="act")
                        stt(M_yb[:, c, :], rb, cv(l, V_RK, c), T[3], ALU.mult, ALU.mult, [tT[0], tT[3], tcv], [trk8[c]])
                        tt(Rt[:, c, :], rb, T[7], ALU.mult, [tT[0], tT[7]], [ttil[0][c]])
                        tt(Kt[:, c, :], T[3], T[8], ALU.mult, [tT[3], tT[8]], [ttil[1][c]])
                        tt(Bt[:, c, :], T[10], T[8], ALU.mult, [tT[10], tT[8]], [ttil[2][c]])
                        stt(At[:, c, :], T[9], -1.0, T[6], ALU.mult, ALU.mult, [tT[9], tT[6]], [ttil[3][c]])
                        cp(Vt[:, c, :], vb, [tT[2]], [ttil[4][c]], eng="act")
                trk8 = [Trk() for _ in range(8)]
                stageA0(0)
                for c in range(8):
                    if c >= 1:
                        stageB(c - 1)
                    if c + 1 <= 7:
                        stageA0(c + 1)
                    stageA1(c)
                stageB(7)
                for c in range(8):
                    b = 6 if c % 2 == 0 else 2
                    mm(bk(b), onesbdb[:], M_yb[:, c, :], True, True, [trk8[c], tcm], [tbank[b]])
                    tt(bon[:, c, :], bk(b), Vt[:, c, :], ALU.mult, [tbank[b], ttil[4][c]], [tbon[c]])
            if DBG[0] == 'prep':
                spos[0] += 4
                for i in range(5):
                    tok = P.op("sp", lambda e, i=i: e.dma_start(out=dbg16[i], in_=U[:, i * 2048:(i + 1) * 2048].bitcast(BF16)),
                               reads=ttil[i], dma_key="dbg")
                    P.q["sp"].append(([tok], None, None, False))
                for i in range(8):
                    tok = P.op("sp", lambda e, i=i: e.dma_start(out=dbg32[i], in_=T[3 + i]), reads=[tT[3 + i]], dma_key="dbg")
                    P.q["sp"].append(([tok], None, None, False))
            if DBG[0] not in ('sgu', 'prep'):
                P.barrier()
                def bd(off):
                    return V16(off, 512).rearrange("p (c n) -> p c n", c=8)
                def st64(off):
                    return V16(off, 256).rearrange("p (c n) -> p c n", c=8)
                Abd = [bd(0), bd(512)]; Bbd = [bd(1024), bd(1536)]; Ttb = [bd(2048), bd(2560)]
                tAbd = [[Trk(), Trk()], [Trk(), Trk()]]; tBbd = [[Trk(), Trk()], [Trk(), Trk()]]
                tTt = [[Trk(), Trk()], [Trk(), Trk()]]
                AkT = st64(3072); ArbT = st64(3328); ArkT = st64(3584)
                Kst = st64(3840); Bst = st64(4096); Vst = st64(4352); Xs = st64(4608); Us = st64(4864); ynb = st64(5120)
                tAk, tArb, tArk, tKst, tBst, tVst, tXs, tUs, tynb = [Trk() for _ in range(9)]
                ysb = Vf(6144, 512); tysb = Trk()
                stat = Vf(6720, 64); tstat = Trk()
                ysq = Vf(6784, 512); tysq = Trk()
                P.op("dve", lambda e: e.memset(Abd[0], 0.0), writes=tAbd[0])
                P.op("dve", lambda e: e.memset(Bbd[0], 0.0), writes=tBbd[0])
                S = Sst[:, l, :, :]
                R0 = slice(0, 64); R1 = slice(64, 128)
                RW = (R0, R1)
                idb = identb[:].unsqueeze(1).broadcast_to([128, 8, 128])
                v3 = lambda ap, n=8: ap.rearrange("p (c n) -> p c n", c=n)
                for q in range(8):
                    cs = slice(q * 64, (q + 1) * 64)
                    prods = [(3, 2, 0), (2, 3, 1), (1, 3, 2), (2, 0, 3), (1, 0, 4)]
                    for hh in range(2):
                        rows = RW[hh]
                        for (li, ri, b) in prods:
                            for c in range(8):
                                mm(bk(b)[rows, c * 64:(c + 1) * 64], til[li][rows, c, cs], til[ri][rows, c, cs], True, True,
                                   [ttil[li][c], ttil[ri][c]], [tbank[b]])
                    for hh in range(2):
                        rows = RW[hh]
                        tt(Abd[0][rows, :, hh * 64:(hh + 1) * 64], v3(bk(0)[rows, :]),
                           cm(CM_MLO, 64, rows).unsqueeze(1).broadcast_to([64, 8, 64]), ALU.mult, [tbank[0], tcm], tAbd[0])
                        tt(Bbd[0][rows, :, hh * 64:(hh + 1) * 64], v3(bk(1)[rows, :]),
                           cm(CM_MUPS, 64, rows).unsqueeze(1).broadcast_to([64, 8, 64]), ALU.mult, [tbank[1], tcm], tBbd[0])
                    for (dst, tdst, moff, b) in ((AkT, tAk, CM_MUPS, 2), (ArbT, tArb, CM_MUPI, 3), (ArkT, tArk, CM_MUPI, 4)):
                        tt(dst, v3(bk(b)), cm(moff, 64).unsqueeze(1).broadcast_to([128, 8, 64]), ALU.mult, [tbank[b], tcm], [tdst])
                    for hh in range(2):
                        rows = RW[hh]
                        for (src, tsrc, b, off) in ((Kt, ttil[1], 5, 0), (Bt, ttil[2], 5, 512), (Vt, ttil[4], 6, 0)):
                            for c in range(8):
                                tr(bk16(b)[rows, off + c * 64:off + (c + 1) * 64], src[rows, c, cs], identb[rows, rows],
                                   [tsrc[c], tcm], [tbank[b]])
                    cp(Kst, v3(bk16(5)[:, 0:512]), [tbank[5]], [tKst], eng="act")
                    cp(Bst, v3(bk16(5)[:, 512:1024]), [tbank[5]], [tBst], eng="act")
                    cp(Vst, v3(bk16(6)[:, 0:512]), [tbank[6]], [tVst], eng="act")
                    def x_terms(hh):
                        rows = RW[hh]
                        for c in range(8):
                            osl = slice(c * 64, (c + 1) * 64)
                            mm(bk(5)[rows, osl], AkT[rows, c, :], Vst[rows, c, :], c == 0, False, [tAk, tVst], [tbank[5]])
                            mm(bk(5)[rows, osl], At[rows, c, cs], S[rows, c, :], False, True, [ttil[3][c], tS[l]], [tbank[5]])
                    def y1_terms(hh):
                        rows = RW[hh]
                        for c in range(8):
                            osl = slice(c * 64, (c + 1) * 64)
                            mm(bk(6)[rows, osl], ArkT[rows, c, :], Vst[rows, c, :], c == 0, False, [tArk, tVst], [tbank[6]])
                            mm(bk(6)[rows, osl], Rt[rows, c, cs], S[rows, c, :], False, False, [ttil[0][c], tS[l]], [tbank[6]])
                    def s1_terms(hh):
                        rows = RW[hh]
                        for c in range(8):
                            osl = slice(c * 64, (c + 1) * 64)
                            mm(bk(1)[rows, osl], Kst[rows, c, :], Vst[rows, c, :], c == 0, False, [tKst, tVst], [tbank[1]])
                            mm(bk(1)[rows, osl], identb[rows, rows], S[rows, c, :], False, False, [tcm, tS[l]], [tbank[1]])
                    def ys2_terms(hh):
                        rows = RW[hh]
                        for c in range(8):
                            osl = slice(c * 64, (c + 1) * 64)
                            mm(bk(6)[rows, osl], ArbT[rows, c, :], Us[rows, c, :], False, True, [tArb, tUs], [tbank[6]])
                        for c in range(8):
                            osl = slice(c * 64, (c + 1) * 64)
                            mm(bk(1)[rows, osl], Bst[rows, c, :], Us[rows, c, :], False, True, [tBst, tUs], [tbank[1]])
                    cur = 0
                    bT = (0, 3); bA = (1, 4); bB = (2, 7)
                    for lev in range(6):
                        nxt = 1 - cur
                        last = lev == 5
                        for half in range(2):
                            for ci in range(4):
                                c = half * 4 + ci
                                osl = slice(ci * 128, (ci + 1) * 128)
                                if lev > 0:
                                    mm(bk(bT[half])[:, osl], Abd[cur][:, c, :], Ttb[cur][:, c, :], True, True,
                                       [tAbd[cur][half], tTt[cur][half]], [tbank[bT[half]]])
                                if not last:
                                    mm(bk(bA[half])[:, osl], Bbd[cur][:, c, :], Abd[cur][:, c, :], True, True,
                                       [tAbd[cur][half], tBbd[cur][half]], [tbank[bA[half]]])
                                    if lev < 4:
                                        mm(bk(bB[half])[:, osl], Abd[cur][:, c, :], Bbd[cur][:, c, :], True, True,
                                           [tAbd[cur][half], tBbd[cur][half]], [tbank[bB[half]]])
                        fill = {0: (x_terms, 0), 1: (y1_terms, 0), 2: (x_terms, 1), 3: (y1_terms, 1), 5: (s1_terms, 0)}.get(lev)
                        if fill is not None:
                            fill[0](fill[1])
                        if lev == 0:
                            tt(Ttb[nxt], Bbd[0], idb, ALU.add, tBbd[0] + [tcm], tTt[nxt])
                        for half in range(2):
                            hs = slice(half * 4, half * 4 + 4)
                            if lev > 0:
                                tt(Ttb[nxt][:, hs, :], Ttb[cur][:, hs, :], v3(bk(bT[half]), 4), ALU.add,
                                   [tTt[cur][half], tbank[bT[half]]], [tTt[nxt][half]])
                            if not last:
                                cp(Abd[nxt][:, hs, :], v3(bk(bA[half]), 4), [tbank[bA[half]]], [tAbd[nxt][half]], eng="act")
                                if lev < 4:
                                    cp(Bbd[nxt][:, hs, :], v3(bk(bB[half]), 4), [tbank[bB[half]]], [tBbd[nxt][half]],
                                       eng="dve" if half == 0 else "act")
                        cur = nxt
                    Tfin = Ttb[cur]; tTfin = tTt[cur]
                    cp(Xs, v3(bk(5)), [tbank[5]], [tXs])
                    for c in range(8):
                        osl = slice(c * 64, (c + 1) * 64)
                        mm(bk(7)[:, osl], Tfin[:, c, :], Xs[:, c, :], True, True, [tTfin[c // 4], tXs], [tbank[7]])
                    s1_terms(1)
                    cp(Us, v3(bk(7)), [tbank[7]], [tUs], eng="act")
                    ys2_terms(0); ys2_terms(1)
                    tt(S, v3(bk(1)), PC[:, :, q:q + 1].broadcast_to([128, 8, 64]), ALU.mult, [tbank[1], tPC], [tS[l]])
                    act(ysb, bk(6), AF.Copy, [tbank[6]], [tysb])
                    act(ysq, bk(6), AF.Square, [tbank[6]], [tysq])
                    s1 = stat[:, 0:8]; s2 = stat[:, 8:16]; mean = stat[:, 16:24]; msq = stat[:, 24:32]; var = stat[:, 32:40]
                    P.op("dve", lambda e, s1=s1: e.tensor_reduce(out=s1, in_=v3(ysb), axis=AX.X, op=ALU.add),
                         reads=[tysb], writes=[tstat])
                    P.op("dve", lambda e, s2=s2: e.tensor_reduce(out=s2, in_=v3(ysq), axis=AX.X, op=ALU.add),
                         reads=[tysq, tstat], writes=[tstat])
                    tsc(mean, s1, 1.0 / 64, None, ALU.mult, ALU.bypass, [tstat], [tstat])
                    tt(msq, mean, mean, ALU.mult, [tstat], [tstat])
                    stt(var, s2, 1.0 / 64, msq, ALU.mult, ALU.subtract, [tstat], [tstat])
                    act(var, var, AF.Sqrt, [tstat], [tstat], bias=GN_EPS)
                    recip(var, var, [tstat], [tstat])
                    y3 = v3(ysb)
                    tt(y3, y3, mean.unsqueeze(2).broadcast_to([128, 8, 64]), ALU.subtract, [tysb, tstat], [tysb])
                    tt(ynb, y3, var.unsqueeze(2).broadcast_to([128, 8, 64]), ALU.mult, [tysb, tstat], [tynb])
                    for hh in range(2):
                        rows = RW[hh]
                        b = 7 if hh == 0 else 5
                        for c in range(8):
                            tr(bk16(b)[rows, 512 + c * 64:512 + (c + 1) * 64], ynb[rows, c, :], identb[rows, rows], [tynb, tcm], [tbank[b]])
                        cp(M_yb[rows, :, cs], v3(bk16(b)[rows, 512:1024]), [tbank[b]], [tyb_all], eng="act")
                tmpa = [ysb, ysq]
                ttmpa = [tysb, tysq]
                for c in range(8):
                    t_ = tmpa[c % 2]; tt__ = ttmpa[c % 2]
                    act(t_, M_yb[:, c, :], AF.Identity, [tyb_all, tcv], [tt__], bias=cv(l, V_GNB, c), scale=cv(l, V_GNG, c))
                    tt(M_yb[:, c, :], t_, bon[:, c, :], ALU.add, [tt__, tbon[c]], [tybc[c]])
                for mp in range(4):
                    sp_, tsp_ = next_slot(l, ("pj",))
                    spv = sp_[:].rearrange("p (k n) -> p k n", k=8)
                    for mm_ in range(2):
                        m = 2 * mp + mm_
                        b = m % 2
                        for c in range(8):
                            mm(bk(b), spv[:, c, mm_ * 128:(mm_ + 1) * 128], M_yb[:, c, :], c == 0, c == 7, [tsp_, tybc[c]], [tbank[b]])
                        t_ = tmpa[m % 2]; tt__ = ttmpa[m % 2]
                        tt(t_, bk(b), sgb[:, m, :], ALU.mult, [tbank[b], tsgb[m]], [tt__])
                        tt(mer[:, m, :], t_, mer[:, m, :], ALU.add, [tt__], [tmer[m]])
            for mp in range(4):
                sp_, tsp_ = next_slot(l, ("pj",))
                spv = sp_[:].rearrange("p (k n) -> p k n", k=8)
                for mm_ in range(2):
                    m = 2 * mp + mm_
                    b = 2 + m % 2
                    for c in range(8):
                        mm(bk(b), spv[:, c, mm_ * 128:(mm_ + 1) * 128], mer[:, c, :], c == 0, c == 7, [tsp_, tmer[c]], [tbank[b]])
                    tt(x[:, m, :], bk(b), x[:, m, :], ALU.add, [tbank[b]], [tx[m]])

        tyb_all = Trk()
        tQ1g = Trk(); tP1g = Trk()
        tybc = [Trk() for _ in range(8)]

        tiles_per_seq = SEQ // TT
        xt = Uf(0, 4096).rearrange("p (s d) -> p s d", s=4)
        yf = Uf(4096, 4096).rearrange("p (c t) -> p c t", c=8)
        for tile in range(ntiles):
            cur_tile[0] = tile
            P.barrier()
            txt = Trk()
            if tile % tiles_per_seq == 0:
                P.op("dve", lambda e: e.memset(Sst[:].rearrange("p a c n -> p (a c n)"), 0.0), writes=[tS[0], tS[1]])
                P.op("dve", lambda e: e.memset(carry[:].rearrange("p w a c -> p (w a c)"), 0.0),
                     writes=[t for w_ in tcar for l_ in w_ for t in l_])
            P.op("sp", lambda e, tile=tile: e.dma_start(
                out=xt, in_=x_d[tile * TT:(tile + 1) * TT, :].rearrange("(s p) d -> p s d", p=128)),
                writes=[txt], dma_key="xin")
            for c in range(8):
                b = c % 2
                for s in range(4):
                    tr(bk(b)[:, s * 128:(s + 1) * 128], xt[:, s, c * 128:(c + 1) * 128], cm(CM_ID, 128), [txt, tcm], [tbank[b]])
                cp(x[:, c, :], bk(b), [tbank[b]], [tx[c]], eng="act" if c % 2 else "dve")
            for l in range(nlayers):
                spos[0] = 0
                ffn(l, 1)
                if stop_after == ("ffn1", l):
                    break
                mixer(l)
                if stop_after == ("mixer", l):
                    break
                ffn(l, 2)
            P.barrier()
            sqf = [V16(0, 256), V16(256, 256)]; tsqf = [Trk(), Trk()]
            rstd = Vf(512, 512); trs = Trk()
            tyf = [Trk() for _ in range(8)]
            if stop_after is None:
                for c in range(8):
                    act(sqf[c % 2], x[:, c, :], AF.Square, [tx[c]], [tsqf[c % 2]])
                    mm(bk(4), onesmeanb[:], sqf[c % 2], c == 0, c == 7, [tsqf[c % 2]], [tbank[4]])
                act(rstd, bk(4), AF.Ln, [tbank[4]], [trs], bias=NORM_EPS)
                act(rstd, rstd, AF.Exp, [trs], [trs], scale=-0.5)
                for c in range(8):
                    stt(yf[:, c, :], x[:, c, :], cv(0, V_FIN, c), rstd, ALU.mult, ALU.mult, [tx[c], trs, tcv], [tyf[c]])
            else:
                for c in range(8):
                    cp(yf[:, c, :], x[:, c, :], [tx[c]], [tyf[c]])
            txo = Trk()
            for s in range(4):
                for hf in range(2):
                    b = 2 + hf
                    for ci in range(4):
                        c = hf * 4 + ci
                        tr(bk(b)[:, ci * 128:(ci + 1) * 128], yf[:, c, s * 128:(s + 1) * 128], cm(CM_ID, 128), [tyf[c], tcm], [tbank[b]])
                    cp(xt[:, s, hf * 512:(hf + 1) * 512], bk(b), [tbank[b]], [txo], eng="act" if hf else "dve")
            tok = P.op("sp", lambda e, tile=tile: e.dma_start(
                out=y_d[tile * TT:(tile + 1) * TT, :].rearrange("(s p) d -> p s d", p=128), in_=xt),
                reads=[txo], dma_key="xout")
            P.q["sp"].append(([tok], None, None, False))
        P.run(nc)
    return nc


_CACHE = {}


def kernel(**inputs):
    inp = {k: np.asarray(v) for k, v in inputs.items()}
    x = inp["x"].astype(np.float32)
    B = x.shape[0]
    per = B // NCORES
    ntiles = per * SEQ // TT
    wstream = build_stream(inp)
    cvec = build_cvec(inp)
    cmat = build_cmat()
    if "nc" not in _CACHE:
        _CACHE["nc"] = build_program(ntiles)
    nc = _CACHE["nc"]
    in_maps = []
    for i in range(NCORES):
        xs = np.ascontiguousarray(x[i * per:(i + 1) * per].reshape(per * SEQ, D))
        in_maps.append({"x": xs, "wstream": wstream, "cvec": cvec, "cmat": cmat})
    res = run_bass_kernel_spmd(nc, in_maps, core_ids=list(range(NCORES)))
    out = np.concatenate([r["y"].reshape(per, SEQ, D) for r in res.results], axis=0)
    return out.astype(np.float32)
```

```python
import contextlib
import math
import numpy as np
import concourse.bass as bass
import concourse.mybir as mybir
from concourse.bass_utils import run_bass_kernel_spmd

F32 = mybir.dt.float32
BF16 = mybir.dt.bfloat16
ALU = mybir.AluOpType
AF = mybir.ActivationFunctionType
AX = mybir.AxisListType

D = 1024
DFF = 2816
NJ = 22
TT = 512
SEQ = 2048
NCORES = 8
R_RING = 6
NORM_EPS = 1e-6
LN_EPS = 1e-5
GN_EPS = 64e-5
C0 = math.exp(-0.5)
DBG = [None]
STATS = {}

(V_N1, V_NM, V_N2, V_LNG, V_LNB, V_MUR, V_MUK, V_MUV, V_W0, V_A0, V_KK, V_KA, V_RK, V_GNG, V_GNB, V_V0,
 V_FIN, V_MUX, V_OMR, V_OMK, V_OMV, V_OMX, V_OKA) = range(23)
NV = 23 * 8

CM_ID = 0
CM_ONESMEAN = 128
CM_ONESBD = 256
CM_MLO = 384
CM_MUPS = 448
CM_MUPI = 512
CM_SCAN = 576
CM_TRIU = 1088
CM_ONES1 = 1216
NCM = 1344


class Trk:
    __slots__ = ("w", "r", "excl")

    def __init__(self, excl=False):
        self.w = None
        self.r = {}
        self.excl = excl


class Prog:
    ENG = ["pe", "act", "dve", "pool", "sp"]

    def __init__(self):
        self.q = {e: [] for e in self.ENG}
        self.cnt = {e: 0 for e in self.ENG}
        self.seen = {e: {} for e in self.ENG}
        self.dma_cnt = {}

    def _emit(self, eng, fn, deps, dma_key=None):
        sn = self.seen[eng]
        d = {}
        for (k, v) in deps:
            if k == "pe" and eng == "pe":
                continue
            if sn.get(k, 0) < v:
                d[k] = max(d.get(k, 0), v)
        for k, v in d.items():
            sn[k] = v
        if dma_key is None:
            self.cnt[eng] += 1
            tok = (eng, self.cnt[eng])
        else:
            self.dma_cnt[dma_key] = self.dma_cnt.get(dma_key, 0) + 16
            tok = (dma_key, self.dma_cnt[dma_key])
        self.q[eng].append((list(d.items()), fn, tok, dma_key is not None))
        return tok

    def op(self, eng, fn, reads=(), writes=(), dma_key=None, extra=()):
        deps = list(extra)
        for t in reads:
            if t.w is not None:
                deps.append(t.w)
            if t.excl:
                deps.extend((k, v) for k, v in t.r.items() if k != eng)
        for t in writes:
            if t.w is not None:
                deps.append(t.w)
            deps.extend(t.r.items())
        tok = self._emit(eng, fn, deps, dma_key)
        k, v = tok
        for t in reads:
            if t.r.get(k, 0) < v:
                t.r[k] = v
        for t in writes:
            t.w = tok
            t.r = {}
        return tok

    def barrier(self, engines=("pe", "act", "dve")):
        toks = [(e, c) for e, c in self.cnt.items() if c > 0 and e != "sp"]
        for e in engines:
            sn = self.seen[e]
            d = {}
            for (k, v) in toks:
                if k == e:
                    continue
                if sn.get(k, 0) < v:
                    d[k] = v
                    sn[k] = v
            if d:
                self.q[e].append((list(d.items()), None, None, False))

    def run(self, nc):
        keys = sorted(set(self.ENG) | set(self.dma_cnt.keys()))
        with contextlib.ExitStack() as st:
            sems = {k: st.enter_context(nc.semaphore("s_" + k)) for k in keys}
            block = st.enter_context(nc.Block())

            waited = set()
            for name in self.ENG:
                for waits, fn, tok, is_dma in self.q[name]:
                    for kv in waits:
                        waited.add(tuple(kv))

            def mk(name):
                def body(e):
                    pending = 0
                    last_idx = max([i for i, it in enumerate(self.q[name]) if it[1] is not None and not it[3]], default=-1)
                    for idx, (waits, fn, tok, is_dma) in enumerate(self.q[name]):
                        for k, v in waits:
                            e.wait_ge(sems[k], v)
                        if fn is None:
                            continue
                        ins = fn(e)
                        if is_dma:
                            ins.then_inc(sems[tok[0]], 16)
                        elif tuple(tok) in waited or idx == last_idx:
                            ins.then_inc(sems[tok[0]], pending + 1)
                            STATS.setdefault(name, []).append(pending + 1)
                            pending = 0
                        else:
                            pending += 1
                return body

            block.tensor(mk("pe"))
            block.scalar(mk("act"))
            block.vector(mk("dve"))
            block.gpsimd(mk("pool"))
            block.sync(mk("sp"))


def in_chunk_order_a():
    return list(range(24, 32)) + list(range(0, 8)) + list(range(16, 24))


def in_chunk_order_b(layer):
    o = [56]
    if layer > 0:
        o.append(57)
    o += list(range(8, 16))
    for c in range(8):
        o += [32 + c, 40 + c, 48 + c]
    return o


def layer_recipe(layer):
    rec = []
    for which in (1, 2):
        if which == 2:
            ca = in_chunk_order_a()
            for i in range(0, len(ca), 2):
                rec.append(("in", layer, ca[i:i + 2]))
            rec.append(("sgu", layer))
            for mp in range(4):
                rec.append(("pj", layer, "a", mp))
            rec.append(("lora", layer))
            cb = in_chunk_order_b(layer)
            for i in range(0, len(cb), 2):
                rec.append(("in", layer, cb[i:i + 2]))
            for mp in range(4):
                rec.append(("pj", layer, "b", mp))
            for mp in range(4):
                rec.append(("pj", layer, "o", mp))
        f = 1 if which == 1 else 2
        for jg in range(11):
            rec.append(("gu", layer, f, 0, jg))
            rec.append(("gu", layer, f, 1, jg))
        for a in range(2):
            for jq in range(6):
                rec.append(("dn", layer, f, a, jq))
    n_ffn = 22 + 12
    ffn1 = rec[:n_ffn]
    rest = rec[n_ffn:]
    return ffn1 + rest


def _kc(arr):
    n = arr.shape[1]
    return np.ascontiguousarray(arr.reshape(8, 128, n).transpose(1, 0, 2)).reshape(128, 8 * n)


def build_stream(inp):
    slots = []
    for layer in range(2):
        w_in = inp["w_in_first"] if layer == 0 else inp["w_in_rest"][layer - 1]
        for r in layer_recipe(layer):
            s = np.zeros((128, 2048), np.float32)
            kind = r[0]
            if kind == "gu":
                _, l, f, half, jg = r
                w = (inp["ffn1_w_gu"] if f == 1 else inp["ffn2_w_gu"])[l]
                base = half * DFF + jg * 256
                s[:] = _kc(w[:, base:base + 256])
            elif kind == "dn":
                _, l, f, a, jq = r
                w = (inp["ffn1_w_down"] if f == 1 else inp["ffn2_w_down"])[l]
                v = s.reshape(128, 4, 512)
                for i in range(4):
                    j = 4 * jq + i
                    if j < NJ:
                        v[:, i, :] = w[j * 128:(j + 1) * 128, a * 512:(a + 1) * 512]
            elif kind == "in":
                _, l, chunks = r
                v = s.reshape(128, 8, 256)
                for jj, ch in enumerate(chunks):
                    cols = w_in[:, ch * 128:min((ch + 1) * 128, w_in.shape[1])]
                    n = cols.shape[1]
                    v[:, :, jj * 128:jj * 128 + n] = cols.reshape(8, 128, n).transpose(1, 0, 2)
            elif kind == "pj":
                _, l, which, mp = r
                w = {"a": inp["w_proj_a"], "b": inp["w_proj_b"], "o": inp["w_out"]}[which][l]
                s[:] = _kc(w[:, mp * 256:(mp + 1) * 256])
            elif kind == "sgu":
                _, l = r
                ws = inp["sgu_w_s"][l]
                s[:, 0:1024] = ws.transpose(2, 0, 1).reshape(128, 1024)
                s[0, 1024:2048] = inp["sgu_b_s"][l].reshape(1024)
            elif kind == "lora":
                _, l = r
                s[0:64, 0:1024] = inp["rwkv_w2"][l]
                s[64:128, 0:1024] = inp["rwkv_a2"][l]
                if l > 0:
                    s[0:32, 1024:2048] = inp["rwkv_v2"][l - 1]
            slots.append(s)
    return np.stack(slots, 0)


def _vec8(v):
    return np.ascontiguousarray(np.asarray(v, np.float32).reshape(8, 128).T)


def build_cvec(inp):
    out = np.zeros((128, 2, NV), np.float32)
    for l in range(2):
        mu = inp["mu_first"] if l == 0 else inp["mu_rest"][l - 1]
        o = out[:, l, :]

        def put(idx, v):
            o[:, idx * 8:(idx + 1) * 8] = _vec8(v)
        put(V_N1, inp["ffn1_norm"][l]); put(V_NM, inp["mix_norm"][l]); put(V_N2, inp["ffn2_norm"][l])
        put(V_LNG, inp["sgu_ln_g"][l]); put(V_LNB, inp["sgu_ln_b"][l])
        put(V_MUR, mu[0:1024]); put(V_MUK, mu[1024:2048]); put(V_MUV, mu[2048:3072])
        put(V_W0, inp["rwkv_w0"][l]); put(V_A0, inp["rwkv_a0"][l]); put(V_KK, inp["rwkv_k_k"][l])
        put(V_KA, inp["rwkv_k_a"][l]); put(V_RK, inp["rwkv_r_k"][l].reshape(1024))
        put(V_GNG, inp["rwkv_gn_g"][l]); put(V_GNB, inp["rwkv_gn_b"][l])
        if l > 0:
            put(V_V0, inp["rwkv_v0"][l - 1])
        put(V_FIN, inp["final_norm"])
        o[:, V_MUX * 8] = mu[3072:3200]
        if l > 0:
            o[0:32, V_MUX * 8 + 1] = mu[3200:3232]
    return out.reshape(128, 2 * NV)


def build_cmat():
    m = np.zeros((128, NCM), np.float32)
    m[:, CM_ID:CM_ID + 128] = np.eye(128)
    m[:, CM_ONESMEAN:CM_ONESMEAN + 128] = 1.0 / 1024.0
    bd = np.zeros((128, 128), np.float32)
    bd[0:64, 0:64] = 1.0
    bd[64:128, 64:128] = 1.0
    m[:, CM_ONESBD:CM_ONESBD + 128] = bd
    i = (np.arange(128) % 64)[:, None]
    j = np.arange(64)[None, :]
    m[:, CM_MLO:CM_MLO + 64] = (j < i)
    m[:, CM_MUPS:CM_MUPS + 64] = (j > i)
    m[:, CM_MUPI:CM_MUPI + 64] = (j >= i)
    sc = np.ones((512,), np.float32)
    sc[::64] = 0.0
    m[:, CM_SCAN:CM_SCAN + 512] = sc[None, :]
    s = np.arange(128)[:, None]
    t = np.arange(128)[None, :]
    m[:, CM_TRIU:CM_TRIU + 128] = (t >= s)
    m[:, CM_ONES1:CM_ONES1 + 128] = 1.0
    return m


def build_program(ntiles, nlayers=2, stop_after=None):
    ntok = ntiles * TT
    nc = bass.Bass("TRN2", target_bir_lowering=False)
    x_d = nc.dram_tensor("x", [ntok, D], F32, kind="ExternalInput").ap()
    rec = [layer_recipe(l) for l in range(2)]
    nslots = sum(len(r) for r in rec)
    ws_d = nc.dram_tensor("wstream", [nslots, 128, 2048], F32, kind="ExternalInput").ap()
    cv_d = nc.dram_tensor("cvec", [128, 2 * NV], F32, kind="ExternalInput").ap()
    cm_d = nc.dram_tensor("cmat", [128, NCM], F32, kind="ExternalInput").ap()
    y_d = nc.dram_tensor("y", [ntok, D], F32, kind="ExternalOutput").ap()
    dbg16 = nc.dram_tensor("dbg16", [8, 128, 4096], BF16, kind="ExternalOutput").ap() if DBG[0] else None
    dbg32 = nc.dram_tensor("dbg32", [8, 128, 512], F32, kind="ExternalOutput").ap() if DBG[0] else None
    P = Prog()
    layer_base = [0, len(rec[0])]

    with contextlib.ExitStack() as st:
        cnt = [0]

        def sb(shape, dt=F32):
            cnt[0] += 1
            return st.enter_context(nc.sbuf_tensor("sb%d" % cnt[0], shape, dt))

        banks = []
        for i in range(8):
            banks.append(st.enter_context(nc.psum_tensor("bank%d" % i, [128, 512], F32)))
        tbank = [Trk(excl=True) for _ in range(8)]

        def bk(i):
            return banks[i][:]

        def bk16(i):
            return banks[i][:].bitcast(BF16)

        x = sb([128, 8, 512]); tx = [Trk() for _ in range(8)]
        ring = [sb([128, 2048], BF16) for _ in range(R_RING)]
        tring = [Trk() for _ in range(R_RING)]
        cvec = sb([128, 2 * NV]); tcv = Trk()
        cmat = sb([128, NCM]); tcm = Trk()
        identb = sb([128, 128], BF16); onesmeanb = sb([128, 128], BF16); onesbdb = sb([128, 128], BF16)
        ones1b = sb([1, 128], BF16)
        Sst = sb([128, 2, 8, 64], BF16); tS = [Trk(), Trk()]
        carry = sb([128, 2, 2, 26]); tcar = [[[Trk() for _ in range(26)] for _ in range(2)] for _ in range(2)]
        cur_tile = [0]
        vfirst = sb([128, 8, 512], BF16); tvf = [Trk() for _ in range(8)]
        M_h = sb([128, 8, 512], BF16)
        M_sga = sb([128, 8, 512], BF16)
        M_sgb = sb([128, 8, 512], BF16)
        M_mer = sb([128, 8, 512], BF16)
        M_yb = sb([128, 8, 512], BF16)
        M_bon = sb([128, 8, 512], BF16)
        lora = sb([128, 2048], BF16)
        U = sb([128, 10240])
        Vr = sb([128, 13312])

        def cv(l, idx, c=0, rows=slice(0, 128)):
            o = l * NV + idx * 8 + c
            return cvec[rows, o:o + 1]

        def cm(off, n, rows=slice(0, 128)):
            return cmat[rows, off:off + n]

        P.op("sp", lambda e: e.dma_start(out=cvec[:], in_=cv_d), writes=[tcv], dma_key="c0")
        P.op("sp", lambda e: e.dma_start(out=cmat[:], in_=cm_d), writes=[tcm], dma_key="c1")
        for l in range(2):
            for (src, dst) in ((V_MUR, V_OMR), (V_MUK, V_OMK), (V_MUV, V_OMV), (V_MUX, V_OMX), (V_KA, V_OKA)):
                P.op("dve", lambda e, l=l, src=src, dst=dst: e.tensor_scalar(
                    out=cvec[:, l * NV + dst * 8:l * NV + dst * 8 + 8], in0=cvec[:, l * NV + src * 8:l * NV + src * 8 + 8],
                    scalar1=-1.0, scalar2=1.0, op0=ALU.mult, op1=ALU.add), reads=[], writes=[tcv])
        P.op("dve", lambda e: e.tensor_copy(out=identb[:], in_=cm(CM_ID, 128)), reads=[tcm], writes=[tcm])
        P.op("dve", lambda e: e.tensor_copy(out=onesmeanb[:], in_=cm(CM_ONESMEAN, 128)), writes=[tcm])
        P.op("dve", lambda e: e.tensor_copy(out=onesbdb[:], in_=cm(CM_ONESBD, 128)), writes=[tcm])
        P.op("dve", lambda e: e.tensor_copy(out=ones1b[:], in_=cmat[0:1, CM_ONES1:CM_ONES1 + 128]), writes=[tcm])
        P.barrier()

        spos = [0]

        def next_slot(layer, expect):
            li = spos[0]
            assert rec[layer][li][0] == expect[0], (rec[layer][li], expect)
            spos[0] += 1
            gi = layer_base[layer] + li
            nslot[0] += 1
            s = nslot[0] % R_RING
            P.op("pool", lambda e, s=s, gi=gi: e.dma_start(out=ring[s][:], in_=ws_d[gi]), writes=[tring[s]],
                 dma_key="w%d" % s)
            return ring[s], tring[s]

        nslot = [0]

        def mm(out, lhsT, rhs, start, stop, reads, writes):
            P.op("pe", lambda e: e.matmul(out, lhsT=lhsT, rhs=rhs, start=start, stop=stop), reads=reads, writes=writes)

        def tr(out, in_, ident, reads, writes):
            P.op("pe", lambda e: e.transpose(out, in_, ident), reads=reads, writes=writes)

        def act(out, in_, func, reads, writes, bias=None, scale=None):
            kw = {}
            if bias is not None:
                kw["bias"] = bias
            if scale is not None:
                kw["scale"] = scale
            P.op("act", lambda e: e.activation(out=out, in_=in_, func=func, **kw), reads=reads, writes=writes)

        def tt(out, in0, in1, op, reads, writes, eng="dve"):
            P.op(eng, lambda e: e.tensor_tensor(out=out, in0=in0, in1=in1, op=op), reads=reads, writes=writes)

        def stt(out, in0, scalar, in1, op0, op1, reads, writes):
            P.op("dve", lambda e: e.scalar_tensor_tensor(out=out, in0=in0, scalar=scalar, in1=in1, op0=op0, op1=op1),
                 reads=reads, writes=writes)

        def tsc(out, in0, s1, s2, op0, op1, reads, writes, eng="dve"):
            P.op(eng, lambda e: e.tensor_scalar(out=out, in0=in0, scalar1=s1, scalar2=s2, op0=op0, op1=op1),
                 reads=reads, writes=writes)

        def cp(out, in_, reads, writes, eng="dve"):
            if eng == "act":
                P.op(eng, lambda e: e.activation(out=out, in_=in_, func=AF.Copy), reads=reads, writes=writes)
            else:
                P.op(eng, lambda e: e.tensor_copy(out=out, in_=in_), reads=reads, writes=writes)

        def recip(out, in_, reads, writes):
            P.op("dve", lambda e: e.reciprocal(out=out, in_=in_), reads=reads, writes=writes)

        def Uf(off, n):
            return U[:, off:off + n]

        def U16(off, n):
            return U[:, off:off + n].bitcast(BF16)

        def Vf(off, n):
            return Vr[:, off:off + n]

        def V16(off, n):
            return Vr[:, off:off + n].bitcast(BF16)

        def rmsnorm(gidx, l, h, th, tmpbase=0):
            sq = [V16(tmpbase, 256), V16(tmpbase + 256, 256)]
            tsq = [Trk(), Trk()]
            rstd = Vf(tmpbase + 512, 512); trs = Trk()
            for c in range(8):
                act(sq[c % 2], x[:, c, :], AF.Square, [tx[c]], [tsq[c % 2]])
                mm(bk(4), onesmeanb[:], sq[c % 2], c == 0, c == 7, [tsq[c % 2]], [tbank[4]])
            act(rstd, bk(4), AF.Ln, [tbank[4]], [trs], bias=NORM_EPS)
            act(rstd, rstd, AF.Exp, [trs], [trs], scale=-0.5)
            for c in range(8):
                stt(h[:, c, :], x[:, c, :], cv(l, gidx, c), rstd, ALU.mult, ALU.mult, [tx[c], trs, tcv], [th[c]])

        def ffn(l, f):
            P.barrier()
            h = U16(0, 2048).rearrange("p (c t) -> p c t", c=8)
            g = U16(2048, 5632).rearrange("p (j t) -> p j t", j=NJ)
            th = [Trk() for _ in range(8)]; tg = [Trk() for _ in range(NJ)]
            sgt = [Uf(7680, 512), Uf(8192, 512)]; tsg = [Trk(), Trk()]
            rmsnorm(V_N1 if f == 1 else V_N2, l, h, th)
            for jg in range(11):
                sg_, tsg_ = next_slot(l, ("gu",))
                su_, tsu_ = next_slot(l, ("gu",))
                sgv = sg_[:].rearrange("p (k n) -> p k n", k=8)
                suv = su_[:].rearrange("p (k n) -> p k n", k=8)
                for jj in range(2):
                    j = 2 * jg + jj
                    bg = j % 2; bu = 2 + j % 2
                    for k in range(8):
                        mm(bk(bg), sgv[:, k, jj * 128:(jj + 1) * 128], h[:, k, :], k == 0, k == 7, [tsg_, th[k]], [tbank[bg]])
                    for k in range(8):
                        mm(bk(bu), suv[:, k, jj * 128:(jj + 1) * 128], h[:, k, :], k == 0, k == 7, [tsu_, th[k]], [tbank[bu]])
                    act(sgt[j % 2], bk(bg), AF.Silu, [tbank[bg]], [tsg[j % 2]])
                    tt(g[:, j, :], sgt[j % 2], bk(bu), ALU.mult, [tsg[j % 2], tbank[bu]], [tg[j]])
            for a in range(2):
                for jq in range(6):
                    sd_, tsd_ = next_slot(l, ("dn",))
                    sdv = sd_[:].rearrange("p (i n) -> p i n", i=4)
                    for i in range(4):
                        j = 4 * jq + i
                        if j >= NJ:
                            continue
                        for m in range(4):
                            bb = (4 if a == 0 else 0) + m
                            mm(bk(bb), sdv[:, i, m * 128:(m + 1) * 128], g[:, j, :], j == 0, j == NJ - 1,
                               [tsd_, tg[j]], [tbank[bb]])
                for m in range(4):
                    c = a * 4 + m
                    bb = (4 if a == 0 else 0) + m
                    stt(x[:, c, :], bk(bb), 0.5, x[:, c, :], ALU.mult, ALU.add, [tbank[bb]], [tx[c]])

        def inproj(slotv, tslot, jj, h, th, b, mcols=128):
            for k in range(8):
                mm(bk(b)[0:mcols, :], slotv[:, k, jj * 128:jj * 128 + mcols], h[:, k, :], k == 0, k == 7,
                   [tslot, th[k]], [tbank[b]])

        class InStream:
            def __init__(self, l, order, h, th, banks=(0, 1)):
                self.l = l; self.order = order; self.h = h; self.th = th; self.i = 0
                self.slot = None; self.banks = banks

            def next(self, mcols=128):
                if self.i % 2 == 0:
                    s_, t_ = next_slot(self.l, ("in",))
                    self.slot = (s_[:].rearrange("p (k n) -> p k n", k=8), t_)
                b = self.banks[self.i % len(self.banks)]
                inproj(self.slot[0], self.slot[1], self.i % 2, self.h, self.th, b, mcols)
                self.i += 1
                return b

        def token_shift(b, l, muidx, omidx, c, cidx, out, tout, rows=slice(0, 128)):
            pw = cur_tile[0] % 2; pr = 1 - pw
            act(out, bk(b)[rows, :], AF.Copy, [tbank[b], tcv], [tout], scale=cv(l, omidx, c, rows))
            cp(carry[rows, pw, l, cidx:cidx + 1], bk(b)[rows, 511:512], [tbank[b]], [tcar[pw][l][cidx]], eng="act")
            stt(out[:, 1:512], bk(b)[rows, 0:511], cv(l, muidx, c, rows), out[:, 1:512], ALU.mult, ALU.add,
                [tbank[b], tcv], [tout])
            stt(out[:, 0:1], carry[rows, pr, l, cidx:cidx + 1], cv(l, muidx, c, rows), out[:, 0:1], ALU.mult, ALU.add,
                [tcar[pr][l][cidx], tcv], [tout])

        def mixer(l):
            P.barrier()
            h = M_h; th = [Trk() for _ in range(8)]
            rmsnorm(V_NM, l, h, th)
            sga = M_sga; tsga = [Trk() for _ in range(8)]
            sgb = M_sgb; tsgb = [Trk() for _ in range(8)]
            mer = M_mer; tmer = [Trk() for _ in range(8)]
            ua = U16(0, 2048).rearrange("p (c t) -> p c t", c=8); tua = [Trk() for _ in range(8)]
            gv = Uf(2048, 4096).rearrange("p (c t) -> p c t", c=8); tgv = [Trk() for _ in range(8)]
            vn = U16(6144, 2048).rearrange("p (c t) -> p c t", c=8); tvn = [Trk() for _ in range(8)]
            vnT = U16(8192, 2048).rearrange("p (c t) -> p c t", c=8); tvnT = [Trk() for _ in range(8)]
            ins = InStream(l, in_chunk_order_a(), h, th, banks=(0, 1, 2, 3))
            sq16 = [V16(1024, 256), V16(1280, 256)]; tsq16 = [Trk(), Trk()]
            gvb = [V16(1536, 256), V16(1792, 256)]; tgvb = [Trk(), Trk()]
            for c in range(8):
                b = ins.next()
                act(gv[:, c, :], bk(b), AF.Gelu_apprx_tanh, [tbank[b]], [tgv[c]])
                act(sq16[c % 2], gv[:, c, :], AF.Square, [tgv[c]], [tsq16[c % 2]])
                cp(gvb[c % 2], gv[:, c, :], [tgv[c]], [tgvb[c % 2]])
                mm(bk(4), onesmeanb[:], gvb[c % 2], c == 0, c == 7, [tgvb[c % 2]], [tbank[4]])
                mm(bk(5), onesmeanb[:], sq16[c % 2], c == 0, c == 7, [tsq16[c % 2]], [tbank[5]])
            mean = Vf(2048, 512); tmean = Trk()
            var = Vf(2560, 512); tvar = Trk()
            act(mean, bk(4), AF.Copy, [tbank[4]], [tmean])
            tt(var, mean, mean, ALU.mult, [tmean], [tvar])
            tt(var, bk(5), var, ALU.subtract, [tbank[5], tvar], [tvar])
            act(var, var, AF.Ln, [tvar], [tvar], bias=LN_EPS)
            act(var, var, AF.Exp, [tvar], [tvar], scale=-0.5)
            tmpl = [Vf(3072, 512), Vf(3584, 512)]; ttmpl = [Trk(), Trk()]
            for c in range(8):
                t_ = tmpl[c % 2]; tt_ = ttmpl[c % 2]
                tt(t_, gv[:, c, :], mean, ALU.subtract, [tgv[c], tmean], [tt_])
                tt(t_, t_, var, ALU.mult, [tt_, tvar], [tt_])
                act(vn[:, c, :], t_, AF.Identity, [tt_, tcv], [tvn[c]], bias=cv(l, V_LNB, c), scale=cv(l, V_LNG, c))
                b = ins.next()
                act(sga[:, c, :], bk(b), AF.Sigmoid, [tbank[b]], [tsga[c]])
            for c in range(8):
                b = ins.next()
                act(ua[:, c, :], bk(b), AF.Gelu_apprx_tanh, [tbank[b]], [tua[c]])
            ssg, tssg = next_slot(l, ("sgu",))
            wsT = V16(4096, 512).rearrange("p (g t) -> p g t", g=8); twsT = Trk()
            tt(wsT, ssg[:, 0:1024].rearrange("p (g t) -> p g t", g=8),
               cm(CM_TRIU, 128).unsqueeze(1).broadcast_to([128, 8, 128]), ALU.mult, [tssg, tcm], [twsT])
            for c in range(8):
                b = 6 + (c // 2) % 2
                off = (c % 2) * 512
                for cc in range(4):
                    tr(bk16(b)[:, off + cc * 128:off + (cc + 1) * 128], vn[:, c, cc * 128:(cc + 1) * 128], identb[:],
                       [tvn[c], tcm], [tbank[b]])
                cp(vnT[:, c, :], bk16(b)[:, off:off + 512], [tbank[b]], [tvnT[c]], eng="act" if c % 2 else "dve")
            for c in range(8):
                b = 2 + c % 2
                for cc in range(4):
                    mm(bk(b)[:, cc * 128:(cc + 1) * 128], vnT[:, c, cc * 128:(cc + 1) * 128], wsT[:, c, :], True, False,
                       [tvnT[c], twsT], [tbank[b]])
                    mm(bk(b)[:, cc * 128:(cc + 1) * 128], ones1b[0:1, :], ssg[0:1, 1024 + c * 128:1024 + (c + 1) * 128],
                       False, True, [tssg, tcm], [tbank[b]])
                tt(ua[:, c, :], bk(b), ua[:, c, :], ALU.mult, [tbank[b]], [tua[c]])
            for mp in range(4):
                sp_, tsp_ = next_slot(l, ("pj",))
                spv = sp_[:].rearrange("p (k n) -> p k n", k=8)
                for mm_ in range(2):
                    m = 2 * mp + mm_
                    b = m % 2
                    for c in range(8):
                        mm(bk(b), spv[:, c, mm_ * 128:(mm_ + 1) * 128], ua[:, c, :], c == 0, c == 7, [tsp_, tua[c]], [tbank[b]])
                    tt(mer[:, m, :], bk(b), sga[:, m, :], ALU.mult, [tbank[b], tsga[m]], [tmer[m]])
            if DBG[0] == 'sgu':
                spos[0] += 1 + (len(in_chunk_order_b(l)) + 1) // 2 + 4
            if DBG[0] != 'sgu':
                P.barrier()
                til = [U16(i * 2048, 2048).rearrange("p (c t) -> p c t", c=8) for i in range(5)]
                ttil = [[Trk() for _ in range(8)] for _ in range(5)]
                Rt, Kt, Bt, At, Vt = til
                bon = M_bon; tbon = [Trk() for _ in range(8)]
                TA = [Vf(i * 512, 512) for i in range(11)]; tTA = [Trk() for _ in range(11)]
                TB = [Vf(7680 + i * 512, 512) for i in range(11)]; tTB = [Trk() for _ in range(11)]
                T = TA; tT = tTA
                sqb = V16(5632, 256); tsqb = Trk()
                rkb = V16(5888, 256); trkb = Trk()
                twad = V16(6144, 256); ttw = Trk()
                vres = V16(6400, 256); tvres = Trk()
                PC = Vf(6656, 64).rearrange("p (c q) -> p c q", c=8); tPC = Trk()
                slo_r, tslo_r = next_slot(l, ("lora",))
                slo = lora; tslo = Trk()
                cp(lora[:], slo_r[:], [tslo_r], [tslo])
                ins = InStream(l, in_chunk_order_b(l), h, th, banks=(0, 1, 7, 6))
                b = ins.next()
                token_shift(b, l, V_MUX, V_OMX, 0, 24, T[0], tT[0])
                act(twad[0:64, :], T[0][0:64, :], AF.Tanh, [tT[0]], [ttw])
                act(twad[64:128, :], T[0][64:128, :], AF.Copy, [tT[0]], [ttw])
                if l > 0:
                    b = ins.next(mcols=32)
                    token_shift(b, l, V_MUX, V_OMX, 1, 25, T[1][0:32, :], tT[1], rows=slice(0, 32))
                    cp(vres[0:32, :], T[1][0:32, :], [tT[1]], [tvres])
                for c in range(8):
                    b = ins.next()
                    act(sgb[:, c, :], bk(b), AF.Sigmoid, [tbank[b]], [tsgb[c]])
                def stageA0(c):
                        T, tT = (TA, tTA) if c % 2 == 0 else (TB, tTB)
                        rb, kb, vb = T[0], T[1], T[2]
                        b = ins.next(); token_shift(b, l, V_MUR, V_OMR, c, c, rb, tT[0])
                        b = ins.next(); token_shift(b, l, V_MUK, V_OMK, c, 8 + c, kb, tT[1])
                        b = ins.next(); token_shift(b, l, V_MUV, V_OMV, c, 16 + c, vb, tT[2])
                def stageA1(c):
                        T, tT = (TA, tTA) if c % 2 == 0 else (TB, tTB)
                        rb, kb, vb = T[0], T[1], T[2]
                        mm(bk(2), slo[0:64, c * 128:(c + 1) * 128], twad[0:64, :], True, True, [tslo, ttw], [tbank[2]])
                        mm(bk(3), slo[64:128, c * 128:(c + 1) * 128], twad[64:128, :], True, True, [tslo, ttw], [tbank[3]])
                        act(T[3], bk(2), AF.Sigmoid, [tbank[2], tcv], [tT[3]], bias=cv(l, V_W0, c))
                        act(T[4], bk(3), AF.Sigmoid, [tbank[3], tcv], [tT[4]], bias=cv(l, V_A0, c))
                        P.op("dve", lambda e, o_=T[5], i_=T[3]: e.tensor_tensor_scan(out=o_, data0=cm(CM_SCAN, 512), data1=i_, initial=0.0,
                                                                   op0=ALU.mult, op1=ALU.add), reads=[tT[3], tcm], writes=[tT[5]])
                        tt(T[6], T[5], T[3], ALU.subtract, [tT[5], tT[3]], [tT[6]])
                        act(T[7], T[5], AF.Exp, [tT[5]], [tT[7]], scale=-C0)
                        act(T[8], T[5], AF.Exp, [tT[5]], [tT[8]], scale=C0)
                        act(T[6], T[6], AF.Exp, [tT[6]], [tT[6]], scale=-C0)
                        cp(PC[:, c, :], T[7].rearrange("p (q t) -> p q t", q=8)[:, :, 63], [tT[7]], [tPC])
                        act(sqb, kb, AF.Square, [tT[1], tcv], [tsqb], scale=cv(l, V_KK, c))
                        mm(bk(4), onesbdb[:], sqb, True, True, [tsqb, tcm], [tbank[4]])
                        tsc(T[10], bk(4), 1e-18, None, ALU.max, ALU.bypass, [tbank[4]], [tT[10]])
                        act(T[10], T[10], AF.Ln, [tT[10]], [tT[10]])
                        act(T[10], T[10], AF.Exp, [tT[10]], [tT[10]], scale=-0.5)
                        stt(T[9], kb, cv(l, V_KK, c), T[10], ALU.mult, ALU.mult, [tT[1], tT[10], tcv], [tT[9]])
                        if l > 0:
                            mm(bk(5)[:, :], slo[0:32, 1024 + c * 128:1024 + (c + 1) * 128], vres[0:32, :], True, True,
                               [tslo, tvres], [tbank[5]])
                            act(T[5], bk(5), AF.Sigmoid, [tbank[5], tcv], [tT[5]], bias=cv(l, V_V0, c))
                def stageB(c):
                        T, tT = (TA, tTA) if c % 2 == 0 else (TB, tTB)
                        rb, kb, vb = T[0], T[1], T[2]
                        tsc(T[3], T[4], cv(l, V_KA, c), cv(l, V_OKA, c), ALU.mult, ALU.add, [tT[4], tcv], [tT[3]])
                        tt(T[3], kb, T[3], ALU.mult, [tT[1], tT[3]], [tT[3]])
                        tt(T[10], T[9], T[4], ALU.mult, [tT[9], tT[4]], [tT[10]])
                        if l > 0:
                            tt(T[4], vfirst[:, c, :], vb, ALU.subtract, [tvf[c], tT[2]], [tT[4]])
                            tt(T[4], T[4], T[5], ALU.mult, [tT[4], tT[5]], [tT[4]])
                            tt(vb, vb, T[4], ALU.add, [tT[2], tT[4]], [tT[2]])
                        else:
                            cp(vfirst[:, c, :], vb, [tT[2]], [tvf[c]], eng="act")
                        stt(M_yb[:, c, :], rb, cv(l, V_RK, c), T[3], ALU.mult, ALU.mult, [tT[0], tT[3], tcv], [trk8[c]])
                        tt(Rt[:, c, :], rb, T[7], ALU.mult, [tT[0], tT[7]], [ttil[0][c]])
                        tt(Kt[:, c, :], T[3], T[8], ALU.mult, [tT[3], tT[8]], [ttil[1][c]])
                        tt(Bt[:, c, :], T[10], T[8], ALU.mult, [tT[10], tT[8]], [ttil[2][c]])
                        stt(At[:, c, :], T[9], -1.0, T[6], ALU.mult, ALU.mult, [tT[9], tT[6]], [ttil[3][c]])
                        cp(Vt[:, c, :], vb, [tT[2]], [ttil[4][c]], eng="act")
                trk8 = [Trk() for _ in range(8)]
                stageA0(0)
                for c in range(8):
                    if c >= 1:
                        stageB(c - 1)
                    if c + 1 <= 7:
                        stageA0(c + 1)
                    stageA1(c)
                stageB(7)
                for c in range(8):
                    b = 6 if c % 2 == 0 else 2
                    mm(bk(b), onesbdb[:], M_yb[:, c, :], True, True, [trk8[c], tcm], [tbank[b]])
                    tt(bon[:, c, :], bk(b), Vt[:, c, :], ALU.mult, [tbank[b], ttil[4][c]], [tbon[c]])
            if DBG[0] == 'prep':
                spos[0] += 4
                for i in range(5):
                    tok = P.op("sp", lambda e, i=i: e.dma_start(out=dbg16[i], in_=U[:, i * 2048:(i + 1) * 2048].bitcast(BF16)),
                               reads=ttil[i], dma_key="dbg")
                    P.q["sp"].append(([tok], None, None, False))
                for i in range(8):
                    tok = P.op("sp", lambda e, i=i: e.dma_start(out=dbg32[i], in_=T[3 + i]), reads=[tT[3 + i]], dma_key="dbg")
                    P.q["sp"].append(([tok], None, None, False))
            if DBG[0] not in ('sgu', 'prep'):
                P.barrier()
                def bd(off):
                    return V16(off, 512).rearrange("p (c n) -> p c n", c=8)
                def st64(off):
                    return V16(off, 256).rearrange("p (c n) -> p c n", c=8)
                Abd = [bd(0), bd(512)]; Bbd = [bd(1024), bd(1536)]; Ttb = [bd(2048), bd(2560)]
                tAbd = [[Trk(), Trk()], [Trk(), Trk()]]; tBbd = [[Trk(), Trk()], [Trk(), Trk()]]
                tTt = [[Trk(), Trk()], [Trk(), Trk()]]
                AkT = st64(3072); ArbT = st64(3328); ArkT = st64(3584)
                Kst = st64(3840); Bst = st64(4096); Vst = st64(4352); Xs = st64(4608); Us = st64(4864); ynb = st64(5120)
                tAk, tArb, tArk, tKst, tBst, tVst, tXs, tUs, tynb = [Trk() for _ in range(9)]
                ysb = Vf(6144, 512); tysb = Trk()
                stat = Vf(6720, 64); tstat = Trk()
                ysq = Vf(6784, 512); tysq = Trk()
                P.op("dve", lambda e: e.memset(Abd[0], 0.0), writes=tAbd[0])
                P.op("dve", lambda e: e.memset(Bbd[0], 0.0), writes=tBbd[0])
                S = Sst[:, l, :, :]
                R0 = slice(0, 64); R1 = slice(64, 128)
                RW = (R0, R1)
                idb = identb[:].unsqueeze(1).broadcast_to([128, 8, 128])
                v3 = lambda ap, n=8: ap.rearrange("p (c n) -> p c n", c=n)
                for q in range(8):
                    cs = slice(q * 64, (q + 1) * 64)
                    prods = [(3, 2, 0), (2, 3, 1), (1, 3, 2), (2, 0, 3), (1, 0, 4)]
                    for hh in range(2):
                        rows = RW[hh]
                        for (li, ri, b) in prods:
                            for c in range(8):
                                mm(bk(b)[rows, c * 64:(c + 1) * 64], til[li][rows, c, cs], til[ri][rows, c, cs], True, True,
                                   [ttil[li][c], ttil[ri][c]], [tbank[b]])
                    for hh in range(2):
                        rows = RW[hh]
                        tt(Abd[0][rows, :, hh * 64:(hh + 1) * 64], v3(bk(0)[rows, :]),
                           cm(CM_MLO, 64, rows).unsqueeze(1).broadcast_to([64, 8, 64]), ALU.mult, [tbank[0], tcm], tAbd[0])
                        tt(Bbd[0][rows, :, hh * 64:(hh + 1) * 64], v3(bk(1)[rows, :]),
                           cm(CM_MUPS, 64, rows).unsqueeze(1).broadcast_to([64, 8, 64]), ALU.mult, [tbank[1], tcm], tBbd[0])
                    for (dst, tdst, moff, b) in ((AkT, tAk, CM_MUPS, 2), (ArbT, tArb, CM_MUPI, 3), (ArkT, tArk, CM_MUPI, 4)):
                        tt(dst, v3(bk(b)), cm(moff, 64).unsqueeze(1).broadcast_to([128, 8, 64]), ALU.mult, [tbank[b], tcm], [tdst])
                    for hh in range(2):
                        rows = RW[hh]
                        for (src, tsrc, b, off) in ((Kt, ttil[1], 5, 0), (Bt, ttil[2], 5, 512), (Vt, ttil[4], 6, 0)):
                            for c in range(8):
                                tr(bk16(b)[rows, off + c * 64:off + (c + 1) * 64], src[rows, c, cs], identb[rows, rows],
                                   [tsrc[c], tcm], [tbank[b]])
                    cp(Kst, v3(bk16(5)[:, 0:512]), [tbank[5]], [tKst], eng="act")
                    cp(Bst, v3(bk16(5)[:, 512:1024]), [tbank[5]], [tBst], eng="act")
                    cp(Vst, v3(bk16(6)[:, 0:512]), [tbank[6]], [tVst], eng="act")
                    def x_terms(hh):
                        rows = RW[hh]
                        for c in range(8):
                            osl = slice(c * 64, (c + 1) * 64)
                            mm(bk(5)[rows, osl], AkT[rows, c, :], Vst[rows, c, :], c == 0, False, [tAk, tVst], [tbank[5]])
                            mm(bk(5)[rows, osl], At[rows, c, cs], S[rows, c, :], False, True, [ttil[3][c], tS[l]], [tbank[5]])
                    def y1_terms(hh):
                        rows = RW[hh]
                        for c in range(8):
                            osl = slice(c * 64, (c + 1) * 64)
                            mm(bk(6)[rows, osl], ArkT[rows, c, :], Vst[rows, c, :], c == 0, False, [tArk, tVst], [tbank[6]])
                            mm(bk(6)[rows, osl], Rt[rows, c, cs], S[rows, c, :], False, False, [ttil[0][c], tS[l]], [tbank[6]])
                    def s1_terms(hh):
                        rows = RW[hh]
                        for c in range(8):
                            osl = slice(c * 64, (c + 1) * 64)
                            mm(bk(1)[rows, osl], Kst[rows, c, :], Vst[rows, c, :], c == 0, False, [tKst, tVst], [tbank[1]])
                            mm(bk(1)[rows, osl], identb[rows, rows], S[rows, c, :], False, False, [tcm, tS[l]], [tbank[1]])
                    def ys2_terms(hh):
                        rows = RW[hh]
                        for c in range(8):
                            osl = slice(c * 64, (c + 1) * 64)
                            mm(bk(6)[rows, osl], ArbT[rows, c, :], Us[rows, c, :], False, True, [tArb, tUs], [tbank[6]])
                        for c in range(8):
                            osl = slice(c * 64, (c + 1) * 64)
                            mm(bk(1)[rows, osl], Bst[rows, c, :], Us[rows, c, :], False, True, [tBst, tUs], [tbank[1]])
                    cur = 0
                    bT = (0, 3); bA = (1, 4); bB = (2, 7)
                    for lev in range(6):
                        nxt = 1 - cur
                        last = lev == 5
                        for half in range(2):
                            for ci in range(4):
                                c = half * 4 + ci
                                osl = slice(ci * 128, (ci + 1) * 128)
                                if lev > 0:
                                    mm(bk(bT[half])[:, osl], Abd[cur][:, c, :], Ttb[cur][:, c, :], True, True,
                                       [tAbd[cur][half], tTt[cur][half]], [tbank[bT[half]]])
                                if not last:
                                    mm(bk(bA[half])[:, osl], Bbd[cur][:, c, :], Abd[cur][:, c, :], True, True,
                                       [tAbd[cur][half], tBbd[cur][half]], [tbank[bA[half]]])
                                    if lev < 4:
                                        mm(bk(bB[half])[:, osl], Abd[cur][:, c, :], Bbd[cur][:, c, :], True, True,
                                           [tAbd[cur][half], tBbd[cur][half]], [tbank[bB[half]]])
                        fill = {0: (x_terms, 0), 1: (y1_terms, 0), 2: (x_terms, 1), 3: (y1_terms, 1), 5: (s1_terms, 0)}.get(lev)
                        if fill is not None:
                            fill[0](fill[1])
                        if lev == 0:
                            tt(Ttb[nxt], Bbd[0], idb, ALU.add, tBbd[0] + [tcm], tTt[nxt])
                        for half in range(2):
                            hs = slice(half * 4, half * 4 + 4)
                            if lev > 0:
                                tt(Ttb[nxt][:, hs, :], Ttb[cur][:, hs, :], v3(bk(bT[half]), 4), ALU.add,
                                   [tTt[cur][half], tbank[bT[half]]], [tTt[nxt][half]])
                            if not last:
                                cp(Abd[nxt][:, hs, :], v3(bk(bA[half]), 4), [tbank[bA[half]]], [tAbd[nxt][half]], eng="act")
                                if lev < 4:
                                    cp(Bbd[nxt][:, hs, :], v3(bk(bB[half]), 4), [tbank[bB[half]]], [tBbd[nxt][half]],
                                       eng="dve" if half == 0 else "act")
                        cur = nxt
                    Tfin = Ttb[cur]; tTfin = tTt[cur]
                    cp(Xs, v3(bk(5)), [tbank[5]], [tXs])
                    for c in range(8):
                        osl = slice(c * 64, (c + 1) * 64)
                        mm(bk(7)[:, osl], Tfin[:, c, :], Xs[:, c, :], True, True, [tTfin[c // 4], tXs], [tbank[7]])
                    s1_terms(1)
                    cp(Us, v3(bk(7)), [tbank[7]], [tUs], eng="act")
                    ys2_terms(0); ys2_terms(1)
                    tt(S, v3(bk(1)), PC[:, :, q:q + 1].broadcast_to([128, 8, 64]), ALU.mult, [tbank[1], tPC], [tS[l]])
                    act(ysb, bk(6), AF.Copy, [tbank[6]], [tysb])
                    act(ysq, bk(6), AF.Square, [tbank[6]], [tysq])
                    s1 = stat[:, 0:8]; s2 = stat[:, 8:16]; mean = stat[:, 16:24]; msq = stat[:, 24:32]; var = stat[:, 32:40]
                    P.op("dve", lambda e, s1=s1: e.tensor_reduce(out=s1, in_=v3(ysb), axis=AX.X, op=ALU.add),
                         reads=[tysb], writes=[tstat])
                    P.op("dve", lambda e, s2=s2: e.tensor_reduce(out=s2, in_=v3(ysq), axis=AX.X, op=ALU.add),
                         reads=[tysq, tstat], writes=[tstat])
                    tsc(mean, s1, 1.0 / 64, None, ALU.mult, ALU.bypass, [tstat], [tstat])
                    tt(msq, mean, mean, ALU.mult, [tstat], [tstat])
                    stt(var, s2, 1.0 / 64, msq, ALU.mult, ALU.subtract, [tstat], [tstat])
                    act(var, var, AF.Sqrt, [tstat], [tstat], bias=GN_EPS)
                    recip(var, var, [tstat], [tstat])
                    y3 = v3(ysb)
                    tt(y3, y3, mean.unsqueeze(2).broadcast_to([128, 8, 64]), ALU.subtract, [tysb, tstat], [tysb])
                    tt(ynb, y3, var.unsqueeze(2).broadcast_to([128, 8, 64]), ALU.mult, [tysb, tstat], [tynb])
                    for hh in range(2):
                        rows = RW[hh]
                        b = 7 if hh == 0 else 5
                        for c in range(8):
                            tr(bk16(b)[rows, 512 + c * 64:512 + (c + 1) * 64], ynb[rows, c, :], identb[rows, rows], [tynb, tcm], [tbank[b]])
                        cp(M_yb[rows, :, cs], v3(bk16(b)[rows, 512:1024]), [tbank[b]], [tyb_all], eng="act")
                tmpa = [ysb, ysq]
                ttmpa = [tysb, tysq]
                for c in range(8):
                    t_ = tmpa[c % 2]; tt__ = ttmpa[c % 2]
                    act(t_, M_yb[:, c, :], AF.Identity, [tyb_all, tcv], [tt__], bias=cv(l, V_GNB, c), scale=cv(l, V_GNG, c))
                    tt(M_yb[:, c, :], t_, bon[:, c, :], ALU.add, [tt__, tbon[c]], [tybc[c]])
                for mp in range(4):
                    sp_, tsp_ = next_slot(l, ("pj",))
                    spv = sp_[:].rearrange("p (k n) -> p k n", k=8)
                    for mm_ in range(2):
                        m = 2 * mp + mm_
                        b = m % 2
                        for c in range(8):
                            mm(bk(b), spv[:, c, mm_ * 128:(mm_ + 1) * 128], M_yb[:, c, :], c == 0, c == 7, [tsp_, tybc[c]], [tbank[b]])
                        t_ = tmpa[m % 2]; tt__ = ttmpa[m % 2]
                        tt(t_, bk(b), sgb[:, m, :], ALU.mult, [tbank[b], tsgb[m]], [tt__])
                        tt(mer[:, m, :], t_, mer[:, m, :], ALU.add, [tt__], [tmer[m]])
            for mp in range(4):
                sp_, tsp_ = next_slot(l, ("pj",))
                spv = sp_[:].rearrange("p (k n) -> p k n", k=8)
                for mm_ in range(2):
                    m = 2 * mp + mm_
                    b = 2 + m % 2
                    for c in range(8):
                        mm(bk(b), spv[:, c, mm_ * 128:(mm_ + 1) * 128], mer[:, c, :], c == 0, c == 7, [tsp_, tmer[c]], [tbank[b]])
                    tt(x[:, m, :], bk(b), x[:, m, :], ALU.add, [tbank[b]], [tx[m]])

        tyb_all = Trk()
        tQ1g = Trk(); tP1g = Trk()
        tybc = [Trk() for _ in range(8)]

        tiles_per_seq = SEQ // TT
        xt = Uf(0, 4096).rearrange("p (s d) -> p s d", s=4)
        yf = Uf(4096, 4096).rearrange("p (c t) -> p c t", c=8)
        for tile in range(ntiles):
            cur_tile[0] = tile
            P.barrier()
            txt = Trk()
            if tile % tiles_per_seq == 0:
                P.op("dve", lambda e: e.memset(Sst[:].rearrange("p a c n -> p (a c n)"), 0.0), writes=[tS[0], tS[1]])
                P.op("dve", lambda e: e.memset(carry[:].rearrange("p w a c -> p (w a c)"), 0.0),
                     writes=[t for w_ in tcar for l_ in w_ for t in l_])
            P.op("sp", lambda e, tile=tile: e.dma_start(
                out=xt, in_=x_d[tile * TT:(tile + 1) * TT, :].rearrange("(s p) d -> p s d", p=128)),
                writes=[txt], dma_key="xin")
            for c in range(8):
                b = c % 2
                for s in range(4):
                    tr(bk(b)[:, s * 128:(s + 1) * 128], xt[:, s, c * 128:(c + 1) * 128], cm(CM_ID, 128), [txt, tcm], [tbank[b]])
                cp(x[:, c, :], bk(b), [tbank[b]], [tx[c]], eng="act" if c % 2 else "dve")
            for l in range(nlayers):
                spos[0] = 0
                ffn(l, 1)
                if stop_after == ("ffn1", l):
                    break
                mixer(l)
                if stop_after == ("mixer", l):
                    break
                ffn(l, 2)
            P.barrier()
            sqf = [V16(0, 256), V16(256, 256)]; tsqf = [Trk(), Trk()]
            rstd = Vf(512, 512); trs = Trk()
            tyf = [Trk() for _ in range(8)]
            if stop_after is None:
                for c in range(8):
                    act(sqf[c % 2], x[:, c, :], AF.Square, [tx[c]], [tsqf[c % 2]])
                    mm(bk(4), onesmeanb[:], sqf[c % 2], c == 0, c == 7, [tsqf[c % 2]], [tbank[4]])
                act(rstd, bk(4), AF.Ln, [tbank[4]], [trs], bias=NORM_EPS)
                act(rstd, rstd, AF.Exp, [trs], [trs], scale=-0.5)
                for c in range(8):
                    stt(yf[:, c, :], x[:, c, :], cv(0, V_FIN, c), rstd, ALU.mult, ALU.mult, [tx[c], trs, tcv], [tyf[c]])
            else:
                for c in range(8):
                    cp(yf[:, c, :], x[:, c, :], [tx[c]], [tyf[c]])
            txo = Trk()
            for s in range(4):
                for hf in range(2):
                    b = 2 + hf
                    for ci in range(4):
                        c = hf * 4 + ci
                        tr(bk(b)[:, ci * 128:(ci + 1) * 128], yf[:, c, s * 128:(s + 1) * 128], cm(CM_ID, 128), [tyf[c], tcm], [tbank[b]])
                    cp(xt[:, s, hf * 512:(hf + 1) * 512], bk(b), [tbank[b]], [txo], eng="act" if hf else "dve")
            tok = P.op("sp", lambda e, tile=tile: e.dma_start(
                out=y_d[tile * TT:(tile + 1) * TT, :].rearrange("(s p) d -> p s d", p=128), in_=xt),
                reads=[txo], dma_key="xout")
            P.q["sp"].append(([tok], None, None, False))
        P.run(nc)
    return nc


_CACHE = {}


def kernel(**inputs):
    inp = {k: np.asarray(v) for k, v in inputs.items()}
    x = inp["x"].astype(np.float32)
    B = x.shape[0]
    per = B // NCORES
    ntiles = per * SEQ // TT
    wstream = build_stream(inp)
    cvec = build_cvec(inp)
    cmat = build_cmat()
    if "nc" not in _CACHE:
        _CACHE["nc"] = build_program(ntiles)
    nc = _CACHE["nc"]
    in_maps = []
    for i in range(NCORES):
        xs = np.ascontiguousarray(x[i * per:(i + 1) * per].reshape(per * SEQ, D))
        in_maps.append({"x": xs, "wstream": wstream, "cvec": cvec, "cmat": cmat})
    res = run_bass_kernel_spmd(nc, in_maps, core_ids=list(range(NCORES)))
    out = np.concatenate([r["y"].reshape(per, SEQ, D) for r in res.results], axis=0)
    return out.astype(np.float32)
```
